# Optimizing a Trainium2 kernel written in Bass

```python
import math
import jax, jax.numpy as jnp
from jax import lax
import numpy as np

D_MODEL = 1024
BATCH = 4
SEQ = 8192
DEPTH = 2

N_META = 16
Q_BLOCK = 128
HEAD_DIM = 64
N_HEADS_A = D_MODEL // (2 * HEAD_DIM)
N_MAPS_A = 2 * N_HEADS_A
N_HEADS_B = D_MODEL // HEAD_DIM
N_BUCKETS = 32
MAX_EXACT = N_BUCKETS // 2
MAX_DISTANCE = 128
N_GROUPS = 4
EXPERTS_PER_GROUP = 8
N_EXPERTS = N_GROUPS * EXPERTS_PER_GROUP
TOP_K_IN_GROUP = 2
D_EXPERT = 512
N_MIXERS = 2
N_LAYERS_A = (DEPTH + 1) // 2
N_LAYERS_B = DEPTH // 2
DN_ALPHA = (2 * DEPTH) ** 0.25
DN_BETA = (8 * DEPTH) ** -0.25
LN_EPS = 1e-5
NEG_INF = -1e30

kernel_name = 'hybrid_diffattn_fox_hmoe_deepnorm'


def layer_norm(x, g, b):
    xf = x.astype(jnp.float32)
    mu = jnp.mean(xf, axis=-1, keepdims=True)
    var = jnp.mean(jnp.square(xf - mu), axis=-1, keepdims=True)
    y = (xf - mu) * lax.rsqrt(var + LN_EPS) * g.astype(jnp.float32) + b.astype(jnp.float32)
    return y.astype(x.dtype)


def rms_norm(x, w):
    xf = x.astype(jnp.float32)
    y = xf * lax.rsqrt(jnp.mean(xf * xf, axis=-1, keepdims=True) + LN_EPS) * w.astype(jnp.float32)
    return y.astype(x.dtype)


def t5_bucket(dist):
    n = jnp.maximum(dist, 0)
    nf = jnp.maximum(n, 1).astype(jnp.float32)
    large = MAX_EXACT + (jnp.log(nf / MAX_EXACT) / math.log(MAX_DISTANCE / MAX_EXACT)
                         * (N_BUCKETS - MAX_EXACT)).astype(jnp.int32)
    large = jnp.minimum(large, N_BUCKETS - 1)
    return jnp.where(n < MAX_EXACT, n, large)


def diff_lambda_init(layer_idx):
    return 0.8 - 0.6 * math.exp(-0.3 * layer_idx)


def sweep_causal_blocks(block_fn, seq_len):
    o_meta = block_fn(0, N_META)
    n_blocks = (seq_len - N_META) // Q_BLOCK
    o_real = lax.map(lambda j: block_fn(N_META + j * Q_BLOCK, Q_BLOCK),
                     jnp.arange(n_blocks, dtype=jnp.int32))
    nb, bsz, _, nh, e = o_real.shape
    o_real = jnp.transpose(o_real, (1, 0, 2, 3, 4)).reshape(bsz, nb * Q_BLOCK, nh, e)
    return jnp.concatenate([o_meta, o_real], axis=1)


def differential_attention(h, w_qkv, lam, subln_w, w_o, rel_bias, lambda_init):
    bsz, seq_len, d = h.shape
    qkv = h @ w_qkv
    q = qkv[..., :d].reshape(bsz, seq_len, N_MAPS_A, HEAD_DIM) * (HEAD_DIM ** -0.5)
    k = qkv[..., d:2 * d].reshape(bsz, seq_len, N_MAPS_A, HEAD_DIM)
    v = qkv[..., 2 * d:].reshape(bsz, seq_len, N_HEADS_A, 2 * HEAD_DIM)
    lamf = lam.astype(jnp.float32)
    lam_full = (jnp.exp(jnp.sum(lamf[0] * lamf[1])) - jnp.exp(jnp.sum(lamf[2] * lamf[3]))
                + lambda_init)
    bias_tab = rel_bias.astype(jnp.float32)
    k_pos = jnp.arange(seq_len, dtype=jnp.int32)

    def block_fn(start, size):
        q_blk = lax.dynamic_slice_in_dim(q, start, size, axis=1)
        q_pos = start + jnp.arange(size, dtype=jnp.int32)
        s = jnp.einsum('bqmd,bkmd->bmqk', q_blk, k, preferred_element_type=jnp.float32)
        dist = q_pos[:, None] - k_pos[None, :]
        s = s + jnp.transpose(bias_tab[t5_bucket(dist)], (2, 0, 1))[None]
        s = jnp.where((dist >= 0)[None, None], s, NEG_INF)
        p = jax.nn.softmax(s, axis=-1).reshape(bsz, N_HEADS_A, 2, size, seq_len)
        a = p[:, :, 0] - lam_full * p[:, :, 1]
        return jnp.einsum('bhqk,bkhe->bqhe', a.astype(v.dtype), v)

    o = sweep_causal_blocks(block_fn, seq_len)
    o = rms_norm(o, subln_w) * (1.0 - lambda_init)
    return o.reshape(bsz, seq_len, d) @ w_o


def forgetting_attention(h, w_in, b_f, w_o):
    bsz, seq_len, d = h.shape
    proj = h @ w_in
    q = proj[..., :d].reshape(bsz, seq_len, N_HEADS_B, HEAD_DIM) * (HEAD_DIM ** -0.5)
    k = proj[..., d:2 * d].reshape(bsz, seq_len, N_HEADS_B, HEAD_DIM)
    v = proj[..., 2 * d:3 * d].reshape(bsz, seq_len, N_HEADS_B, HEAD_DIM)
    log_f = jax.nn.log_sigmoid((proj[..., 3 * d:] + b_f).astype(jnp.float32))
    cum = jnp.transpose(jnp.cumsum(log_f, axis=1), (0, 2, 1))
    k_pos = jnp.arange(seq_len, dtype=jnp.int32)

    def block_fn(start, size):
        q_blk = lax.dynamic_slice_in_dim(q, start, size, axis=1)
        cum_q = lax.dynamic_slice_in_dim(cum, start, size, axis=2)
        q_pos = start + jnp.arange(size, dtype=jnp.int32)
        s = jnp.einsum('bqhd,bkhd->bhqk', q_blk, k, preferred_element_type=jnp.float32)
        s = s + cum_q[:, :, :, None] - cum[:, :, None, :]
        dist = q_pos[:, None] - k_pos[None, :]
        s = jnp.where((dist >= 0)[None, None], s, NEG_INF)
        p = jax.nn.softmax(s, axis=-1)
        return jnp.einsum('bhqk,bkhd->bqhd', p.astype(v.dtype), v)

    o = sweep_causal_blocks(block_fn, seq_len)
    return o.reshape(bsz, seq_len, d) @ w_o


def hierarchical_moe(h, w_rg, b_rg, w_re, b_re, w_gate, w_up, w_down):
    bsz, seq_len, d = h.shape
    t = h.reshape(-1, d)
    g_logits = (t @ w_rg + b_rg).astype(jnp.float32)
    g_prob = jax.nn.softmax(g_logits, axis=-1)
    g_sel = jnp.argmax(g_logits, axis=-1)
    p_group = jnp.take_along_axis(g_prob, g_sel[:, None], axis=1)[:, 0]
    e_logits = (jnp.einsum('nd,dge->nge', t, w_re) + b_re).astype(jnp.float32)
    e_in = jnp.take_along_axis(e_logits, g_sel[:, None, None], axis=1)[:, 0]
    top_v, top_i = lax.top_k(e_in, TOP_K_IN_GROUP)
    w_top = jax.nn.softmax(top_v, axis=-1)
    within = jnp.sum(w_top[..., None] * jax.nn.one_hot(top_i, EXPERTS_PER_GROUP), axis=1)
    gates = (p_group[:, None, None] * jax.nn.one_hot(g_sel, N_GROUPS)[:, :, None]
             * within[:, None, :]).reshape(-1, N_EXPERTS).astype(t.dtype)
    out = jnp.zeros_like(t)
    for e in range(N_EXPERTS):
        u = jax.nn.silu(t @ w_gate[e]) * (t @ w_up[e])
        out = out + gates[:, e:e + 1] * (u @ w_down[e])
    return out.reshape(bsz, seq_len, d)


def setup_inputs(seed: int = 0) -> dict:
    key = jax.random.key(seed)
    ks = jax.random.split(key, 24)
    d = D_MODEL
    nrm = jax.random.normal
    f32 = jnp.float32
    v_scale_a = jnp.concatenate([jnp.ones((2 * d,), f32), jnp.full((d,), DN_BETA, f32)])
    v_scale_b = jnp.concatenate([jnp.ones((2 * d,), f32), jnp.full((d,), DN_BETA, f32),
                                 jnp.ones((N_HEADS_B,), f32)])
    return {
        'x': nrm(ks[0], (BATCH, SEQ, d), f32),
        'meta_tokens': nrm(ks[1], (N_META, d), f32),
        'rel_bias': 0.5 * nrm(ks[2], (N_BUCKETS, N_MAPS_A), f32),
        'diff_w_qkv': nrm(ks[3], (N_LAYERS_A, d, 3 * d), f32) * (d ** -0.5) * v_scale_a,
        'diff_lambda': 0.1 * nrm(ks[4], (N_LAYERS_A, 4, HEAD_DIM), f32),
        'diff_subln': 1.0 + 0.02 * nrm(ks[5], (N_LAYERS_A, 2 * HEAD_DIM), f32),
        'diff_w_o': nrm(ks[6], (N_LAYERS_A, d, d), f32) * (d ** -0.5) * DN_BETA,
        'fox_w_in': nrm(ks[7], (N_LAYERS_B, d, 3 * d + N_HEADS_B), f32) * (d ** -0.5) * v_scale_b,
        'fox_b_f': jax.random.uniform(ks[8], (N_LAYERS_B, N_HEADS_B), f32, minval=1.0, maxval=4.0),
        'fox_w_o': nrm(ks[9], (N_LAYERS_B, d, d), f32) * (d ** -0.5) * DN_BETA,
        'ln_mix_g': 1.0 + 0.02 * nrm(ks[10], (DEPTH, d), f32),
        'ln_mix_b': 0.02 * nrm(ks[11], (DEPTH, d), f32),
        'ln_ffn_g': 1.0 + 0.02 * nrm(ks[12], (DEPTH, d), f32),
        'ln_ffn_b': 0.02 * nrm(ks[13], (DEPTH, d), f32),
        'router_group_w': nrm(ks[14], (DEPTH, d, N_GROUPS), f32) * (d ** -0.5),
        'router_group_b': 0.01 * nrm(ks[15], (DEPTH, N_GROUPS), f32),
        'router_expert_w': nrm(ks[16], (DEPTH, d, N_GROUPS, EXPERTS_PER_GROUP), f32) * (d ** -0.5),
        'router_expert_b': 0.01 * nrm(ks[17], (DEPTH, N_GROUPS, EXPERTS_PER_GROUP), f32),
        'expert_w_gate': nrm(ks[18], (DEPTH, N_EXPERTS, d, D_EXPERT), f32) * (d ** -0.5),
        'expert_w_up': nrm(ks[19], (DEPTH, N_EXPERTS, d, D_EXPERT), f32) * (d ** -0.5),
        'expert_w_down': nrm(ks[20], (DEPTH, N_EXPERTS, D_EXPERT, d), f32) * (D_EXPERT ** -0.5) * DN_BETA,
    }


def reference(x, meta_tokens, rel_bias, diff_w_qkv, diff_lambda, diff_subln, diff_w_o,
              fox_w_in, fox_b_f, fox_w_o, ln_mix_g, ln_mix_b, ln_ffn_g, ln_ffn_b,
              router_group_w, router_group_b, router_expert_w, router_expert_b,
              expert_w_gate, expert_w_up, expert_w_down):
    bsz = x.shape[0]
    meta = jnp.broadcast_to(meta_tokens[None].astype(x.dtype), (bsz, N_META, x.shape[-1]))
    h = jnp.concatenate([meta, x], axis=1)
    for i in range(DEPTH):
        j = i // N_MIXERS
        if i % N_MIXERS == 0:
            mix = differential_attention(h, diff_w_qkv[j], diff_lambda[j], diff_subln[j],
                                         diff_w_o[j], rel_bias, diff_lambda_init(i))
        else:
            mix = forgetting_attention(h, fox_w_in[j], fox_b_f[j], fox_w_o[j])
        h = layer_norm(DN_ALPHA * h + mix, ln_mix_g[i], ln_mix_b[i])
        ffn = hierarchical_moe(h, router_group_w[i], router_group_b[i], router_expert_w[i],
                               router_expert_b[i], expert_w_gate[i], expert_w_up[i], expert_w_down[i])
        h = layer_norm(DN_ALPHA * h + ffn, ln_ffn_g[i], ln_ffn_b[i])
    return h[:, N_META:]
```

```python
import math
from contextlib import ExitStack

import numpy as np
import concourse.bass as bass
import concourse.mybir as mybir
from concourse.bass_utils import run_bass_kernel_spmd

F32 = mybir.dt.float32
BF16 = mybir.dt.bfloat16
AF = mybir.ActivationFunctionType
ALU = mybir.AluOpType
AX = mybir.AxisListType

D = 1024
DC = 8
N_META = 16
NEXP = 32
DEXP = 512
DEPTH = 2
ALPHA = (2 * DEPTH) ** 0.25
LN_EPS = 1e-5
NEGM = -30000.0
NDS = 56
NBULK = 8
NSP = 20
import os
DEBUG = os.environ.get("KDEBUG", "0") == "1"
DBG = {}


def diff_lambda_init(layer_idx):
    return 0.8 - 0.6 * math.exp(-0.3 * layer_idx)


class Buf:
    __slots__ = ("w", "r", "excl")

    def __init__(self, excl=False):
        self.w = None
        self.r = {}
        self.excl = excl


class Prog:
    def __init__(self, nc, es):
        self.nc = nc
        self.E = {"pe": nc.tensor, "act": nc.scalar, "dve": nc.vector, "pool": nc.gpsimd, "sp": nc.sync}
        self.semobj = {}
        for k in self.E:
            self.semobj[k] = es.enter_context(nc.semaphore("s_" + k))
        self.cnt = {k: 0 for k in self.E}
        self.pending = {k: False for k in self.E}
        self.waited = {k: {} for k in self.E}
        self.dval = [0] * NDS
        for i in range(NDS):
            self.semobj[("d", i)] = es.enter_context(nc.semaphore("d%d" % i))
        self.dnext = 0
        self.bnext = 0
        self.pnext = 0
        self.nins = 0

    def _wait(self, eng, tok):
        key, val = tok
        if self.waited[eng].get(key, 0) >= val:
            return
        self.E[eng].wait_ge(self.semobj[key], val)
        self.waited[eng][key] = val

    def _deps(self, eng, reads, writes):
        toks = []
        for b in reads:
            if b.w is not None:
                toks.append(b.w)
            if b.excl:
                toks.extend(b.r.items())
        for b in writes:
            if b.w is not None:
                toks.append(b.w)
            toks.extend(b.r.items())
        for t in toks:
            if eng == "pe" and t[0] == "pe":
                continue
            self._wait(eng, t)

    def _mark(self, tok, reads, writes):
        for b in reads:
            if b.excl:
                b.w = tok
                b.r = {}
            else:
                b.r[tok[0]] = tok[1]
        for b in writes:
            b.w = tok
            b.r = {}

    def op(self, eng, fn, reads=(), writes=(), inc=True):
        self._deps(eng, reads, writes)
        ins = fn(self.E[eng])
        self.nins += 1
        if inc:
            self.cnt[eng] += 1
            ins.then_inc(self.semobj[eng], 1)
            tok = (eng, self.cnt[eng])
            self.pending[eng] = False
        else:
            tok = (eng, self.cnt[eng] + 1)
            self.pending[eng] = True
        self._mark(tok, reads, writes)
        return tok

    def dma(self, eng, out, in_, reads=(), writes=(), bulk=False):
        if bulk:
            i = NDS - NBULK + self.bnext
            self.bnext = (self.bnext + 1) % NBULK
        elif eng == "pool":
            i = NSP + self.pnext
            self.pnext = (self.pnext + 1) % (NDS - NBULK - NSP)
        else:
            i = self.dnext
            self.dnext = (i + 1) % NSP
        key = ("d", i)
        if self.dval[i] > 0:
            self._wait(eng, (key, self.dval[i]))
        self._deps(eng, reads, writes)
        ins = self.E[eng].dma_start(out=out, in_=in_)
        self.nins += 1
        self.dval[i] += 16
        ins.then_inc(self.semobj[key], 16)
        tok = (key, self.dval[i])
        self._mark(tok, reads, writes)
        return tok

    def idma(self, out, in_, out_off=None, in_off=None, reads=(), writes=()):
        eng = "pool"
        i = NSP + self.pnext
        self.pnext = (self.pnext + 1) % (NDS - NBULK - NSP)
        key = ("d", i)
        if self.dval[i] > 0:
            self._wait(eng, (key, self.dval[i]))
        self._deps(eng, reads, writes)
        oo = bass.IndirectOffsetOnAxis(ap=out_off, axis=0) if out_off is not None else None
        io_ = bass.IndirectOffsetOnAxis(ap=in_off, axis=0) if in_off is not None else None
        ins = self.E[eng].indirect_dma_start(out=out, out_offset=oo, in_=in_, in_offset=io_)
        self.nins += 1
        self.dval[i] += 16
        ins.then_inc(self.semobj[key], 16)
        tok = (key, self.dval[i])
        self._mark(tok, reads, writes)
        return tok

    def barrier(self):
        for k in self.E:
            assert not self.pending[k], k
        toks = [(k, self.cnt[k]) for k in self.E if self.cnt[k] > 0]
        toks += [(("d", i), v) for i, v in enumerate(self.dval) if v > 0]
        for eng in self.E:
            for t in toks:
                self._wait(eng, t)


def _divisor_le(n, m):
    for d in range(min(n, m), 0, -1):
        if n % d == 0:
            return d
    return 1


class LayerEmitter:
    def __init__(self, nc, P, es, LP):
        self.nc = nc
        self.P = P
        self.LP = LP
        self.NB = LP // 128
        self.NJ = self.NB // 2
        self.TO = self.NJ * 128
        self.ps = []
        self.psb = []
        for i in range(8):
            t = es.enter_context(nc.psum_tensor("ps%d" % i, [128, 512], F32))
            self.ps.append(t)
            self.psb.append(Buf(excl=True))
        self.psrr = 0
        self.evrr = 0

    def sb(self, es, name, shape, dt):
        self.sbn = getattr(self, "sbn", 0) + 1
        return es.enter_context(self.nc.sbuf_tensor("sb%d_%s" % (self.sbn, name), shape, dt))

    def next_bank(self):
        i = self.psrr
        self.psrr = (i + 1) % 8
        return i

    def ev_eng(self):
        self.evrr ^= 1
        return "act" if self.evrr else "dve"

    def copy(self, eng, out, in_, reads, writes, scale=None):
        P = self.P
        if eng == "act":
            if scale is None:
                return P.op("act", lambda e: e.activation(out=out, in_=in_, func=AF.Copy), reads, writes)
            return P.op("act", lambda e: e.activation(out=out, in_=in_, func=AF.Copy, scale=scale), reads, writes)
        if scale is None:
            return P.op(eng, lambda e: e.tensor_copy(out=out, in_=in_), reads, writes)
        return P.op(eng, lambda e: e.tensor_scalar(out, in_, scale, None, ALU.mult), reads, writes)

    def emit_layer(self, ltype, li, io, es_layer):
        nc, P = self.nc, self.P
        LP, NB, NJ, TO = self.LP, self.NB, self.NJ, self.TO
        ps, psb = self.ps, self.psb
        fox = ltype == "fox"
        NCOL = 3088 if fox else 3072
        lam_init = diff_lambda_init(li)
        do_kv = io.get("do_kv", True)
        q_mode = io.get("q_mode", "static")

        ident = self.sb(es_layer, "ident%d" % li, [128, 128], F32)
        self.ident = ident
        ones_bf = self.sb(es_layer, "ones_bf%d" % li, [128, 128], BF16)
        ones_f = self.sb(es_layer, "ones_f%d" % li, [128, 128], F32)
        cb = Buf()
        P.dma("sp", ident[:], io["ident"], writes=[cb])
        P.op("dve", lambda e: e.memset(ones_bf[:], 1.0), writes=[cb])
        P.op("dve", lambda e: e.memset(ones_f[:], 1.0), writes=[cb])
        SEL = self.sb(es_layer, "SEL%d" % li, [128, NJ, NEXP], F32)
        SEL1 = self.sb(es_layer, "SEL1_%d" % li, [128, NJ, NEXP], F32)
        W12 = self.sb(es_layer, "W12_%d" % li, [128, NJ, 2], F32)
        GATES = (SEL, SEL1, W12)
        self.ones_bf = ones_bf
        NCK = None
        if fox:
            NCK = self.sb(es_layer, "nck%d" % li, [128, NB * 16], F32)

        xT_seq = io["xT_seq"].rearrange("(dc p) t -> p dc t", p=128)
        xT_own = io["xT_own"].rearrange("(dc p) t -> p dc t", p=128) if q_mode == "static" else None
        w_in = io["w_in"].rearrange("(dc p) n -> p dc n", p=128)
        KT_d, QT_d, V_d = io["KT_d"], io["QT_d"], io["V_d"]
        KT_dv = KT_d.rearrange("(c p) t -> p c t", p=128)
        QT_dv = QT_d.rearrange("(c p) t -> p c t", p=128)
        V_dv = V_d.rearrange("(b p) n -> p b n", p=128)

        if fox:
            with ExitStack() as es:
                pairs = _divisor_le(NJ, 11)
                CH = pairs * 256
                nchunks = NJ // pairs
                Wf = self.sb(es, "wf", [128, DC, 16], BF16)
                nbf = self.sb(es, "nbf", [16, 1], F32)
                npar = self.sb(es, "npar", [16, 2], F32)
                ones_s = self.sb(es, "ones_s", [16, CH], F32)
                carry = self.sb(es, "carry", [16, 1], F32)
                hTs = [self.sb(es, "hTf%d" % i, [128, DC, 512], BF16) for i in range(2)]
                hTb = [Buf(), Buf()]
                Et = self.sb(es, "Et", [16, 512], F32)
                LF = self.sb(es, "LF", [16, CH], F32)
                CN = self.sb(es, "CN", [16, CH], F32)
                T1 = self.sb(es, "T1", [16, pairs * 128], F32)
                CQ = self.sb(es, "CQ", [16, pairs * 128], F32)
                R1 = self.sb(es, "R1", [16, pairs * 128], F32)
                R2 = self.sb(es, "R2", [16, pairs * 128], F32)
                C3 = self.sb(es, "C3", [16, 3, pairs * 128], BF16)
                b_wf, b_nbf, b_par, b_ones, b_carry = Buf(), Buf(), Buf(), Buf(), Buf()
                b_Et, b_LF, b_CN, b_T1, b_CQ, b_R1, b_R2, b_C3 = (Buf() for _ in range(8))
                b_nck = Buf()
                P.dma("pool", Wf[:], w_in[:, :, 3072:3088], writes=[b_wf])
                P.dma("sp", nbf[:], io["b_f"], writes=[b_nbf])
                P.op("dve", lambda e: e.tensor_scalar(nbf[:], nbf[:], -1.0, None, ALU.mult), reads=[b_nbf], writes=[b_nbf])
                P.dma("sp", npar[:], io["npar"], writes=[b_par])
                P.op("dve", lambda e: e.memset(ones_s[:], 1.0), writes=[b_ones])
                P.op("dve", lambda e: e.memset(carry[:], 0.0), writes=[b_carry])
                tcount = 0
                for ci in range(nchunks):
                    t0 = ci * CH
                    off = 0
                    while off < CH:
                        n = min(512, CH - off)
                        s = tcount % 2
                        tcount += 1
                        P.dma("pool", hTs[s][:, :, 0:n], xT_seq[:, :, t0 + off:t0 + off + n], writes=[hTb[s]])
                        bk = self.next_bank()
                        for dc in range(DC):
                            P.op("pe", lambda e, dc=dc, s=s, n=n, bk=bk: e.matmul(
                                ps[bk][0:16, 0:n], Wf[:, dc, :], hTs[s][:, dc, 0:n], start=(dc == 0), stop=(dc == DC - 1)),
                                reads=[b_wf, hTb[s]], writes=[psb[bk]], inc=(dc == DC - 1))
                        P.op("act", lambda e, n=n, bk=bk: e.activation(out=Et[:, 0:n], in_=ps[bk][0:16, 0:n], func=AF.Exp,
                                                                 bias=nbf[:, 0:1], scale=-1.0),
                             reads=[psb[bk], b_nbf], writes=[b_Et])
                        P.op("act", lambda e, n=n, off=off: e.activation(out=LF[:, off:off + n], in_=Et[:, 0:n], func=AF.Ln,
                                                                       bias=1.0, scale=1.0),
                             reads=[b_Et], writes=[b_LF])
                        off += n
                    P.op("dve", lambda e: e.tensor_tensor_scan(CN[:, :], ones_s[:, :], LF[:, :], carry[:, 0:1], ALU.mult, ALU.add),
                         reads=[b_ones, b_LF, b_carry], writes=[b_CN])
                    P.op("dve", lambda e: e.tensor_copy(out=carry[:, 0:1], in_=CN[:, CH - 1:CH]), reads=[b_CN], writes=[b_carry])
                    nblk = CH // 128
                    bk = self.next_bank()
                    for b in range(nblk):
                        P.op("pe", lambda e, b=b, bk=bk: e.transpose(ps[bk][:, b * 16:(b + 1) * 16], CN[:, b * 128:(b + 1) * 128],
                                                                  ident[0:16, 0:16]),
                             reads=[b_CN, cb], writes=[psb[bk]], inc=(b == nblk - 1))
                    g0 = t0 // 128
                    P.op("act", lambda e, bk=bk, g0=g0, nblk=nblk: e.activation(out=NCK[:, g0 * 16:(g0 + nblk) * 16],
                                                                               in_=ps[bk][:, 0:nblk * 16], func=AF.Copy),
                         reads=[psb[bk]], writes=[b_nck])
                    CNv = CN[:, :].rearrange("p (a two q) -> p a two q", two=2, q=128)
                    T1v = T1[:, :].rearrange("p (a q) -> p a q", q=128)
                    CQv = CQ[:, :].rearrange("p (a q) -> p a q", q=128)
                    P.op("dve", lambda e: e.tensor_scalar(T1v, CNv[:, :, 1, :], npar[:, 1:2], None, ALU.mult),
                         reads=[b_CN, b_par], writes=[b_T1])
                    P.op("dve", lambda e: e.scalar_tensor_tensor(CQv, CNv[:, :, 0, :], npar[:, 0:1], T1v, ALU.mult, ALU.add),
                         reads=[b_CN, b_par, b_T1], writes=[b_CQ])
                    P.op("dve", lambda e: e.tensor_copy(out=C3[:, 0, :], in_=CQ[:, :]), reads=[b_CQ], writes=[b_C3])
                    P.op("dve", lambda e: e.tensor_tensor(R1[:, :], CQ[:, :], C3[:, 0, :], ALU.subtract), reads=[b_CQ, b_C3], writes=[b_R1])
                    P.op("dve", lambda e: e.tensor_copy(out=C3[:, 1, :], in_=R1[:, :]), reads=[b_R1], writes=[b_C3])
                    P.op("dve", lambda e: e.tensor_tensor(R2[:, :], R1[:, :], C3[:, 1, :], ALU.subtract), reads=[b_R1, b_C3], writes=[b_R2])
                    P.op("dve", lambda e: e.tensor_copy(out=C3[:, 2, :], in_=R2[:, :]), reads=[b_R2], writes=[b_C3])
                    o0 = ci * pairs * 128
                    P.dma("sp", io["CQ3_d"][:, :, o0:o0 + pairs * 128], C3[:, :, :], reads=[b_C3])
                P.barrier()

        with ExitStack() as es:
            Wb = self.sb(es, "Wb", [128, DC, 3072], BF16)
            b_W = Buf()
            for pc in range(6 if do_kv else 2):
                P.dma("pool", Wb[:, :, pc * 512:(pc + 1) * 512], w_in[:, :, pc * 512:(pc + 1) * 512], writes=[b_W])
            if q_mode == "dyn":
                stq = [self.sb(es, "stq%d" % i, [128, 8, 256], BF16) for i in range(2)]
                stqb = [Buf(), Buf()]
                tmpq = self.sb(es, "tmpq", [128, 2, 128], F32)
                b_tmpq = Buf()
                pars = self.sb(es, "pars", [128, 2], F32)
                b_pars = Buf()
                P.dma("sp", pars[:], io["pars"], writes=[b_pars])
            hTs = [self.sb(es, "hTa%d" % i, [128, DC, 512], BF16) for i in range(2)]
            hTb = [Buf(), Buf()]
            stk = [self.sb(es, "stk%d" % i, [128, 8, 512], BF16) for i in range(2)]
            stkb = [Buf(), Buf()]
            stv = [self.sb(es, "stv%d" % i, [128, 4, 1024], BF16) for i in range(2)]
            stvb = [Buf(), Buf()]
            tcount = 0
            for t0 in (range(0, LP, 512) if do_kv else []):
                n = min(512, LP - t0)
                s = tcount % 2
                tcount += 1
                P.dma("pool", hTs[s][:, :, 0:n], xT_seq[:, :, t0:t0 + n], writes=[hTb[s]])
                for c in range(8):
                    bk = self.next_bank()
                    for dc in range(DC):
                        P.op("pe", lambda e, c=c, dc=dc, s=s, n=n, bk=bk: e.matmul(
                            ps[bk][:, 0:n], Wb[:, dc, 1024 + c * 128:1024 + (c + 1) * 128], hTs[s][:, dc, 0:n],
                            start=(dc == 0), stop=(dc == DC - 1)),
                            reads=[b_W, hTb[s]], writes=[psb[bk]], inc=(dc == DC - 1))
                    self.copy(self.ev_eng(), stk[s][:, c, 0:n], ps[bk][:, 0:n], [psb[bk]], [stkb[s]])
                P.dma("sp", KT_dv[:, :, t0:t0 + n], stk[s][:, :, 0:n], reads=[stkb[s]])
                nb = n // 128
                for b in range(nb):
                    for half in range(2):
                        bk = self.next_bank()
                        for dc in range(DC):
                            P.op("pe", lambda e, b=b, half=half, dc=dc, s=s, bk=bk: e.matmul(
                                ps[bk][:, :], hTs[s][:, dc, b * 128:(b + 1) * 128],
                                Wb[:, dc, 2048 + half * 512:2048 + (half + 1) * 512],
                                start=(dc == 0), stop=(dc == DC - 1)),
                                reads=[b_W, hTb[s]], writes=[psb[bk]], inc=(dc == DC - 1))
                        self.copy(self.ev_eng(), stv[s][:, b, half * 512:(half + 1) * 512], ps[bk][:, :], [psb[bk]], [stvb[s]])
                blk0 = t0 // 128
                P.dma("sp", V_dv[:, blk0:blk0 + nb, :], stv[s][:, 0:nb, :], reads=[stvb[s]])
                if q_mode == "dyn":
                    npair = n // 256
                    for c in range(8):
                        bk = self.next_bank()
                        for dc in range(DC):
                            P.op("pe", lambda e, c=c, dc=dc, s=s, n=n, bk=bk: e.matmul(
                                ps[bk][:, 0:n], Wb[:, dc, c * 128:(c + 1) * 128], hTs[s][:, dc, 0:n],
                                start=(dc == 0), stop=(dc == DC - 1)),
                                reads=[b_W, hTb[s]], writes=[psb[bk]], inc=(dc == DC - 1))
                        psv = ps[bk][:, 0:n].rearrange("p (a two q) -> p a two q", two=2, q=128)
                        P.op("dve", lambda e, psv=psv, npair=npair: e.tensor_scalar(
                            tmpq[:, 0:npair, :], psv[:, :, 1, :], pars[:, 1:2], None, ALU.mult),
                            reads=[psb[bk], b_pars], writes=[b_tmpq])
                        P.op("dve", lambda e, psv=psv, npair=npair, c=c, s=s: e.scalar_tensor_tensor(
                            stq[s][:, c, 0:npair * 128].rearrange("p (a q) -> p a q", q=128), psv[:, :, 0, :], pars[:, 0:1],
                            tmpq[:, 0:npair, :], ALU.mult, ALU.add),
                            reads=[psb[bk], b_pars, b_tmpq], writes=[stqb[s]])
                    P.dma("sp", QT_dv[:, :, t0 // 2:t0 // 2 + npair * 128], stq[s][:, :, 0:npair * 128], reads=[stqb[s]])
            for t0 in (range(0, TO, 512) if q_mode == "static" else []):
                n = min(512, TO - t0)
                s = tcount % 2
                tcount += 1
                P.dma("pool", hTs[s][:, :, 0:n], xT_own[:, :, t0:t0 + n], writes=[hTb[s]])
                for c in range(8):
                    bk = self.next_bank()
                    for dc in range(DC):
                        P.op("pe", lambda e, c=c, dc=dc, s=s, n=n, bk=bk: e.matmul(
                            ps[bk][:, 0:n], Wb[:, dc, c * 128:(c + 1) * 128], hTs[s][:, dc, 0:n],
                            start=(dc == 0), stop=(dc == DC - 1)),
                            reads=[b_W, hTb[s]], writes=[psb[bk]], inc=(dc == DC - 1))
                    self.copy(self.ev_eng(), stk[s][:, c, 0:n], ps[bk][:, 0:n], [psb[bk]], [stkb[s]], scale=0.125)
                P.dma("sp", QT_dv[:, :, t0:t0 + n], stk[s][:, :, 0:n], reads=[stkb[s]])
            P.barrier()

        with ExitStack() as es_x:
            XT = self.sb(es_x, "XT", [128, DC, TO], BF16)
            self._attention(ltype, li, io, XT, ident, ones_bf, ones_f, NCK, lam_init)
            P.barrier()
            self._post_attention(li, io, XT, ident, GATES)
            P.barrier()
        self._moe_routed(li, io, GATES)
        P.barrier()

    def _attention(self, ltype, li, io, XT, ident, ones_bf, ones_f, NCK, lam_init):
        nc, P = self.nc, self.P
        LP, NB, NJ, TO = self.LP, self.NB, self.NJ, self.TO
        ps, psb = self.ps, self.psb
        fox = ltype == "fox"
        Kd = 67 if fox else 64
        KT_d, QT_d, V_d = io["KT_d"], io["QT_d"], io["V_d"]
        with ExitStack() as es:
            KT = [self.sb(es, "KT%d" % i, [128, LP], BF16) for i in range(2)]
            QT = [self.sb(es, "QT%d" % i, [128, TO], BF16) for i in range(2)]
            Vl = [self.sb(es, "Vl%d" % i, [128, NB, 128], BF16) for i in range(2)]
            ktb, qtb, vlb = [Buf(), Buf()], [Buf(), Buf()], [Buf(), Buf()]
            NPT = 4
            PT = [self.sb(es, "PT%d" % i, [128, 512], BF16) for i in range(NPT)]
            ptb = [Buf() for _ in range(NPT)]
            rS = self.sb(es, "rS", [128, 512], F32)
            tO = self.sb(es, "tO", [128, 512], F32)
            b_rS, b_tO = Buf(), Buf()
            mask = self.sb(es, "mask", [128, 3, 128], F32)
            b_mask = Buf()
            P.dma("sp", mask[:], io["nb_mask"].rearrange("s k q -> k s q"), writes=[b_mask])
            if fox:
                for i in range(2):
                    P.op("pool", lambda e, i=i: e.memset(KT[i][64:67, :], 1.0), writes=[ktb[i]])
                    oth = 1 - i
                    P.op("pool", lambda e, i=i, oth=oth: e.memset(Vl[i][:, :, oth * 64:(oth + 1) * 64], 0.0), writes=[vlb[i]])
                NBt = [mask, mask]
                nbb = [b_mask, b_mask]
            else:
                NBt = [self.sb(es, "NBt%d" % i, [128, 3, 128], F32) for i in range(2)]
                nbb = [Buf(), Buf()]
                Kd = 128
                for i in range(2):
                    P.op("pool", lambda e, i=i: e.memset(KT[i][64:128, :], 0.0), writes=[ktb[i]])
                    P.op("pool", lambda e, i=i: e.memset(QT[i][64:128, :], 0.0), writes=[qtb[i]])
                B31 = self.sb(es, "B31", [128, 16], F32)
                lam = self.sb(es, "lam", [128, 256], F32)
                lpr = self.sb(es, "lpr", [128, 128], F32)
                lsum = self.sb(es, "lsum", [128, 2], F32)
                nlam = self.sb(es, "nlam", [128, 1], F32)
                gsub = self.sb(es, "gsub", [128, 1], F32)
                dA = self.sb(es, "dA", [128, TO], F32)
                dd = self.sb(es, "dd", [128, 512], F32)
                sq = self.sb(es, "sq", [128, 512], BF16)
                rstd = self.sb(es, "rstd", [128, 512], F32)
                b_B31, b_lam, b_nlam, b_gsub, b_dd, b_sq, b_rstd = (Buf() for _ in range(7))
                P.dma("sp", B31[:], io["b31"].partition_broadcast(128), writes=[b_B31])
                P.dma("sp", lam[:], io["lam"].partition_broadcast(128), writes=[b_lam])
                P.dma("sp", gsub[:], io["subln"], writes=[b_gsub])
                P.op("dve", lambda e: e.tensor_scalar(gsub[:], gsub[:], 1.0 - lam_init, None, ALU.mult), reads=[b_gsub], writes=[b_gsub])
                lamv = lam[:, :].rearrange("p (a two d) -> p a two d", two=2, d=64)
                lprv = lpr[:, :].rearrange("p (a d) -> p a d", d=64)
                P.op("dve", lambda e: e.tensor_tensor(lprv, lamv[:, :, 0, :], lamv[:, :, 1, :], ALU.mult), reads=[b_lam], writes=[b_lam])
                P.op("dve", lambda e: e.reduce_sum(lsum[:, 0:1], lpr[:, 0:64], AX.X), reads=[b_lam], writes=[b_nlam])
                P.op("dve", lambda e: e.reduce_sum(lsum[:, 1:2], lpr[:, 64:128], AX.X), reads=[b_lam], writes=[b_nlam])
                P.op("act", lambda e: e.activation(out=lsum[:, :], in_=lsum[:, :], func=AF.Exp), reads=[b_nlam], writes=[b_nlam])
                P.op("dve", lambda e: e.scalar_tensor_tensor(nlam[:, 0:1], lsum[:, 1:2], -lam_init, lsum[:, 0:1], ALU.add, ALU.subtract),
                     reads=[b_nlam], writes=[b_nlam])
                dAb = {}

            def load_map(m):
                s = m % 2
                P.dma("sp", KT[s][0:64, :], KT_d[m * 64:(m + 1) * 64, :], writes=[ktb[s]])
                P.dma("sp", QT[s][0:64, :], QT_d[m * 64:(m + 1) * 64, :], writes=[qtb[s]])
                if fox:
                    P.dma("sp", QT[s][64:67, :], io["CQ3_d"][m, :, :], writes=[qtb[s]])
                    P.dma("sp", Vl[s][:, :, s * 64:(s + 1) * 64],
                          V_d.rearrange("(b p) n -> p b n", p=128)[:, :, m * 64:(m + 1) * 64], writes=[vlb[s]])
                else:
                    P.dma("sp", Vl[s][:, :, :], V_d.rearrange("(b p) n -> p b n", p=128)[:, :, (m // 2) * 128:(m // 2 + 1) * 128], writes=[vlb[s]])
                    P.dma("sp", NBt[s][:], io["nb_T"][:, m].rearrange("s k q -> k s q"), writes=[nbb[s]])
                    P.op("dve", lambda e, s=s, m=m: e.scalar_tensor_tensor(NBt[s][:], NBt[s][:], B31[:, m:m + 1], mask[:],
                                                                            ALU.subtract, ALU.add),
                         reads=[nbb[s], b_B31, b_mask], writes=[nbb[s]])

            for (dst, src) in getattr(self, "pending_conv", []):
                P.dma("pool", dst, src, bulk=True)
            self.pending_conv = []
            load_map(0)
            NG = (NJ + 3) // 4
            LA = 2
            SB = [0, 1, 2]
            OS = [(3, 4), (5, 6)]
            gcount = 0
            pend1, pend2 = [], []
            for m in range(16):
                s = m % 2
                if m + 1 < 16:
                    load_map(m + 1)
                c = m // 2
                half = m % 2
                for g in range(NG):
                    j0 = 4 * g
                    j1 = min(j0 + 4, NJ)
                    nq = (j1 - j0) * 128
                    nkb = 2 * j1
                    bo, bs_ = OS[gcount % 2]
                    gcount += 1
                    info = {}
                    tail0 = max(2 * j0 - 1, 0)
                    bulk = list(range(0, tail0))
                    tail = list(range(tail0, nkb))
                    if len(bulk) > 1 and not fox:
                        head = bulk[:max(1, len(bulk) - len(tail))]
                        rest = bulk[len(head):]
                        order = list(head)
                        for a in range(max(len(rest), len(tail))):
                            if a < len(rest):
                                order.append(rest[a])
                            if a < len(tail):
                                order.append(tail[a])
                    else:
                        order = bulk + tail
                    assert sorted(order) == list(range(nkb)) and order[0] == 0
                    for i in range(nkb + LA):
                        if i == 3 and pend1:
                            pend1.pop(0)()
                        if i == 7 and pend2:
                            pend2.pop(0)()
                        if i < nkb:
                            kb = order[i]
                            jmin = max(j0, kb // 2)
                            qoff = (jmin - j0) * 128
                            sbk = SB[i % 3]
                            pt = i % NPT
                            info[i] = (qoff, pt)
                            P.op("pe", lambda e, kb=kb, qoff=qoff, sbk=sbk, s=s: e.matmul(
                                ps[sbk][:, qoff:nq], KT[s][0:Kd, kb * 128:(kb + 1) * 128],
                                QT[s][0:Kd, j0 * 128 + qoff:j0 * 128 + nq], start=True, stop=True),
                                reads=[ktb[s], qtb[s]], writes=[psb[sbk]])
                            for j in range(jmin, j1):
                                slot = kb - (2 * j - 1)
                                if slot < 0 or slot > 2:
                                    continue
                                if fox and slot == 0:
                                    continue
                                cq = (j - j0) * 128
                                P.op("dve", lambda e, sbk=sbk, cq=cq, slot=slot, s=s: e.tensor_tensor(
                                    ps[sbk][:, cq:cq + 128], ps[sbk][:, cq:cq + 128], NBt[s][:, slot, :], ALU.add),
                                    reads=[psb[sbk], nbb[s]], writes=[psb[sbk]])
                            if fox:
                                P.op("act", lambda e, sbk=sbk, qoff=qoff, pt=pt, kb=kb, m=m: e.activation(
                                    out=PT[pt][:, qoff:nq], in_=ps[sbk][:, qoff:nq], func=AF.Exp,
                                    bias=NCK[:, kb * 16 + m:kb * 16 + m + 1], scale=1.0),
                                    reads=[psb[sbk]], writes=[ptb[pt]])
                            else:
                                P.op("act", lambda e, sbk=sbk, qoff=qoff, pt=pt: e.activation(
                                    out=PT[pt][:, qoff:nq], in_=ps[sbk][:, qoff:nq], func=AF.Exp),
                                    reads=[psb[sbk]], writes=[ptb[pt]])
                        if i >= LA:
                            ii = i - LA
                            kb = order[ii]
                            qoff, pt = info.pop(ii)
                            P.op("pe", lambda e, kb=kb, ii=ii, qoff=qoff, pt=pt, s=s, bo=bo: e.matmul(
                                ps[bo][:, qoff:nq], Vl[s][:, kb, :], PT[pt][:, qoff:nq], start=(ii == 0), stop=(ii == nkb - 1)),
                                reads=[vlb[s], ptb[pt]], writes=[psb[bo]], inc=False)
                            P.op("pe", lambda e, ii=ii, qoff=qoff, pt=pt, bs_=bs_: e.matmul(
                                ps[bs_][:, qoff:nq], ones_bf[:, :], PT[pt][:, qoff:nq], start=(ii == 0), stop=(ii == nkb - 1)),
                                reads=[ptb[pt]], writes=[psb[bs_]], inc=True)
                    q0 = j0 * 128

                    def evac1(g=g, c=c, half=half, q0=q0, nq=nq, bo=bo, bs_=bs_):
                        if fox:
                            r0, r1 = half * 64, (half + 1) * 64
                            P.op("dve", lambda e: e.reciprocal(rS[r0:r1, 0:nq], ps[bs_][r0:r1, 0:nq]),
                                 reads=[psb[bs_]], writes=[b_rS])
                            P.op("dve", lambda e: e.tensor_tensor(
                                XT[r0:r1, c, q0:q0 + nq], ps[bo][r0:r1, 0:nq], rS[r0:r1, 0:nq], ALU.mult),
                                reads=[psb[bo], b_rS], writes=[])
                            return
                        P.op("dve", lambda e: e.reciprocal(rS[:, 0:nq], ps[bs_][:, 0:nq]), reads=[psb[bs_]], writes=[b_rS])
                        if half == 0:
                            dAb[g] = Buf()
                            P.op("dve", lambda e: e.tensor_tensor(dA[:, q0:q0 + nq], ps[bo][:, 0:nq], rS[:, 0:nq], ALU.mult),
                                 reads=[psb[bo], b_rS], writes=[dAb[g]])
                        else:
                            P.op("dve", lambda e: e.tensor_tensor(tO[:, 0:nq], ps[bo][:, 0:nq], rS[:, 0:nq], ALU.mult),
                                 reads=[psb[bo], b_rS], writes=[b_tO])
                            P.op("dve", lambda e: e.scalar_tensor_tensor(dd[:, 0:nq], tO[:, 0:nq], nlam[:, 0:1], dA[:, q0:q0 + nq],
                                                                         ALU.mult, ALU.add),
                                 reads=[b_tO, b_nlam, dAb[g]], writes=[b_dd])
                            P.op("act", lambda e: e.activation(out=sq[:, 0:nq], in_=dd[:, 0:nq], func=AF.Square), reads=[b_dd], writes=[b_sq])

                    def evac2(g=g, c=c, half=half, q0=q0, nq=nq):
                        if fox or half == 0:
                            return
                        P.op("pe", lambda e: e.matmul(ps[7][:, 0:nq], ones_bf[:, :], sq[:, 0:nq], start=True, stop=True),
                             reads=[b_sq], writes=[psb[7]])
                        P.op("dve", lambda e: e.tensor_scalar(rstd[:, 0:nq], ps[7][:, 0:nq], 1.0 / 128.0, LN_EPS, ALU.mult, ALU.add),
                             reads=[psb[7]], writes=[b_rstd])
                        P.op("act", lambda e: e.activation(out=rstd[:, 0:nq], in_=rstd[:, 0:nq], func=AF.Ln), reads=[b_rstd], writes=[b_rstd])
                        P.op("act", lambda e: e.activation(out=rstd[:, 0:nq], in_=rstd[:, 0:nq], func=AF.Exp, scale=-0.5), reads=[b_rstd], writes=[b_rstd])
                        P.op("dve", lambda e: e.scalar_tensor_tensor(XT[:, c, q0:q0 + nq], dd[:, 0:nq], gsub[:, 0:1], rstd[:, 0:nq],
                                                                     ALU.mult, ALU.mult),
                             reads=[b_dd, b_gsub, b_rstd], writes=[])

                    pend1.append(evac1)
                    pend2.append(evac2)
            for f_ in pend1 + pend2:
                f_()

    def _layernorm(self, es, tag):
        P = self.P
        st = self.sb(es, "st" + tag, [128, 2, 6], F32)
        mv = self.sb(es, "mv" + tag, [128, 2], F32)
        rs = self.sb(es, "rs" + tag, [128, 1], F32)
        b_st, b_mv, b_rs = Buf(), Buf(), Buf()

        def ln(Z, b_Z, out, b_out, Gt, Bt, b_gb):
            for h in range(2):
                P.op("dve", lambda e, h=h: e.bn_stats(st[:, h, :], Z[:, h * 512:(h + 1) * 512]), reads=[b_Z], writes=[b_st])
            P.op("dve", lambda e: e.bn_aggr(mv[:, :], st[:, :, :].rearrange("p a b -> p (a b)")), reads=[b_st], writes=[b_mv])
            P.op("dve", lambda e: e.tensor_scalar(rs[:, :], mv[:, 1:2], LN_EPS, None, ALU.add), reads=[b_mv], writes=[b_rs])
            P.op("act", lambda e: e.activation(out=rs[:, :], in_=rs[:, :], func=AF.Ln), reads=[b_rs], writes=[b_rs])
            P.op("act", lambda e: e.activation(out=rs[:, :], in_=rs[:, :], func=AF.Exp, scale=-0.5), reads=[b_rs], writes=[b_rs])
            P.op("dve", lambda e: e.tensor_scalar(Z[:, :], Z[:, :], mv[:, 0:1], rs[:, 0:1], ALU.subtract, ALU.mult),
                 reads=[b_Z, b_mv, b_rs], writes=[b_Z])
            P.op("dve", lambda e: e.tensor_tensor(Z[:, :], Z[:, :], Gt[:, :], ALU.mult), reads=[b_Z, b_gb], writes=[b_Z])
            P.op("dve", lambda e: e.tensor_tensor(out, Z[:, :], Bt[:, :], ALU.add), reads=[b_Z, b_gb], writes=[b_out])
        return ln

    def _post_attention(self, li, io, XT, ident, GATES):
        nc, P = self.nc, self.P
        NJ, TO = self.NJ, self.TO
        ps, psb = self.ps, self.psb
        with ExitStack() as es:
            Wo = self.sb(es, "Wo", [128, DC, 1024], BF16)
            b_Wo = Buf()
            wo_v = io["w_o"].rearrange("(dc p) n -> p dc n", p=128)
            for h in range(2):
                P.dma("pool", Wo[:, :, h * 512:(h + 1) * 512], wo_v[:, :, h * 512:(h + 1) * 512], writes=[b_Wo])
            Gt = self.sb(es, "Gt", [128, 1024], F32)
            Bt = self.sb(es, "Bt", [128, 1024], F32)
            b_gb = Buf()
            P.dma("sp", Gt[:], io["lnm_g"].partition_broadcast(128), writes=[b_gb])
            P.dma("sp", Bt[:], io["lnm_b"].partition_broadcast(128), writes=[b_gb])
            Wr = self.sb(es, "Wr", [128, DC, 36], F32)
            Br = self.sb(es, "Br", [128, 36], F32)
            b_Wr = Buf()
            P.dma("sp", Wr[:], io["w_r"].rearrange("(dc p) n -> p dc n", p=128), writes=[b_Wr])
            P.dma("sp", Br[:], io["b_r"].partition_broadcast(128), writes=[b_Wr])
            NR = 3
            Hres = [self.sb(es, "Hres%d" % i, [128, 1024], F32) for i in range(NR)]
            hrb = [Buf() for _ in range(NR)]
            Z = self.sb(es, "Z", [128, 1024], F32)
            b_Z = Buf()
            H1 = [self.sb(es, "H1_%d" % i, [128, 1024], F32) for i in range(2)]
            h1b = [Buf(), Buf()]
            HTf = self.sb(es, "HTf", [128, DC, 128], F32)
            b_HTf = Buf()
            Lg = self.sb(es, "Lg", [128, 36], F32)
            EM = self.sb(es, "EM", [128, 32], F32)
            sm = self.sb(es, "sm", [128, 16], F32)
            oh = self.sb(es, "oh", [128, 4], F32)
            pen = self.sb(es, "pen", [128, 4], F32)
            ge = self.sb(es, "ge", [128, 4], F32)
            M8 = self.sb(es, "M8", [128, 8], F32)
            e2 = self.sb(es, "e2", [128, 2], F32)
            sel = self.sb(es, "sel", [128, 32], F32)
            ew = self.sb(es, "ew", [128, 32], F32)
            b_r = Buf()
            ln = self._layernorm(es, "m")
            h1T_dv = io["H1T_d"].rearrange("(dc p) t -> p dc t", p=128)

            res_mode = io.get("res_mode", "static")
            if res_mode == "dyn":
                Hp = [self.sb(es, "Hp%d" % i, [128, 2, 1024], F32) for i in range(NR)]
                hpb = [Buf() for _ in range(NR)]
                par = self.sb(es, "par", [128, 2], F32)
                b_par = Buf()
                P.dma("sp", par[:], io["par"], writes=[b_par])

            def load_res(j, sl):
                if res_mode == "static":
                    P.dma("sp", Hres[sl][:], io["x_own"][j * 128:(j + 1) * 128, :], writes=[hrb[sl]])
                else:
                    P.dma("sp", Hp[sl][:], io["h2_d"][2 * j * 128:(2 * j + 2) * 128, :].rearrange("(two p) n -> p two n", p=128),
                          writes=[hpb[sl]])

            def select_res(sl):
                if res_mode != "static":
                    P.op("dve", lambda e, sl=sl: e.tensor_scalar(Hres[sl][:, :], Hp[sl][:, 1, :], par[:, 1:2], None, ALU.mult),
                         reads=[hpb[sl], b_par], writes=[hrb[sl]])
                    P.op("dve", lambda e, sl=sl: e.scalar_tensor_tensor(Hres[sl][:, :], Hp[sl][:, 0, :], par[:, 0:1], Hres[sl][:, :],
                                                                         ALU.mult, ALU.add),
                         reads=[hpb[sl], b_par, hrb[sl]], writes=[hrb[sl]])

            wo_banks = {}

            def emit_wo(j):
                t0 = j * 128
                bks = []
                for h in range(2):
                    bk = self.next_bank()
                    for dc in range(DC):
                        P.op("pe", lambda e, dc=dc, h=h, bk=bk: e.matmul(
                            ps[bk][:, :], XT[:, dc, t0:t0 + 128], Wo[:, dc, h * 512:(h + 1) * 512],
                            start=(dc == 0), stop=(dc == DC - 1)),
                            reads=[b_Wo], writes=[psb[bk]], inc=(dc == DC - 1))
                    bks.append(bk)
                wo_banks[j] = bks

            def emit_ln(j):
                s = j % 2
                sl = j % NR
                t0 = j * 128
                select_res(sl)
                for h, bk in enumerate(wo_banks.pop(j)):
                    P.op("dve", lambda e, h=h, bk=bk, sl=sl: e.scalar_tensor_tensor(
                        Z[:, h * 512:(h + 1) * 512], Hres[sl][:, h * 512:(h + 1) * 512], ALPHA, ps[bk][:, :], ALU.mult, ALU.add),
                        reads=[hrb[sl], psb[bk]], writes=[b_Z])
                ln(Z, b_Z, H1[s][:, :], h1b[s], Gt, Bt, b_gb)
                P.dma("pool", io["h1_d"][t0:t0 + 128, :], H1[s][:, :], reads=[h1b[s]])

            def emit_tr(j):
                s = j % 2
                for q in range(2):
                    bk = self.next_bank()
                    for k in range(4):
                        dc = q * 4 + k
                        P.op("pe", lambda e, dc=dc, k=k, bk=bk, s=s: e.transpose(
                            ps[bk][:, k * 128:(k + 1) * 128], H1[s][:, dc * 128:(dc + 1) * 128], ident[:, :]),
                            reads=[h1b[s]], writes=[psb[bk]], inc=(k == 3))
                    P.op("act", lambda e, q=q, bk=bk: e.activation(
                        out=HTf[:, q * 4:(q + 1) * 4, :], in_=ps[bk][:, :].rearrange("p (a b) -> p a b", b=128), func=AF.Copy),
                        reads=[psb[bk]], writes=[b_HTf])
                bk = self.next_bank()
                for dc in range(DC):
                    P.op("pe", lambda e, dc=dc, bk=bk: e.matmul(ps[bk][:, 0:36], HTf[:, dc, :], Wr[:, dc, :],
                                                                 start=(dc == 0), stop=(dc == DC - 1)),
                         reads=[b_HTf, b_Wr], writes=[psb[bk]], inc=(dc == DC - 1))
                return bk

            def emit_chain(j, bk):
                R = [b_r]
                P.op("dve", lambda e, bk=bk: e.tensor_tensor(Lg[:, :], ps[bk][:, 0:36], Br[:, :], ALU.add), reads=[psb[bk], b_Wr], writes=R)
                P.op("dve", lambda e: e.reduce_max(sm[:, 0:1], Lg[:, 0:4], AX.X), reads=R, writes=R)
                P.op("dve", lambda e: e.tensor_scalar(oh[:, :], Lg[:, 0:4], sm[:, 0:1], None, ALU.is_ge), reads=R, writes=R)
                P.op("dve", lambda e: e.tensor_scalar(sm[:, 1:2], sm[:, 0:1], -1.0, None, ALU.mult), reads=R, writes=R)
                P.op("act", lambda e: e.activation(out=ge[:, :], in_=Lg[:, 0:4], func=AF.Exp, bias=sm[:, 1:2], scale=1.0), reads=R, writes=R)
                P.op("dve", lambda e: e.reduce_sum(sm[:, 2:3], ge[:, :], AX.X), reads=R, writes=R)
                P.op("dve", lambda e: e.tensor_scalar(pen[:, :], oh[:, :], 1.0, 1e30, ALU.subtract, ALU.mult), reads=R, writes=R)
                for g in range(4):
                    P.op("dve", lambda e, g=g: e.tensor_scalar(EM[:, g * 8:(g + 1) * 8], Lg[:, 4 + g * 8:4 + (g + 1) * 8],
                                                                 pen[:, g:g + 1], None, ALU.add), reads=R, writes=R)
                P.op("dve", lambda e: e.max(out=M8[:, :], in_=EM[:, :]), reads=R, writes=R)
                P.op("dve", lambda e: e.tensor_scalar(sel[:, :], EM[:, :], M8[:, 1:2], None, ALU.is_ge), reads=R, writes=R)
                P.op("dve", lambda e: e.tensor_scalar(sm[:, 3:4], M8[:, 0:1], -1.0, None, ALU.mult), reads=R, writes=R)
                P.op("act", lambda e: e.activation(out=ew[:, :], in_=EM[:, :], func=AF.Exp, bias=sm[:, 3:4], scale=1.0), reads=R, writes=R)
                P.op("act", lambda e: e.activation(out=e2[:, :], in_=M8[:, 0:2], func=AF.Exp, bias=sm[:, 3:4], scale=1.0), reads=R, writes=R)
                P.op("dve", lambda e: e.reduce_sum(sm[:, 4:5], e2[:, :], AX.X), reads=R, writes=R)
                P.op("dve", lambda e: e.tensor_tensor(sm[:, 5:6], sm[:, 4:5], sm[:, 2:3], ALU.mult), reads=R, writes=R)
                P.op("dve", lambda e: e.reciprocal(sm[:, 6:7], sm[:, 5:6]), reads=R, writes=R)
                SELt, SEL1t, W12t = GATES
                P.op("dve", lambda e, j=j: e.tensor_copy(out=SELt[:, j, :], in_=sel[:, :]), reads=R, writes=R)
                P.op("dve", lambda e, j=j: e.tensor_scalar(SEL1t[:, j, :], EM[:, :], M8[:, 0:1], None, ALU.is_ge), reads=R, writes=R)
                P.op("dve", lambda e, j=j: e.tensor_copy(out=W12t[:, j, 0:1], in_=sm[:, 6:7]), reads=R, writes=R)
                P.op("dve", lambda e, j=j: e.tensor_tensor(W12t[:, j, 1:2], sm[:, 6:7], e2[:, 1:2], ALU.mult), reads=R, writes=R)

            load_res(0, 0)
            if NJ > 1:
                load_res(1, 1)
            emit_wo(0)
            emit_ln(0)
            for j in range(NJ):
                if j + 2 < NJ:
                    load_res(j + 2, (j + 2) % NR)
                if j + 1 < NJ:
                    emit_wo(j + 1)
                rbk = emit_tr(j)
                if j + 1 < NJ:
                    emit_ln(j + 1)
                emit_chain(j, rbk)

    def _moe(self, li, io, GATES):
        nc, P = self.nc, self.P
        NJ, TO = self.NJ, self.TO
        ps, psb = self.ps, self.psb
        npass = (NJ + 10) // 11
        base = NJ // npass
        sizes = [base + (1 if i < NJ % npass else 0) for i in range(npass)]
        PBmax = max(sizes)
        with ExitStack() as es:
            XTs = self.sb(es, "XTs", [128, DC, PBmax * 128], BF16)
            b_X = Buf()
            ACC = self.sb(es, "ACC", [128, PBmax, 1024], F32)
            accb = [Buf() for _ in range(PBmax)]
            Wg = [self.sb(es, "Wg%d" % i, [128, DC, DEXP], BF16) for i in range(2)]
            Wu = [self.sb(es, "Wu%d" % i, [128, DC, DEXP], BF16) for i in range(2)]
            Wd = [self.sb(es, "Wd%d" % i, [128, 4, 1024], BF16) for i in range(2)]
            wgb, wub, wdb = [Buf(), Buf()], [Buf(), Buf()], [Buf(), Buf()]
            AT = [self.sb(es, "AT%d" % i, [128, 4, 512], BF16) for i in range(2)]
            atb = [Buf(), Buf()]
            SG = [self.sb(es, "SG%d" % i, [128, 512], F32) for i in range(2)]
            sgb = [Buf(), Buf()]
            Gt = self.sb(es, "Gt2", [128, 1024], F32)
            Bt = self.sb(es, "Bt2", [128, 1024], F32)
            b_gb = Buf()
            P.dma("sp", Gt[:], io["lnf_g"].partition_broadcast(128), writes=[b_gb])
            P.dma("sp", Bt[:], io["lnf_b"].partition_broadcast(128), writes=[b_gb])
            Hres = [self.sb(es, "Hr2_%d" % i, [128, 1024], F32) for i in range(2)]
            hrb = [Buf(), Buf()]
            Z = self.sb(es, "Z2", [128, 1024], F32)
            b_Z = Buf()
            H2 = [self.sb(es, "H2_%d" % i, [128, 1024], F32) for i in range(2)]
            h2b = [Buf(), Buf()]
            ln = self._layernorm(es, "f")
            if io.get("sink", "final") != "final":
                HTo = [self.sb(es, "HTo%d" % i, [128, DC, 128], BF16) for i in range(2)]
                htob = [Buf(), Buf()]
                hT2_dv = io["hT2_d"].rearrange("(dc p) t -> p dc t", p=128)
            h1T_dv = io["H1T_d"].rearrange("(dc p) t -> p dc t", p=128)
            wg_v = io["w_gate"].rearrange("e (dc p) f -> e p dc f", p=128)
            wu_v = io["w_up"].rearrange("e (dc p) f -> e p dc f", p=128)
            wd_v = io["w_down"].rearrange("e (fc p) n -> e p fc n", p=128)

            def load_w(e):
                s = e % 2
                P.dma("pool", Wg[s][:], wg_v[e], writes=[wgb[s]])
                P.dma("pool", Wu[s][:], wu_v[e], writes=[wub[s]])
                P.dma("pool", Wd[s][:], wd_v[e], writes=[wdb[s]])

            tilec = 0
            fcc = 0
            jb = 0
            for pi in range(npass):
                PB = sizes[pi]
                ntok = PB * 128
                tok0 = jb * 128
                P.dma("sp", XTs[:, :, 0:ntok], h1T_dv[:, :, tok0:tok0 + ntok], writes=[b_X])
                load_w(0)
                for e in range(NEXP):
                    s = e % 2
                    if e + 1 < NEXP:
                        load_w(e + 1)
                    for off in range(0, ntok, 512):
                        n = min(512, ntok - off)
                        ta = tilec % 2
                        tilec += 1
                        for fc in range(4):
                            bg = [0, 1][fcc % 2]
                            bu = [2, 3][fcc % 2]
                            fcc += 1
                            for dc in range(DC):
                                P.op("pe", lambda e_, dc=dc, fc=fc, s=s, bg=bg, off=off, n=n: e_.matmul(
                                    ps[bg][:, 0:n], Wg[s][:, dc, fc * 128:(fc + 1) * 128], XTs[:, dc, off:off + n],
                                    start=(dc == 0), stop=(dc == DC - 1)),
                                    reads=[wgb[s], b_X], writes=[psb[bg]], inc=(dc == DC - 1))
                            for dc in range(DC):
                                P.op("pe", lambda e_, dc=dc, fc=fc, s=s, bu=bu, off=off, n=n: e_.matmul(
                                    ps[bu][:, 0:n], Wu[s][:, dc, fc * 128:(fc + 1) * 128], XTs[:, dc, off:off + n],
                                    start=(dc == 0), stop=(dc == DC - 1)),
                                    reads=[wub[s], b_X], writes=[psb[bu]], inc=(dc == DC - 1))
                            sg = fcc % 2
                            P.op("act", lambda e_, bg=bg, sg=sg, n=n: e_.activation(out=SG[sg][:, 0:n], in_=ps[bg][:, 0:n], func=AF.Silu),
                                 reads=[psb[bg]], writes=[sgb[sg]])
                            P.op("dve", lambda e_, bu=bu, sg=sg, ta=ta, fc=fc, n=n: e_.tensor_tensor(
                                AT[ta][:, fc, 0:n], SG[sg][:, 0:n], ps[bu][:, 0:n], ALU.mult),
                                reads=[sgb[sg], psb[bu]], writes=[atb[ta]])
                        for b in range(n // 128):
                            jj = (off // 128) + b
                            for h in range(2):
                                by = [4, 5, 6, 7][(b * 2 + h) % 4]
                                for fc in range(4):
                                    P.op("pe", lambda e_, fc=fc, b=b, h=h, by=by, ta=ta, s=s: e_.matmul(
                                        ps[by][:, :], AT[ta][:, fc, b * 128:(b + 1) * 128], Wd[s][:, fc, h * 512:(h + 1) * 512],
                                        start=(fc == 0), stop=(fc == 3)),
                                        reads=[atb[ta], wdb[s]], writes=[psb[by]], inc=(fc == 3))
                                gcol = GATES[:, jb + jj, e:e + 1]
                                if e == 0:
                                    P.op("dve", lambda e_, by=by, jj=jj, h=h, gcol=gcol: e_.tensor_scalar(
                                        ACC[:, jj, h * 512:(h + 1) * 512], ps[by][:, :], gcol, None, ALU.mult),
                                        reads=[psb[by]], writes=[accb[jj]])
                                else:
                                    P.op("dve", lambda e_, by=by, jj=jj, h=h, gcol=gcol: e_.scalar_tensor_tensor(
                                        ACC[:, jj, h * 512:(h + 1) * 512], ps[by][:, :], gcol, ACC[:, jj, h * 512:(h + 1) * 512],
                                        ALU.mult, ALU.add),
                                        reads=[psb[by], accb[jj]], writes=[accb[jj]])
                P.dma("sp", Hres[0][:], io["h1_d"][tok0:tok0 + 128, :], writes=[hrb[0]])
                for jj in range(PB):
                    s = jj % 2
                    j = jb + jj
                    if jj + 1 < PB:
                        P.dma("sp", Hres[1 - s][:], io["h1_d"][(j + 1) * 128:(j + 2) * 128, :], writes=[hrb[1 - s]])
                    P.op("dve", lambda e_, jj=jj, s=s: e_.scalar_tensor_tensor(
                        Z[:, :], Hres[s][:, :], ALPHA, ACC[:, jj, :], ALU.mult, ALU.add),
                        reads=[hrb[s], accb[jj]], writes=[b_Z])
                    ln(Z, b_Z, H2[s][:, :], h2b[s], Gt, Bt, b_gb)
                    sink = io.get("sink", "final")
                    if sink == "final":
                        P.dma("sp", io["h_out"][j * 128:(j + 1) * 128, :], H2[s][:, :], reads=[h2b[s]])
                    else:
                        gb = 2 * j + sink
                        P.dma("sp", io["h2_d"][gb * 128:(gb + 1) * 128, :], H2[s][:, :], reads=[h2b[s]])
                        for q in range(2):
                            bk = self.next_bank()
                            for k in range(4):
                                dc = q * 4 + k
                                P.op("pe", lambda e_, dc=dc, k=k, bk=bk, s=s: e_.transpose(
                                    ps[bk][:, k * 128:(k + 1) * 128], H2[s][:, dc * 128:(dc + 1) * 128], self.ident[:, :]),
                                    reads=[h2b[s]], writes=[psb[bk]], inc=(k == 3))
                            self.copy(self.ev_eng(), HTo[s][:, q * 4:(q + 1) * 4, :],
                                      ps[bk][:, :].rearrange("p (a b) -> p a b", b=128), [psb[bk]], [htob[s]])
                        P.dma("sp", hT2_dv[:, :, gb * 128:(gb + 1) * 128], HTo[s][:, :, :], reads=[htob[s]])
                jb += PB


    def queue_wconv(self, io):
        lst = getattr(self, "pending_conv", [])
        Wall_d = io["Wall_d"]
        for e in range(NEXP):
            rows = Wall_d[e * 128:(e + 1) * 128, :]
            lst.append((rows[:, 0:4096].rearrange("p (dc f) -> p dc f", f=512),
                        io["w_gate"][e].rearrange("(dc p) f -> p dc f", p=128)))
            lst.append((rows[:, 4096:8192].rearrange("p (dc f) -> p dc f", f=512),
                        io["w_up"][e].rearrange("(dc p) f -> p dc f", p=128)))
            lst.append((rows[:, 8192:12288].rearrange("p (fc n) -> p fc n", n=1024),
                        io["w_down"][e].rearrange("(fc p) n -> p fc n", p=128)))
        self.pending_conv = lst

    def _moe_routed(self, li, io, GATES):
        nc, P = self.nc, self.P
        NJ, TO = self.NJ, self.TO
        ps, psb = self.ps, self.psb
        SEL, SEL1, W12 = GATES
        TS = 256
        NT = (2 * TO + TS - 1) // TS + NEXP
        Xs_d, Ys_d, Wall_d = io["Xs_d"], io["Ys_d"], io["Wall_d"]
        ones_bf = self.ones_bf
        for (dst, src) in getattr(self, "pending_conv", []):
            P.dma("pool", dst, src, bulk=True)
        self.pending_conv = []
        with ExitStack() as es:
            LT = self.sb(es, "LT", [128, 128], BF16)
            iota = self.sb(es, "iota", [128, 1], F32)
            SELb = self.sb(es, "SELb", [128, NJ, NEXP], BF16)
            RSb = self.sb(es, "RSb", [128, NJ, NEXP], BF16)
            RANK = self.sb(es, "RANK", [128, NJ, NEXP], F32)
            TMP = self.sb(es, "TMPr", [128, NJ, NEXP], F32)
            SEL2 = self.sb(es, "SEL2", [128, NJ, NEXP], F32)
            TOT = self.sb(es, "TOT", [128, NEXP], F32)
            M1 = self.sb(es, "M1", [128, NEXP], F32)
            PADD = self.sb(es, "PADD", [128, NEXP], F32)
            END = self.sb(es, "END", [128, NEXP], F32)
            START = self.sb(es, "START", [128, NEXP], F32)
            ones32 = self.sb(es, "ones32", [128, NEXP], F32)
            cmp_ = self.sb(es, "cmp", [128, NEXP], F32)
            POSf = self.sb(es, "POSf", [128, 2, NJ], F32)
            POSi = self.sb(es, "POSi", [128, 2, NJ], mybir.dt.int32)
            EID = self.sb(es, "EID", [128, NT], F32)
            IDX = self.sb(es, "IDX", [128, NT], mybir.dt.int32)
            b_c = Buf()
            R = [b_c]
            P.dma("pool", LT[:], io["ltri"], writes=R)
            P.dma("sp", iota[:], io["iota_p"], writes=R)
            P.op("dve", lambda e: e.memset(ones32[:], 1.0), writes=R)
            P.op("dve", lambda e: e.tensor_copy(out=SELb[:], in_=SEL[:]), reads=R, writes=R)
            P.op("dve", lambda e: e.tensor_copy(out=RSb[:, 0, :], in_=SELb[:, 0, :]), reads=R, writes=R)
            for j in range(1, NJ):
                P.op("dve", lambda e, j=j: e.tensor_tensor(RSb[:, j, :], RSb[:, j - 1, :], SELb[:, j, :], ALU.add), reads=R, writes=R)
            for j in range(NJ):
                bk = self.next_bank()
                P.op("pe", lambda e, j=j, bk=bk: e.matmul(ps[bk][:, 0:NEXP], LT[:, :], SELb[:, j, :], start=True, stop=(j == 0)),
                     reads=R, writes=[psb[bk]], inc=(j == 0))
                if j > 0:
                    P.op("pe", lambda e, j=j, bk=bk: e.matmul(ps[bk][:, 0:NEXP], ones_bf[:, :], RSb[:, j - 1, :], start=False, stop=True),
                         reads=R, writes=[psb[bk]])
                P.op("act", lambda e, j=j, bk=bk: e.activation(out=RANK[:, j, :], in_=ps[bk][:, 0:NEXP], func=AF.Copy),
                     reads=[psb[bk]] + R, writes=R)
            bk = self.next_bank()
            P.op("pe", lambda e, bk=bk: e.matmul(ps[bk][:, 0:NEXP], ones_bf[:, :], RSb[:, NJ - 1, :], start=True, stop=True),
                 reads=R, writes=[psb[bk]])
            P.op("act", lambda e, bk=bk: e.activation(out=TOT[:, :], in_=ps[bk][:, 0:NEXP], func=AF.Copy), reads=[psb[bk]] + R, writes=R)
            P.op("dve", lambda e: e.memset(M1[:, :], 0.0), reads=R, writes=R)
            for m in range((2 * TO + TS - 1) // TS + 1):
                P.op("dve", lambda e, m=m: e.scalar_tensor_tensor(M1[:, :], TOT[:, :], float(m * TS), M1[:, :], ALU.is_gt, ALU.add),
                     reads=R, writes=R)
            P.op("dve", lambda e: e.tensor_scalar(PADD[:, :], M1[:, :], float(TS), None, ALU.mult), reads=R, writes=R)
            P.op("dve", lambda e: e.tensor_tensor_scan(END[:, :], ones32[:, :], PADD[:, :], 0.0, ALU.mult, ALU.add), reads=R, writes=R)
            P.op("dve", lambda e: e.tensor_tensor(START[:, :], END[:, :], PADD[:, :], ALU.subtract), reads=R, writes=R)
            for j in range(NJ):
                P.op("dve", lambda e, j=j: e.tensor_tensor(RANK[:, j, :], RANK[:, j, :], START[:, :], ALU.add), reads=R, writes=R)
            P.op("dve", lambda e: e.tensor_tensor(TMP[:], SEL1[:], RANK[:], ALU.mult), reads=R, writes=R)
            P.op("dve", lambda e: e.reduce_sum(POSf[:, 0, :], TMP[:], AX.X), reads=R, writes=R)
            P.op("dve", lambda e: e.tensor_tensor(SEL2[:], SEL[:], SEL1[:], ALU.subtract), reads=R, writes=R)
            P.op("dve", lambda e: e.tensor_tensor(TMP[:], SEL2[:], RANK[:], ALU.mult), reads=R, writes=R)
            P.op("dve", lambda e: e.reduce_sum(POSf[:, 1, :], TMP[:], AX.X), reads=R, writes=R)
            P.op("dve", lambda e: e.tensor_copy(out=POSi[:], in_=POSf[:]), reads=R, writes=R)
            for t in range(NT):
                P.op("dve", lambda e, t=t: e.tensor_scalar(cmp_[:, :], END[:, :], float(t * TS), 0.0, ALU.is_le, ALU.add,
                                                           accum_out=EID[:, t:t + 1]), reads=R, writes=R)
            P.op("dve", lambda e: e.tensor_scalar(EID[:, :], EID[:, :], float(NEXP - 1), 128.0, ALU.min, ALU.mult), reads=R, writes=R)
            P.op("dve", lambda e: e.tensor_scalar(EID[:, :], EID[:, :], iota[:, 0:1], None, ALU.add), reads=R, writes=R)
            P.op("dve", lambda e: e.tensor_copy(out=IDX[:], in_=EID[:]), reads=R, writes=R)

            Hb = [self.sb(es, "Hb%d" % i, [128, 1024], F32) for i in range(2)]
            hbb = [Buf(), Buf()]
            for j in range(NJ):
                s = j % 2
                P.dma("sp", Hb[s][:], io["h1_d"][j * 128:(j + 1) * 128, :], writes=[hbb[s]])
                for r in range(2):
                    P.idma(Xs_d[:, :], Hb[s][:, :], out_off=POSi[:, r, j:j + 1], reads=[hbb[s]] + R)
            P.barrier()

            Wall = [self.sb(es, "Wall%d" % i, [128, 12288], BF16) for i in range(2)]
            wlb = [Buf(), Buf()]
            Xsl = [self.sb(es, "Xsl%d" % i, [128, 2, 1024], F32) for i in range(2)]
            xslb = [Buf(), Buf()]
            XsT = [self.sb(es, "XsT%d" % i, [128, DC, TS], BF16) for i in range(2)]
            xstb = [Buf(), Buf()]
            AT = [self.sb(es, "ATr%d" % i, [128, 4, TS], BF16) for i in range(2)]
            atb = [Buf(), Buf()]
            SG = [self.sb(es, "SGr%d" % i, [128, TS], F32) for i in range(2)]
            sgb = [Buf(), Buf()]
            Yst = [self.sb(es, "Yst%d" % i, [128, 2, 1024], F32) for i in range(2)]
            ystb = [Buf(), Buf()]
            Xs_v = Xs_d.rearrange("(t s p) n -> t p s n", s=2, p=128)
            Ys_v = Ys_d.rearrange("(t s p) n -> t p s n", s=2, p=128)

            def load_tile(t):
                s = t % 2
                P.idma(Wall[s][:, :], Wall_d[:, :], in_off=IDX[:, t:t + 1], reads=R, writes=[wlb[s]])
                P.dma("sp", Xsl[s][:], Xs_v[t], writes=[xslb[s]])

            load_tile(0)
            fcc = 0
            for t in range(NT):
                s = t % 2
                if t + 1 < NT:
                    load_tile(t + 1)
                for sub in range(2):
                    for q in range(2):
                        bk = self.next_bank()
                        for k in range(4):
                            dc = q * 4 + k
                            P.op("pe", lambda e, dc=dc, k=k, bk=bk, s=s, sub=sub: e.transpose(
                                ps[bk][:, k * 128:(k + 1) * 128], Xsl[s][:, sub, dc * 128:(dc + 1) * 128], self.ident[:, :]),
                                reads=[xslb[s]], writes=[psb[bk]], inc=(k == 3))
                        self.copy(self.ev_eng(), XsT[s][:, q * 4:(q + 1) * 4, sub * 128:(sub + 1) * 128],
                                  ps[bk][:, :].rearrange("p (a b) -> p a b", b=128), [psb[bk]], [xstb[s]])
                for fc in range(4):
                    bg = self.next_bank()
                    for dc in range(DC):
                        P.op("pe", lambda e, dc=dc, fc=fc, s=s, bg=bg: e.matmul(
                            ps[bg][:, 0:TS], Wall[s][:, dc * 512 + fc * 128:dc * 512 + (fc + 1) * 128], XsT[s][:, dc, :],
                            start=(dc == 0), stop=(dc == DC - 1)),
                            reads=[wlb[s], xstb[s]], writes=[psb[bg]], inc=(dc == DC - 1))
                    bu = self.next_bank()
                    for dc in range(DC):
                        P.op("pe", lambda e, dc=dc, fc=fc, s=s, bu=bu: e.matmul(
                            ps[bu][:, 0:TS], Wall[s][:, 4096 + dc * 512 + fc * 128:4096 + dc * 512 + (fc + 1) * 128], XsT[s][:, dc, :],
                            start=(dc == 0), stop=(dc == DC - 1)),
                            reads=[wlb[s], xstb[s]], writes=[psb[bu]], inc=(dc == DC - 1))
                    sg = fcc % 2
                    fcc += 1
                    P.op("act", lambda e, bg=bg, sg=sg: e.activation(out=SG[sg][:, :], in_=ps[bg][:, 0:TS], func=AF.Silu),
                         reads=[psb[bg]], writes=[sgb[sg]])
                    P.op("dve", lambda e, bu=bu, sg=sg, s=s, fc=fc: e.tensor_tensor(AT[s][:, fc, :], SG[sg][:, :], ps[bu][:, 0:TS], ALU.mult),
                         reads=[sgb[sg], psb[bu]], writes=[atb[s]])
                for sub in range(2):
                    for h in range(2):
                        by = self.next_bank()
                        for fc in range(4):
                            P.op("pe", lambda e, fc=fc, sub=sub, h=h, by=by, s=s: e.matmul(
                                ps[by][:, :], AT[s][:, fc, sub * 128:(sub + 1) * 128],
                                Wall[s][:, 8192 + fc * 1024 + h * 512:8192 + fc * 1024 + (h + 1) * 512],
                                start=(fc == 0), stop=(fc == 3)),
                                reads=[atb[s], wlb[s]], writes=[psb[by]], inc=(fc == 3))
                        self.copy(self.ev_eng(), Yst[s][:, sub, h * 512:(h + 1) * 512], ps[by][:, :], [psb[by]], [ystb[s]])
                P.dma("sp", Ys_v[t], Yst[s][:], reads=[ystb[s]])
            P.barrier()

            Gt = self.sb(es, "Gt2", [128, 1024], F32)
            Bt = self.sb(es, "Bt2", [128, 1024], F32)
            b_gb = Buf()
            P.dma("sp", Gt[:], io["lnf_g"].partition_broadcast(128), writes=[b_gb])
            P.dma("sp", Bt[:], io["lnf_b"].partition_broadcast(128), writes=[b_gb])
            Hres = [self.sb(es, "Hr2_%d" % i, [128, 1024], F32) for i in range(2)]
            hrb = [Buf(), Buf()]
            Y1 = [self.sb(es, "Y1_%d" % i, [128, 1024], F32) for i in range(2)]
            Y2 = [self.sb(es, "Y2_%d" % i, [128, 1024], F32) for i in range(2)]
            y1b, y2b = [Buf(), Buf()], [Buf(), Buf()]
            Z = self.sb(es, "Z2", [128, 1024], F32)
            b_Z = Buf()
            H2 = [self.sb(es, "H2_%d" % i, [128, 1024], F32) for i in range(2)]
            h2b = [Buf(), Buf()]
            ln = self._layernorm(es, "f")
            sink = io.get("sink", "final")
            if sink != "final":
                HTo = [self.sb(es, "HTo%d" % i, [128, DC, 128], BF16) for i in range(2)]
                htob = [Buf(), Buf()]
                hT2_dv = io["hT2_d"].rearrange("(dc p) t -> p dc t", p=128)

            def load_blk(j):
                s = j % 2
                P.dma("sp", Hres[s][:], io["h1_d"][j * 128:(j + 1) * 128, :], writes=[hrb[s]])
                P.idma(Y1[s][:, :], Ys_d[:, :], in_off=POSi[:, 0, j:j + 1], reads=R, writes=[y1b[s]])
                P.idma(Y2[s][:, :], Ys_d[:, :], in_off=POSi[:, 1, j:j + 1], reads=R, writes=[y2b[s]])

            load_blk(0)
            for j in range(NJ):
                s = j % 2
                if j + 1 < NJ:
                    load_blk(j + 1)
                P.op("dve", lambda e, j=j, s=s: e.tensor_scalar(Z[:, :], Y1[s][:, :], W12[:, j, 0:1], None, ALU.mult),
                     reads=[y1b[s]], writes=[b_Z])
                P.op("dve", lambda e, j=j, s=s: e.scalar_tensor_tensor(Z[:, :], Y2[s][:, :], W12[:, j, 1:2], Z[:, :], ALU.mult, ALU.add),
                     reads=[y2b[s], b_Z], writes=[b_Z])
                P.op("dve", lambda e, s=s: e.scalar_tensor_tensor(Z[:, :], Hres[s][:, :], ALPHA, Z[:, :], ALU.mult, ALU.add),
                     reads=[hrb[s], b_Z], writes=[b_Z])
                ln(Z, b_Z, H2[s][:, :], h2b[s], Gt, Bt, b_gb)
                if sink == "final":
                    P.dma("sp", io["h_out"][j * 128:(j + 1) * 128, :], H2[s][:, :], reads=[h2b[s]])
                else:
                    gb = 2 * j + sink
                    P.dma("sp", io["h2_d"][gb * 128:(gb + 1) * 128, :], H2[s][:, :], reads=[h2b[s]])
                    for q in range(2):
                        bk = self.next_bank()
                        for k in range(4):
                            dc = q * 4 + k
                            P.op("pe", lambda e_, dc=dc, k=k, bk=bk, s=s: e_.transpose(
                                ps[bk][:, k * 128:(k + 1) * 128], H2[s][:, dc * 128:(dc + 1) * 128], self.ident[:, :]),
                                reads=[h2b[s]], writes=[psb[bk]], inc=(k == 3))
                        self.copy(self.ev_eng(), HTo[s][:, q * 4:(q + 1) * 4, :],
                                  ps[bk][:, :].rearrange("p (a b) -> p a b", b=128), [psb[bk]], [htob[s]])
                    P.dma("sp", hT2_dv[:, :, gb * 128:(gb + 1) * 128], HTo[s][:, :, :], reads=[htob[s]])


def build_fused_program(LP):
    nc = bass.Bass("TRN2", target_bir_lowering=False)
    NB = LP // 128
    NJ = NB // 2
    TO = NJ * 128

    def din(name, shape, dt=F32):
        return nc.dram_tensor(name, list(shape), dt, kind="ExternalInput").ap()

    def dscr(name, shape, dt):
        return nc.dram_tensor(name, list(shape), dt, kind="Internal").ap()

    g = {}
    g["xT_seq0"] = din("xT_seq0", [D, LP])
    g["xT_own0"] = din("xT_own0", [2, D, TO])
    g["x_own0"] = din("x_own0", [2, TO, D])
    g["ident"] = din("ident", [128, 128])
    g["nb_mask_s"] = din("nb_mask_s", [2, 3, 128, 128])
    g["nb_T_s"] = din("nb_T_s", [2, 3, 16, 128, 128])
    g["nb_mask_d"] = din("nb_mask_d", [3, 128, 128])
    g["b31"] = din("b31", [16])
    g["lam"] = din("lam", [256])
    g["subln"] = din("subln", [128, 1])
    g["b_f"] = din("b_f", [16, 1])
    g["npar"] = din("npar", [16, 2])
    g["par"] = din("par", [128, 2])
    g["pars"] = din("pars", [128, 2])
    for li in range(2):
        g["w_in%d" % li] = din("w_in%d" % li, [D, 3072 if li == 0 else 3088])
        g["w_o%d" % li] = din("w_o%d" % li, [D, D])
        for k in ("lnm_g", "lnm_b", "lnf_g", "lnf_b"):
            g["%s%d" % (k, li)] = din("%s%d" % (k, li), [D])
        g["w_r%d" % li] = din("w_r%d" % li, [D, 36])
        g["b_r%d" % li] = din("b_r%d" % li, [36])
        g["w_gate%d" % li] = din("w_gate%d" % li, [NEXP, D, DEXP])
        g["w_up%d" % li] = din("w_up%d" % li, [NEXP, D, DEXP])
        g["w_down%d" % li] = din("w_down%d" % li, [NEXP, DEXP, D])
    g["CQ3_d"] = dscr("CQ3_d", [16, 3, TO], BF16)
    g["KT_d"] = dscr("KT_d", [D, LP], BF16)
    g["QT_d"] = dscr("QT_d", [D, TO], BF16)
    g["V_d"] = dscr("V_d", [LP, D], BF16)
    g["h1_d"] = dscr("h1_d", [TO, D], F32)
    g["H1T_d"] = dscr("H1T_d", [D, TO], BF16)
    g["h2_d"] = dscr("h2_d", [LP, D], F32)
    g["hT2_d"] = dscr("hT2_d", [D, LP], BF16)
    g["h_out"] = nc.dram_tensor("h_out", [TO, D], F32, kind="ExternalOutput").ap()
    NT = (2 * TO + 255) // 256 + NEXP
    g["Xs_d"] = dscr("Xs_d", [NT * 256, D], F32)
    g["Ys_d"] = dscr("Ys_d", [NT * 256, D], F32)
    g["Wall_d0"] = dscr("Wall_d0", [NEXP * 128, 12288], BF16)
    g["Wall_d1"] = dscr("Wall_d1", [NEXP * 128, 12288], BF16)
    g["ltri"] = din("ltri", [128, 128])
    g["iota_p"] = din("iota_p", [128, 1])

    def layer_io(li):
        io = {}
        for k in ("w_in", "w_o", "lnm_g", "lnm_b", "lnf_g", "lnf_b", "w_r", "b_r", "w_gate", "w_up", "w_down"):
            io[k] = g["%s%d" % (k, li)]
        for k in ("ident", "KT_d", "QT_d", "V_d", "h1_d", "H1T_d", "h2_d", "hT2_d", "h_out", "CQ3_d",
                  "b31", "lam", "subln", "b_f", "npar", "par", "pars", "Xs_d", "Ys_d", "ltri", "iota_p"):
            io[k] = g[k]
        io["Wall_d"] = g["Wall_d%d" % li]
        return io

    with ExitStack() as es:
        P = Prog(nc, es)
        em = LayerEmitter(nc, P, es, LP)
        for c in range(2):
            io = layer_io(0)
            io.update(xT_seq=g["xT_seq0"], xT_own=g["xT_own0"][c], x_own=g["x_own0"][c],
                      nb_mask=g["nb_mask_s"][c], nb_T=g["nb_T_s"][c],
                      do_kv=(c == 0), q_mode="static", res_mode="static", sink=c)
            em.queue_wconv(layer_io(c))
            with ExitStack() as es_layer:
                em.emit_layer("diff", 0, io, es_layer)
            P.barrier()
        io = layer_io(1)
        io.update(xT_seq=g["hT2_d"], nb_mask=g["nb_mask_d"], do_kv=True, q_mode="dyn", res_mode="dyn", sink="final")
        with ExitStack() as es_layer:
            em.emit_layer("fox", 1, io, es_layer)
        P.barrier()
    return nc, P


def _t5_bucket_np(dist):
    n = np.maximum(np.asarray(dist, np.int32), 0)
    nf = np.maximum(n, 1).astype(np.float32)
    large = 16 + (np.log(nf / np.float32(16)) / np.float32(math.log(128 / 16)) * np.float32(16)).astype(np.int32)
    large = np.minimum(large, 31)
    return np.where(n < 16, n, large)


def _near_tables(c):
    qi = np.arange(128)[None, :]
    ki = np.arange(128)[:, None]
    idx = np.zeros((3, 128, 128), np.int32)
    msk = np.zeros((3, 128, 128), np.float32)
    for s in range(3):
        dist = (c + 1 - s) * 128 + qi - ki
        idx[s] = _t5_bucket_np(np.maximum(dist, 0))
        msk[s] = np.where(dist < 0, NEGM, 0.0)
    return idx, msk


_PROG_CACHE = {}


def _get_prog(LP):
    if LP not in _PROG_CACHE:
        _PROG_CACHE[LP] = build_fused_program(LP)[0]
    return _PROG_CACHE[LP]


def kernel(**inputs):
    inp = {k: np.asarray(v) for k, v in inputs.items()}
    x = inp["x"].astype(np.float32, copy=False)
    B, S, _ = x.shape
    L = S + N_META
    LP = ((L + 255) // 256) * 256
    NB = LP // 128
    NJ = NB // 2
    h = np.zeros((B, LP, D), np.float32)
    h[:, :N_META] = inp["meta_tokens"][None]
    h[:, N_META:L] = x
    nc = _get_prog(LP)
    ca = np.ascontiguousarray
    common = {"ident": np.eye(128, dtype=np.float32)}
    common["ltri"] = np.triu(np.ones((128, 128), np.float32), 1)
    common["iota_p"] = np.arange(128, dtype=np.float32).reshape(128, 1)
    idx0, msk0 = _near_tables(0)
    idx1, msk1 = _near_tables(1)
    rb = inp["rel_bias"]
    common["nb_mask_s"] = ca(np.stack([msk0, msk1]))
    common["nb_T_s"] = ca(np.stack([np.transpose(rb[idx0], (0, 3, 1, 2)), np.transpose(rb[idx1], (0, 3, 1, 2))]))
    common["b31"] = ca(rb[31])
    common["lam"] = ca(inp["diff_lambda"][0].reshape(256))
    common["subln"] = ca(inp["diff_subln"][0].reshape(128, 1))
    common["b_f"] = ca(inp["fox_b_f"][0].reshape(16, 1))
    for li in range(2):
        common["w_in%d" % li] = ca(inp["diff_w_qkv"][0] if li == 0 else inp["fox_w_in"][0])
        common["w_o%d" % li] = ca(inp["diff_w_o"][0] if li == 0 else inp["fox_w_o"][0])
        common["lnm_g%d" % li] = ca(inp["ln_mix_g"][li])
        common["lnm_b%d" % li] = ca(inp["ln_mix_b"][li])
        common["lnf_g%d" % li] = ca(inp["ln_ffn_g"][li])
        common["lnf_b%d" % li] = ca(inp["ln_ffn_b"][li])
        common["w_r%d" % li] = ca(np.concatenate([inp["router_group_w"][li], inp["router_expert_w"][li].reshape(D, 32)], axis=1))
        common["b_r%d" % li] = ca(np.concatenate([inp["router_group_b"][li], inp["router_expert_b"][li].reshape(32)]))
        common["w_gate%d" % li] = ca(inp["expert_w_gate"][li])
        common["w_up%d" % li] = ca(inp["expert_w_up"][li])
        common["w_down%d" % li] = ca(inp["expert_w_down"][li])
    in_maps = []
    for b in range(B):
        hT = ca(h[b].T)
        hb = h[b].reshape(NJ, 2, 128, D)
        own = ca(np.transpose(hb, (1, 0, 2, 3)).reshape(2, NJ * 128, D))
        ownT = ca(np.transpose(own, (0, 2, 1)))
        for c in range(2):
            m = dict(common)
            m["xT_seq0"] = hT
            m["x_own0"] = own
            m["xT_own0"] = ownT
            m["nb_mask_d"] = msk0 if c == 0 else msk1
            sel = np.zeros((128, 2), np.float32)
            sel[:, c] = 1.0
            m["par"] = sel
            m["pars"] = sel * np.float32(0.125)
            m["npar"] = ca(-sel[:16])
            in_maps.append(m)
    res = run_bass_kernel_spmd(nc, in_maps, core_ids=list(range(len(in_maps))))
    out = np.zeros((B, NJ, 2, 128, D), np.float32)
    k = 0
    for b in range(B):
        for c in range(2):
            out[b, :, c] = np.asarray(res.results[k]["h_out"]).reshape(NJ, 128, D)
            k += 1
    return ca(out.reshape(B, LP, D)[:, N_META:L])
```

```python
import math
from contextlib import ExitStack

import numpy as np
import concourse.bass as bass
import concourse.mybir as mybir
from concourse.bass_utils import run_bass_kernel_spmd

F32 = mybir.dt.float32
BF16 = mybir.dt.bfloat16
AF = mybir.ActivationFunctionType
ALU = mybir.AluOpType
AX = mybir.AxisListType

D = 1024
DC = 8
N_META = 16
NEXP = 32
DEXP = 512
DEPTH = 2
ALPHA = (2 * DEPTH) ** 0.25
LN_EPS = 1e-5
NEGM = -30000.0
NDS = 56
NBULK = 8
NSP = 20
import os
DEBUG = os.environ.get("KDEBUG", "0") == "1"
DBG = {}


def diff_lambda_init(layer_idx):
    return 0.8 - 0.6 * math.exp(-0.3 * layer_idx)


class Buf:
    __slots__ = ("w", "r", "excl")

    def __init__(self, excl=False):
        self.w = None
        self.r = {}
        self.excl = excl


class Prog:
    def __init__(self, nc, es):
        self.nc = nc
        self.E = {"pe": nc.tensor, "act": nc.scalar, "dve": nc.vector, "pool": nc.gpsimd, "sp": nc.sync}
        self.semobj = {}
        for k in self.E:
            self.semobj[k] = es.enter_context(nc.semaphore("s_" + k))
        self.cnt = {k: 0 for k in self.E}
        self.pending = {k: False for k in self.E}
        self.waited = {k: {} for k in self.E}
        self.dval = [0] * NDS
        for i in range(NDS):
            self.semobj[("d", i)] = es.enter_context(nc.semaphore("d%d" % i))
        self.dnext = 0
        self.bnext = 0
        self.pnext = 0
        self.nins = 0

    def _wait(self, eng, tok):
        key, val = tok
        if self.waited[eng].get(key, 0) >= val:
            return
        self.E[eng].wait_ge(self.semobj[key], val)
        self.waited[eng][key] = val

    def _deps(self, eng, reads, writes):
        toks = []
        for b in reads:
            if b.w is not None:
                toks.append(b.w)
            if b.excl:
                toks.extend(b.r.items())
        for b in writes:
            if b.w is not None:
                toks.append(b.w)
            toks.extend(b.r.items())
        for t in toks:
            if eng == "pe" and t[0] == "pe":
                continue
            self._wait(eng, t)

    def _mark(self, tok, reads, writes):
        for b in reads:
            if b.excl:
                b.w = tok
                b.r = {}
            else:
                b.r[tok[0]] = tok[1]
        for b in writes:
            b.w = tok
            b.r = {}

    def op(self, eng, fn, reads=(), writes=(), inc=True):
        self._deps(eng, reads, writes)
        ins = fn(self.E[eng])
        self.nins += 1
        if inc:
            self.cnt[eng] += 1
            ins.then_inc(self.semobj[eng], 1)
            tok = (eng, self.cnt[eng])
            self.pending[eng] = False
        else:
            tok = (eng, self.cnt[eng] + 1)
            self.pending[eng] = True
        self._mark(tok, reads, writes)
        return tok

    def dma(self, eng, out, in_, reads=(), writes=(), bulk=False):
        if bulk:
            i = NDS - NBULK + self.bnext
            self.bnext = (self.bnext + 1) % NBULK
        elif eng == "pool":
            i = NSP + self.pnext
            self.pnext = (self.pnext + 1) % (NDS - NBULK - NSP)
        else:
            i = self.dnext
            self.dnext = (i + 1) % NSP
        key = ("d", i)
        if self.dval[i] > 0:
            self._wait(eng, (key, self.dval[i]))
        self._deps(eng, reads, writes)
        ins = self.E[eng].dma_start(out=out, in_=in_)
        self.nins += 1
        self.dval[i] += 16
        ins.then_inc(self.semobj[key], 16)
        tok = (key, self.dval[i])
        self._mark(tok, reads, writes)
        return tok

    def idma(self, out, in_, out_off=None, in_off=None, reads=(), writes=()):
        eng = "pool"
        i = NSP + self.pnext
        self.pnext = (self.pnext + 1) % (NDS - NBULK - NSP)
        key = ("d", i)
        if self.dval[i] > 0:
            self._wait(eng, (key, self.dval[i]))
        self._deps(eng, reads, writes)
        oo = bass.IndirectOffsetOnAxis(ap=out_off, axis=0) if out_off is not None else None
        io_ = bass.IndirectOffsetOnAxis(ap=in_off, axis=0) if in_off is not None else None
        ins = self.E[eng].indirect_dma_start(out=out, out_offset=oo, in_=in_, in_offset=io_)
        self.nins += 1
        self.dval[i] += 16
        ins.then_inc(self.semobj[key], 16)
        tok = (key, self.dval[i])
        self._mark(tok, reads, writes)
        return tok

    def barrier(self):
        for k in self.E:
            assert not self.pending[k], k
        toks = [(k, self.cnt[k]) for k in self.E if self.cnt[k] > 0]
        toks += [(("d", i), v) for i, v in enumerate(self.dval) if v > 0]
        for eng in self.E:
            for t in toks:
                self._wait(eng, t)


def _divisor_le(n, m):
    for d in range(min(n, m), 0, -1):
        if n % d == 0:
            return d
    return 1


class LayerEmitter:
    def __init__(self, nc, P, es, LP):
        self.nc = nc
        self.P = P
        self.LP = LP
        self.NB = LP // 128
        self.NJ = self.NB // 2
        self.TO = self.NJ * 128
        self.ps = []
        self.psb = []
        for i in range(8):
            t = es.enter_context(nc.psum_tensor("ps%d" % i, [128, 512], F32))
            self.ps.append(t)
            self.psb.append(Buf(excl=True))
        self.psrr = 0
        self.evrr = 0

    def sb(self, es, name, shape, dt):
        self.sbn = getattr(self, "sbn", 0) + 1
        return es.enter_context(self.nc.sbuf_tensor("sb%d_%s" % (self.sbn, name), shape, dt))

    def next_bank(self):
        i = self.psrr
        self.psrr = (i + 1) % 8
        return i

    def ev_eng(self):
        self.evrr ^= 1
        return "act" if self.evrr else "dve"

    def copy(self, eng, out, in_, reads, writes, scale=None):
        P = self.P
        if eng == "act":
            if scale is None:
                return P.op("act", lambda e: e.activation(out=out, in_=in_, func=AF.Copy), reads, writes)
            return P.op("act", lambda e: e.activation(out=out, in_=in_, func=AF.Copy, scale=scale), reads, writes)
        if scale is None:
            return P.op(eng, lambda e: e.tensor_copy(out=out, in_=in_), reads, writes)
        return P.op(eng, lambda e: e.tensor_scalar(out, in_, scale, None, ALU.mult), reads, writes)

    def emit_layer(self, ltype, li, io, es_layer):
        nc, P = self.nc, self.P
        LP, NB, NJ, TO = self.LP, self.NB, self.NJ, self.TO
        ps, psb = self.ps, self.psb
        fox = ltype == "fox"
        NCOL = 3088 if fox else 3072
        lam_init = diff_lambda_init(li)
        do_kv = io.get("do_kv", True)
        q_mode = io.get("q_mode", "static")

        ident = self.sb(es_layer, "ident%d" % li, [128, 128], F32)
        self.ident = ident
        ones_bf = self.sb(es_layer, "ones_bf%d" % li, [128, 128], BF16)
        ones_f = self.sb(es_layer, "ones_f%d" % li, [128, 128], F32)
        cb = Buf()
        P.dma("sp", ident[:], io["ident"], writes=[cb])
        P.op("dve", lambda e: e.memset(ones_bf[:], 1.0), writes=[cb])
        P.op("dve", lambda e: e.memset(ones_f[:], 1.0), writes=[cb])
        SEL = self.sb(es_layer, "SEL%d" % li, [128, NJ, NEXP], F32)
        SEL1 = self.sb(es_layer, "SEL1_%d" % li, [128, NJ, NEXP], F32)
        W12 = self.sb(es_layer, "W12_%d" % li, [128, NJ, 2], F32)
        GATES = (SEL, SEL1, W12)
        self.ones_bf = ones_bf
        NCK = None
        if fox:
            NCK = self.sb(es_layer, "nck%d" % li, [128, NB * 16], F32)

        xT_seq = io["xT_seq"].rearrange("(dc p) t -> p dc t", p=128)
        xT_own = io["xT_own"].rearrange("(dc p) t -> p dc t", p=128) if q_mode == "static" else None
        w_in = io["w_in"].rearrange("(dc p) n -> p dc n", p=128)
        KT_d, QT_d, V_d = io["KT_d"], io["QT_d"], io["V_d"]
        KT_dv = KT_d.rearrange("(c p) t -> p c t", p=128)
        QT_dv = QT_d.rearrange("(c p) t -> p c t", p=128)
        V_dv = V_d.rearrange("(b p) n -> p b n", p=128)

        if fox:
            with ExitStack() as es:
                pairs = _divisor_le(NJ, 11)
                CH = pairs * 256
                nchunks = NJ // pairs
                Wf = self.sb(es, "wf", [128, DC, 16], BF16)
                nbf = self.sb(es, "nbf", [16, 1], F32)
                npar = self.sb(es, "npar", [16, 2], F32)
                ones_s = self.sb(es, "ones_s", [16, CH], F32)
                carry = self.sb(es, "carry", [16, 1], F32)
                hTs = [self.sb(es, "hTf%d" % i, [128, DC, 512], BF16) for i in range(2)]
                hTb = [Buf(), Buf()]
                Et = self.sb(es, "Et", [16, 512], F32)
                LF = self.sb(es, "LF", [16, CH], F32)
                CN = self.sb(es, "CN", [16, CH], F32)
                T1 = self.sb(es, "T1", [16, pairs * 128], F32)
                CQ = self.sb(es, "CQ", [16, pairs * 128], F32)
                R1 = self.sb(es, "R1", [16, pairs * 128], F32)
                R2 = self.sb(es, "R2", [16, pairs * 128], F32)
                C3 = self.sb(es, "C3", [16, 3, pairs * 128], BF16)
                b_wf, b_nbf, b_par, b_ones, b_carry = Buf(), Buf(), Buf(), Buf(), Buf()
                b_Et, b_LF, b_CN, b_T1, b_CQ, b_R1, b_R2, b_C3 = (Buf() for _ in range(8))
                b_nck = Buf()
                P.dma("pool", Wf[:], w_in[:, :, 3072:3088], writes=[b_wf])
                P.dma("sp", nbf[:], io["b_f"], writes=[b_nbf])
                P.op("dve", lambda e: e.tensor_scalar(nbf[:], nbf[:], -1.0, None, ALU.mult), reads=[b_nbf], writes=[b_nbf])
                P.dma("sp", npar[:], io["npar"], writes=[b_par])
                P.op("dve", lambda e: e.memset(ones_s[:], 1.0), writes=[b_ones])
                P.op("dve", lambda e: e.memset(carry[:], 0.0), writes=[b_carry])
                tcount = 0
                for ci in range(nchunks):
                    t0 = ci * CH
                    off = 0
                    while off < CH:
                        n = min(512, CH - off)
                        s = tcount % 2
                        tcount += 1
                        P.dma("pool", hTs[s][:, :, 0:n], xT_seq[:, :, t0 + off:t0 + off + n], writes=[hTb[s]])
                        bk = self.next_bank()
                        for dc in range(DC):
                            P.op("pe", lambda e, dc=dc, s=s, n=n, bk=bk: e.matmul(
                                ps[bk][0:16, 0:n], Wf[:, dc, :], hTs[s][:, dc, 0:n], start=(dc == 0), stop=(dc == DC - 1)),
                                reads=[b_wf, hTb[s]], writes=[psb[bk]], inc=(dc == DC - 1))
                        P.op("act", lambda e, n=n, bk=bk: e.activation(out=Et[:, 0:n], in_=ps[bk][0:16, 0:n], func=AF.Exp,
                                                                 bias=nbf[:, 0:1], scale=-1.0),
                             reads=[psb[bk], b_nbf], writes=[b_Et])
                        P.op("act", lambda e, n=n, off=off: e.activation(out=LF[:, off:off + n], in_=Et[:, 0:n], func=AF.Ln,
                                                                       bias=1.0, scale=1.0),
                             reads=[b_Et], writes=[b_LF])
                        off += n
                    P.op("dve", lambda e: e.tensor_tensor_scan(CN[:, :], ones_s[:, :], LF[:, :], carry[:, 0:1], ALU.mult, ALU.add),
                         reads=[b_ones, b_LF, b_carry], writes=[b_CN])
                    P.op("dve", lambda e: e.tensor_copy(out=carry[:, 0:1], in_=CN[:, CH - 1:CH]), reads=[b_CN], writes=[b_carry])
                    nblk = CH // 128
                    bk = self.next_bank()
                    for b in range(nblk):
                        P.op("pe", lambda e, b=b, bk=bk: e.transpose(ps[bk][:, b * 16:(b + 1) * 16], CN[:, b * 128:(b + 1) * 128],
                                                                  ident[0:16, 0:16]),
                             reads=[b_CN, cb], writes=[psb[bk]], inc=(b == nblk - 1))
                    g0 = t0 // 128
                    P.op("act", lambda e, bk=bk, g0=g0, nblk=nblk: e.activation(out=NCK[:, g0 * 16:(g0 + nblk) * 16],
                                                                               in_=ps[bk][:, 0:nblk * 16], func=AF.Copy),
                         reads=[psb[bk]], writes=[b_nck])
                    CNv = CN[:, :].rearrange("p (a two q) -> p a two q", two=2, q=128)
                    T1v = T1[:, :].rearrange("p (a q) -> p a q", q=128)
                    CQv = CQ[:, :].rearrange("p (a q) -> p a q", q=128)
                    P.op("dve", lambda e: e.tensor_scalar(T1v, CNv[:, :, 1, :], npar[:, 1:2], None, ALU.mult),
                         reads=[b_CN, b_par], writes=[b_T1])
                    P.op("dve", lambda e: e.scalar_tensor_tensor(CQv, CNv[:, :, 0, :], npar[:, 0:1], T1v, ALU.mult, ALU.add),
                         reads=[b_CN, b_par, b_T1], writes=[b_CQ])
                    P.op("dve", lambda e: e.tensor_copy(out=C3[:, 0, :], in_=CQ[:, :]), reads=[b_CQ], writes=[b_C3])
                    P.op("dve", lambda e: e.tensor_tensor(R1[:, :], CQ[:, :], C3[:, 0, :], ALU.subtract), reads=[b_CQ, b_C3], writes=[b_R1])
                    P.op("dve", lambda e: e.tensor_copy(out=C3[:, 1, :], in_=R1[:, :]), reads=[b_R1], writes=[b_C3])
                    P.op("dve", lambda e: e.tensor_tensor(R2[:, :], R1[:, :], C3[:, 1, :], ALU.subtract), reads=[b_R1, b_C3], writes=[b_R2])
                    P.op("dve", lambda e: e.tensor_copy(out=C3[:, 2, :], in_=R2[:, :]), reads=[b_R2], writes=[b_C3])
                    o0 = ci * pairs * 128
                    P.dma("sp", io["CQ3_d"][:, :, o0:o0 + pairs * 128], C3[:, :, :], reads=[b_C3])
                P.barrier()

        with ExitStack() as es:
            Wb = self.sb(es, "Wb", [128, DC, 3072], BF16)
            b_W = Buf()
            for pc in range(6 if do_kv else 2):
                P.dma("pool", Wb[:, :, pc * 512:(pc + 1) * 512], w_in[:, :, pc * 512:(pc + 1) * 512], writes=[b_W])
            if q_mode == "dyn":
                stq = [self.sb(es, "stq%d" % i, [128, 8, 256], BF16) for i in range(2)]
                stqb = [Buf(), Buf()]
                tmpq = self.sb(es, "tmpq", [128, 2, 128], F32)
                b_tmpq = Buf()
                pars = self.sb(es, "pars", [128, 2], F32)
                b_pars = Buf()
                P.dma("sp", pars[:], io["pars"], writes=[b_pars])
            hTs = [self.sb(es, "hTa%d" % i, [128, DC, 512], BF16) for i in range(2)]
            hTb = [Buf(), Buf()]
            stk = [self.sb(es, "stk%d" % i, [128, 8, 512], BF16) for i in range(2)]
            stkb = [Buf(), Buf()]
            stv = [self.sb(es, "stv%d" % i, [128, 4, 1024], BF16) for i in range(2)]
            stvb = [Buf(), Buf()]
            tcount = 0
            for t0 in (range(0, LP, 512) if do_kv else []):
                n = min(512, LP - t0)
                s = tcount % 2
                tcount += 1
                P.dma("pool", hTs[s][:, :, 0:n], xT_seq[:, :, t0:t0 + n], writes=[hTb[s]])
                for c in range(8):
                    bk = self.next_bank()
                    for dc in range(DC):
                        P.op("pe", lambda e, c=c, dc=dc, s=s, n=n, bk=bk: e.matmul(
                            ps[bk][:, 0:n], Wb[:, dc, 1024 + c * 128:1024 + (c + 1) * 128], hTs[s][:, dc, 0:n],
                            start=(dc == 0), stop=(dc == DC - 1)),
                            reads=[b_W, hTb[s]], writes=[psb[bk]], inc=(dc == DC - 1))
                    self.copy(self.ev_eng(), stk[s][:, c, 0:n], ps[bk][:, 0:n], [psb[bk]], [stkb[s]])
                P.dma("sp", KT_dv[:, :, t0:t0 + n], stk[s][:, :, 0:n], reads=[stkb[s]])
                nb = n // 128
                for b in range(nb):
                    for half in range(2):
                        bk = self.next_bank()
                        for dc in range(DC):
                            P.op("pe", lambda e, b=b, half=half, dc=dc, s=s, bk=bk: e.matmul(
                                ps[bk][:, :], hTs[s][:, dc, b * 128:(b + 1) * 128],
                                Wb[:, dc, 2048 + half * 512:2048 + (half + 1) * 512],
                                start=(dc == 0), stop=(dc == DC - 1)),
                                reads=[b_W, hTb[s]], writes=[psb[bk]], inc=(dc == DC - 1))
                        self.copy(self.ev_eng(), stv[s][:, b, half * 512:(half + 1) * 512], ps[bk][:, :], [psb[bk]], [stvb[s]])
                blk0 = t0 // 128
                P.dma("sp", V_dv[:, blk0:blk0 + nb, :], stv[s][:, 0:nb, :], reads=[stvb[s]])
                if q_mode == "dyn":
                    npair = n // 256
                    for c in range(8):
                        bk = self.next_bank()
                        for dc in range(DC):
                            P.op("pe", lambda e, c=c, dc=dc, s=s, n=n, bk=bk: e.matmul(
                                ps[bk][:, 0:n], Wb[:, dc, c * 128:(c + 1) * 128], hTs[s][:, dc, 0:n],
                                start=(dc == 0), stop=(dc == DC - 1)),
                                reads=[b_W, hTb[s]], writes=[psb[bk]], inc=(dc == DC - 1))
                        psv = ps[bk][:, 0:n].rearrange("p (a two q) -> p a two q", two=2, q=128)
                        P.op("dve", lambda e, psv=psv, npair=npair: e.tensor_scalar(
                            tmpq[:, 0:npair, :], psv[:, :, 1, :], pars[:, 1:2], None, ALU.mult),
                            reads=[psb[bk], b_pars], writes=[b_tmpq])
                        P.op("dve", lambda e, psv=psv, npair=npair, c=c, s=s: e.scalar_tensor_tensor(
                            stq[s][:, c, 0:npair * 128].rearrange("p (a q) -> p a q", q=128), psv[:, :, 0, :], pars[:, 0:1],
                            tmpq[:, 0:npair, :], ALU.mult, ALU.add),
                            reads=[psb[bk], b_pars, b_tmpq], writes=[stqb[s]])
                    P.dma("sp", QT_dv[:, :, t0 // 2:t0 // 2 + npair * 128], stq[s][:, :, 0:npair * 128], reads=[stqb[s]])
            for t0 in (range(0, TO, 512) if q_mode == "static" else []):
                n = min(512, TO - t0)
                s = tcount % 2
                tcount += 1
                P.dma("pool", hTs[s][:, :, 0:n], xT_own[:, :, t0:t0 + n], writes=[hTb[s]])
                for c in range(8):
                    bk = self.next_bank()
                    for dc in range(DC):
                        P.op("pe", lambda e, c=c, dc=dc, s=s, n=n, bk=bk: e.matmul(
                            ps[bk][:, 0:n], Wb[:, dc, c * 128:(c + 1) * 128], hTs[s][:, dc, 0:n],
                            start=(dc == 0), stop=(dc == DC - 1)),
                            reads=[b_W, hTb[s]], writes=[psb[bk]], inc=(dc == DC - 1))
                    self.copy(self.ev_eng(), stk[s][:, c, 0:n], ps[bk][:, 0:n], [psb[bk]], [stkb[s]], scale=0.125)
                P.dma("sp", QT_dv[:, :, t0:t0 + n], stk[s][:, :, 0:n], reads=[stkb[s]])
            P.barrier()

        with ExitStack() as es_x:
            XT = self.sb(es_x, "XT", [128, DC, TO], BF16)
            self._attention(ltype, li, io, XT, ident, ones_bf, ones_f, NCK, lam_init)
            P.barrier()
            self._post_attention(li, io, XT, ident, GATES)
            P.barrier()
        self._moe_routed(li, io, GATES)
        P.barrier()

    def _attention(self, ltype, li, io, XT, ident, ones_bf, ones_f, NCK, lam_init):
        nc, P = self.nc, self.P
        LP, NB, NJ, TO = self.LP, self.NB, self.NJ, self.TO
        ps, psb = self.ps, self.psb
        fox = ltype == "fox"
        Kd = 67 if fox else 64
        KT_d, QT_d, V_d = io["KT_d"], io["QT_d"], io["V_d"]
        with ExitStack() as es:
            KT = [self.sb(es, "KT%d" % i, [128, LP], BF16) for i in range(2)]
            QT = [self.sb(es, "QT%d" % i, [128, TO], BF16) for i in range(2)]
            Vl = [self.sb(es, "Vl%d" % i, [128, NB, 128], BF16) for i in range(2)]
            ktb, qtb, vlb = [Buf(), Buf()], [Buf(), Buf()], [Buf(), Buf()]
            NPT = 4
            PT = [self.sb(es, "PT%d" % i, [128, 512], BF16) for i in range(NPT)]
            ptb = [Buf() for _ in range(NPT)]
            rS = self.sb(es, "rS", [128, 512], F32)
            tO = self.sb(es, "tO", [128, 512], F32)
            b_rS, b_tO = Buf(), Buf()
            mask = self.sb(es, "mask", [128, 3, 128], F32)
            b_mask = Buf()
            P.dma("sp", mask[:], io["nb_mask"].rearrange("s k q -> k s q"), writes=[b_mask])
            if fox:
                for i in range(2):
                    P.op("pool", lambda e, i=i: e.memset(KT[i][64:67, :], 1.0), writes=[ktb[i]])
                    oth = 1 - i
                    P.op("pool", lambda e, i=i, oth=oth: e.memset(Vl[i][:, :, oth * 64:(oth + 1) * 64], 0.0), writes=[vlb[i]])
                NBt = [mask, mask]
                nbb = [b_mask, b_mask]
            else:
                NBt = [self.sb(es, "NBt%d" % i, [128, 3, 128], F32) for i in range(2)]
                nbb = [Buf(), Buf()]
                Kd = 128
                for i in range(2):
                    P.op("pool", lambda e, i=i: e.memset(KT[i][64:128, :], 0.0), writes=[ktb[i]])
                    P.op("pool", lambda e, i=i: e.memset(QT[i][64:128, :], 0.0), writes=[qtb[i]])
                B31 = self.sb(es, "B31", [128, 16], F32)
                lam = self.sb(es, "lam", [128, 256], F32)
                lpr = self.sb(es, "lpr", [128, 128], F32)
                lsum = self.sb(es, "lsum", [128, 2], F32)
                nlam = self.sb(es, "nlam", [128, 1], F32)
                gsub = self.sb(es, "gsub", [128, 1], F32)
                dA = self.sb(es, "dA", [128, TO], F32)
                dd = self.sb(es, "dd", [128, 512], F32)
                sq = self.sb(es, "sq", [128, 512], BF16)
                rstd = self.sb(es, "rstd", [128, 512], F32)
                b_B31, b_lam, b_nlam, b_gsub, b_dd, b_sq, b_rstd = (Buf() for _ in range(7))
                P.dma("sp", B31[:], io["b31"].partition_broadcast(128), writes=[b_B31])
                P.dma("sp", lam[:], io["lam"].partition_broadcast(128), writes=[b_lam])
                P.dma("sp", gsub[:], io["subln"], writes=[b_gsub])
                P.op("dve", lambda e: e.tensor_scalar(gsub[:], gsub[:], 1.0 - lam_init, None, ALU.mult), reads=[b_gsub], writes=[b_gsub])
                lamv = lam[:, :].rearrange("p (a two d) -> p a two d", two=2, d=64)
                lprv = lpr[:, :].rearrange("p (a d) -> p a d", d=64)
                P.op("dve", lambda e: e.tensor_tensor(lprv, lamv[:, :, 0, :], lamv[:, :, 1, :], ALU.mult), reads=[b_lam], writes=[b_lam])
                P.op("dve", lambda e: e.reduce_sum(lsum[:, 0:1], lpr[:, 0:64], AX.X), reads=[b_lam], writes=[b_nlam])
                P.op("dve", lambda e: e.reduce_sum(lsum[:, 1:2], lpr[:, 64:128], AX.X), reads=[b_lam], writes=[b_nlam])
                P.op("act", lambda e: e.activation(out=lsum[:, :], in_=lsum[:, :], func=AF.Exp), reads=[b_nlam], writes=[b_nlam])
                P.op("dve", lambda e: e.scalar_tensor_tensor(nlam[:, 0:1], lsum[:, 1:2], -lam_init, lsum[:, 0:1], ALU.add, ALU.subtract),
                     reads=[b_nlam], writes=[b_nlam])
                dAb = {}

            def load_map(m):
                s = m % 2
                if not fox:
                    P.dma("sp", NBt[s][:], io["nb_T"][:, m].rearrange("s k q -> k s q"), writes=[nbb[s]])
                P.dma("sp", KT[s][0:64, :], KT_d[m * 64:(m + 1) * 64, :], writes=[ktb[s]])
                P.dma("sp", QT[s][0:64, :], QT_d[m * 64:(m + 1) * 64, :], writes=[qtb[s]])
                if fox:
                    P.dma("sp", QT[s][64:67, :], io["CQ3_d"][m, :, :], writes=[qtb[s]])
                    P.dma("sp", Vl[s][:, :, s * 64:(s + 1) * 64],
                          V_d.rearrange("(b p) n -> p b n", p=128)[:, :, m * 64:(m + 1) * 64], writes=[vlb[s]])
                else:
                    P.dma("sp", Vl[s][:, :, :], V_d.rearrange("(b p) n -> p b n", p=128)[:, :, (m // 2) * 128:(m // 2 + 1) * 128], writes=[vlb[s]])

            def fix_map(m):
                if not fox:
                    s = m % 2
                    P.op("dve", lambda e, s=s, m=m: e.scalar_tensor_tensor(NBt[s][:], NBt[s][:], B31[:, m:m + 1], mask[:],
                                                                            ALU.subtract, ALU.add),
                         reads=[nbb[s], b_B31, b_mask], writes=[nbb[s]])

            load_map(0)
            fix_map(0)
            dq = []
            itc = [0]
            last_pe = [None]

            def run_due():
                while dq and dq[0][0] <= itc[0]:
                    dq.pop(0)[1]()
            NG = (NJ + 3) // 4
            LA = 2
            SB = [0, 1, 2]
            OS = [(3, 4), (5, 6)]
            gcount = 0
            for m in range(16):
                s = m % 2
                if m + 1 < 16:
                    load_map(m + 1)
                c = m // 2
                half = m % 2
                for g in range(NG):
                    j0 = 4 * g
                    j1 = min(j0 + 4, NJ)
                    nq = (j1 - j0) * 128
                    nkb = 2 * j1
                    bo, bs_ = OS[gcount % 2]
                    gcount += 1
                    info = {}
                    tail0 = max(2 * j0 - 1, 0)
                    bulk = list(range(0, tail0))
                    tail = list(range(tail0, nkb))
                    if len(bulk) > 1 and not fox:
                        head = bulk[:max(1, len(bulk) - len(tail))]
                        rest = bulk[len(head):]
                        order = list(head)
                        for a in range(max(len(rest), len(tail))):
                            if a < len(rest):
                                order.append(rest[a])
                            if a < len(tail):
                                order.append(tail[a])
                    else:
                        order = bulk + tail
                    assert sorted(order) == list(range(nkb)) and order[0] == 0
                    for i in range(nkb + LA):
                        run_due()
                        itc[0] += 1
                        if i < nkb:
                            kb = order[i]
                            jmin = max(j0, kb // 2)
                            qoff = (jmin - j0) * 128
                            sbk = SB[i % 3]
                            pt = i % NPT
                            info[i] = (qoff, pt)
                            P.op("pe", lambda e, kb=kb, qoff=qoff, sbk=sbk, s=s: e.matmul(
                                ps[sbk][:, qoff:nq], KT[s][0:Kd, kb * 128:(kb + 1) * 128],
                                QT[s][0:Kd, j0 * 128 + qoff:j0 * 128 + nq], start=True, stop=True),
                                reads=[ktb[s], qtb[s]], writes=[psb[sbk]])
                            for j in range(jmin, j1):
                                slot = kb - (2 * j - 1)
                                if slot < 0 or slot > 2:
                                    continue
                                if fox and slot == 0:
                                    continue
                                cq = (j - j0) * 128
                                P.op("dve", lambda e, sbk=sbk, cq=cq, slot=slot, s=s: e.tensor_tensor(
                                    ps[sbk][:, cq:cq + 128], ps[sbk][:, cq:cq + 128], NBt[s][:, slot, :], ALU.add),
                                    reads=[psb[sbk], nbb[s]], writes=[psb[sbk]])
                            if fox:
                                P.op("act", lambda e, sbk=sbk, qoff=qoff, pt=pt, kb=kb, m=m: e.activation(
                                    out=PT[pt][:, qoff:nq], in_=ps[sbk][:, qoff:nq], func=AF.Exp,
                                    bias=NCK[:, kb * 16 + m:kb * 16 + m + 1], scale=1.0),
                                    reads=[psb[sbk]], writes=[ptb[pt]])
                            else:
                                P.op("act", lambda e, sbk=sbk, qoff=qoff, pt=pt: e.activation(
                                    out=PT[pt][:, qoff:nq], in_=ps[sbk][:, qoff:nq], func=AF.Exp),
                                    reads=[psb[sbk]], writes=[ptb[pt]])
                        if i >= LA:
                            ii = i - LA
                            kb = order[ii]
                            qoff, pt = info.pop(ii)
                            P.op("pe", lambda e, kb=kb, ii=ii, qoff=qoff, pt=pt, s=s, bo=bo: e.matmul(
                                ps[bo][:, qoff:nq], Vl[s][:, kb, :], PT[pt][:, qoff:nq], start=(ii == 0), stop=(ii == nkb - 1)),
                                reads=[vlb[s], ptb[pt]], writes=[psb[bo]], inc=False)
                            last_pe[0] = P.op("pe", lambda e, ii=ii, qoff=qoff, pt=pt, bs_=bs_: e.matmul(
                                ps[bs_][:, qoff:nq], ones_bf[:, :], PT[pt][:, qoff:nq], start=(ii == 0), stop=(ii == nkb - 1)),
                                reads=[ptb[pt]], writes=[psb[bs_]], inc=True)
                    q0 = j0 * 128

                    def stA(g=g, c=c, half=half, q0=q0, nq=nq, bo=bo, bs_=bs_):
                        if fox:
                            r0, r1 = half * 64, (half + 1) * 64
                            P.op("dve", lambda e: e.reciprocal(rS[r0:r1, 0:nq], ps[bs_][r0:r1, 0:nq]),
                                 reads=[psb[bs_]], writes=[b_rS])
                            P.op("dve", lambda e: e.tensor_tensor(
                                XT[r0:r1, c, q0:q0 + nq], ps[bo][r0:r1, 0:nq], rS[r0:r1, 0:nq], ALU.mult),
                                reads=[psb[bo], b_rS], writes=[])
                            return
                        P.op("act", lambda e: e.activation(out=rS[:, 0:nq], in_=ps[bs_][:, 0:nq], func=AF.Ln), reads=[psb[bs_]], writes=[b_rS])
                        P.op("act", lambda e: e.activation(out=rS[:, 0:nq], in_=rS[:, 0:nq], func=AF.Exp, scale=-1.0), reads=[b_rS], writes=[b_rS])

                    def stB(g=g, c=c, half=half, q0=q0, nq=nq, bo=bo, bs_=bs_):
                        if half == 0:
                            dAb[g] = Buf()
                            P.op("dve", lambda e: e.tensor_tensor(dA[:, q0:q0 + nq], ps[bo][:, 0:nq], rS[:, 0:nq], ALU.mult),
                                 reads=[psb[bo], b_rS], writes=[dAb[g]])
                        else:
                            P.op("dve", lambda e: e.tensor_tensor(tO[:, 0:nq], ps[bo][:, 0:nq], rS[:, 0:nq], ALU.mult),
                                 reads=[psb[bo], b_rS], writes=[b_tO])
                            P.op("dve", lambda e: e.scalar_tensor_tensor(dd[:, 0:nq], tO[:, 0:nq], nlam[:, 0:1], dA[:, q0:q0 + nq],
                                                                         ALU.mult, ALU.add),
                                 reads=[b_tO, b_nlam, dAb[g]], writes=[b_dd])

                    def stC1(nq=nq):
                        P.op("act", lambda e: e.activation(out=sq[:, 0:nq], in_=dd[:, 0:nq], func=AF.Square), reads=[b_dd], writes=[b_sq])

                    def stC2(nq=nq):
                        P.op("pe", lambda e: e.matmul(ps[7][:, 0:nq], ones_bf[:, :], sq[:, 0:nq], start=True, stop=True),
                             reads=[b_sq], writes=[psb[7]])
                        P.op("dve", lambda e: e.tensor_scalar(rstd[:, 0:nq], ps[7][:, 0:nq], 1.0 / 128.0, LN_EPS, ALU.mult, ALU.add),
                             reads=[psb[7]], writes=[b_rstd])

                    def stC3(nq=nq):
                        P.op("act", lambda e: e.activation(out=rstd[:, 0:nq], in_=rstd[:, 0:nq], func=AF.Ln), reads=[b_rstd], writes=[b_rstd])
                        P.op("act", lambda e: e.activation(out=rstd[:, 0:nq], in_=rstd[:, 0:nq], func=AF.Exp, scale=-0.5), reads=[b_rstd], writes=[b_rstd])

                    def stC4(c=c, q0=q0, nq=nq):
                        P.op("dve", lambda e: e.scalar_tensor_tensor(XT[:, c, q0:q0 + nq], dd[:, 0:nq], gsub[:, 0:1], rstd[:, 0:nq],
                                                                     ALU.mult, ALU.mult),
                             reads=[b_dd, b_gsub, b_rstd], writes=[])

                    T = itc[0]
                    dq.append((T + 3, stA))
                    if not fox:
                        dq.append((T + 5, stB))
                        if half == 1:
                            dq.append((T + 8, stC1))
                            dq.append((T + 10, stC2))
                            dq.append((T + 12, stC3))
                            dq.append((T + 14, stC4))
                    if m == 1 and g == 0:
                        gate = Buf()
                        gate.w = last_pe[0]
                        first = True
                        for (dst, src) in getattr(self, "pending_conv", []):
                            P.dma("pool", dst, src, reads=([gate] if first else []), bulk=True)
                            first = False
                        self.pending_conv = []
                if m + 1 < 16:
                    fix_map(m + 1)
            for _, f_ in dq:
                f_()

    def _layernorm(self, es, tag):
        P = self.P
        st = self.sb(es, "st" + tag, [128, 2, 6], F32)
        mv = self.sb(es, "mv" + tag, [128, 2], F32)
        rs = self.sb(es, "rs" + tag, [128, 1], F32)
        b_st, b_mv, b_rs = Buf(), Buf(), Buf()

        def ln(Z, b_Z, out, b_out, Gt, Bt, b_gb):
            for h in range(2):
                P.op("dve", lambda e, h=h: e.bn_stats(st[:, h, :], Z[:, h * 512:(h + 1) * 512]), reads=[b_Z], writes=[b_st])
            P.op("dve", lambda e: e.bn_aggr(mv[:, :], st[:, :, :].rearrange("p a b -> p (a b)")), reads=[b_st], writes=[b_mv])
            P.op("dve", lambda e: e.tensor_scalar(rs[:, :], mv[:, 1:2], LN_EPS, None, ALU.add), reads=[b_mv], writes=[b_rs])
            P.op("act", lambda e: e.activation(out=rs[:, :], in_=rs[:, :], func=AF.Ln), reads=[b_rs], writes=[b_rs])
            P.op("act", lambda e: e.activation(out=rs[:, :], in_=rs[:, :], func=AF.Exp, scale=-0.5), reads=[b_rs], writes=[b_rs])
            P.op("dve", lambda e: e.tensor_scalar(Z[:, :], Z[:, :], mv[:, 0:1], rs[:, 0:1], ALU.subtract, ALU.mult),
                 reads=[b_Z, b_mv, b_rs], writes=[b_Z])
            P.op("dve", lambda e: e.tensor_tensor(Z[:, :], Z[:, :], Gt[:, :], ALU.mult), reads=[b_Z, b_gb], writes=[b_Z])
            P.op("dve", lambda e: e.tensor_tensor(out, Z[:, :], Bt[:, :], ALU.add), reads=[b_Z, b_gb], writes=[b_out])
        return ln

    def _post_attention(self, li, io, XT, ident, GATES):
        nc, P = self.nc, self.P
        NJ, TO = self.NJ, self.TO
        ps, psb = self.ps, self.psb
        with ExitStack() as es:
            Wo = self.sb(es, "Wo", [128, DC, 1024], BF16)
            b_Wo = Buf()
            wo_v = io["w_o"].rearrange("(dc p) n -> p dc n", p=128)
            for h in range(2):
                P.dma("pool", Wo[:, :, h * 512:(h + 1) * 512], wo_v[:, :, h * 512:(h + 1) * 512], writes=[b_Wo])
            Gt = self.sb(es, "Gt", [128, 1024], F32)
            Bt = self.sb(es, "Bt", [128, 1024], F32)
            b_gb = Buf()
            P.dma("sp", Gt[:], io["lnm_g"].partition_broadcast(128), writes=[b_gb])
            P.dma("sp", Bt[:], io["lnm_b"].partition_broadcast(128), writes=[b_gb])
            Wr = self.sb(es, "Wr", [128, DC, 36], F32)
            Br = self.sb(es, "Br", [128, 36], F32)
            b_Wr = Buf()
            P.dma("sp", Wr[:], io["w_r"].rearrange("(dc p) n -> p dc n", p=128), writes=[b_Wr])
            P.dma("sp", Br[:], io["b_r"].partition_broadcast(128), writes=[b_Wr])
            NR = 3
            Hres = [self.sb(es, "Hres%d" % i, [128, 1024], F32) for i in range(NR)]
            hrb = [Buf() for _ in range(NR)]
            Z = self.sb(es, "Z", [128, 1024], F32)
            b_Z = Buf()
            H1 = [self.sb(es, "H1_%d" % i, [128, 1024], F32) for i in range(2)]
            h1b = [Buf(), Buf()]
            HTf = self.sb(es, "HTf", [128, DC, 128], F32)
            b_HTf = Buf()
            Lg = self.sb(es, "Lg", [128, 36], F32)
            EM = self.sb(es, "EM", [128, 32], F32)
            sm = self.sb(es, "sm", [128, 16], F32)
            oh = self.sb(es, "oh", [128, 4], F32)
            pen = self.sb(es, "pen", [128, 4], F32)
            ge = self.sb(es, "ge", [128, 4], F32)
            M8 = self.sb(es, "M8", [128, 8], F32)
            e2 = self.sb(es, "e2", [128, 2], F32)
            sel = self.sb(es, "sel", [128, 32], F32)
            ew = self.sb(es, "ew", [128, 32], F32)
            b_r = Buf()
            ln = self._layernorm(es, "m")
            h1T_dv = io["H1T_d"].rearrange("(dc p) t -> p dc t", p=128)

            res_mode = io.get("res_mode", "static")
            if res_mode == "dyn":
                Hp = [self.sb(es, "Hp%d" % i, [128, 2, 1024], F32) for i in range(NR)]
                hpb = [Buf() for _ in range(NR)]
                par = self.sb(es, "par", [128, 2], F32)
                b_par = Buf()
                P.dma("sp", par[:], io["par"], writes=[b_par])

            def load_res(j, sl):
                if res_mode == "static":
                    P.dma("sp", Hres[sl][:], io["x_own"][j * 128:(j + 1) * 128, :], writes=[hrb[sl]])
                else:
                    P.dma("sp", Hp[sl][:], io["h2_d"][2 * j * 128:(2 * j + 2) * 128, :].rearrange("(two p) n -> p two n", p=128),
                          writes=[hpb[sl]])

            def select_res(sl):
                if res_mode != "static":
                    P.op("dve", lambda e, sl=sl: e.tensor_scalar(Hres[sl][:, :], Hp[sl][:, 1, :], par[:, 1:2], None, ALU.mult),
                         reads=[hpb[sl], b_par], writes=[hrb[sl]])
                    P.op("dve", lambda e, sl=sl: e.scalar_tensor_tensor(Hres[sl][:, :], Hp[sl][:, 0, :], par[:, 0:1], Hres[sl][:, :],
                                                                         ALU.mult, ALU.add),
                         reads=[hpb[sl], b_par, hrb[sl]], writes=[hrb[sl]])

            wo_banks = {}

            def emit_wo(j):
                t0 = j * 128
                bks = []
                for h in range(2):
                    bk = self.next_bank()
                    for dc in range(DC):
                        P.op("pe", lambda e, dc=dc, h=h, bk=bk: e.matmul(
                            ps[bk][:, :], XT[:, dc, t0:t0 + 128], Wo[:, dc, h * 512:(h + 1) * 512],
                            start=(dc == 0), stop=(dc == DC - 1)),
                            reads=[b_Wo], writes=[psb[bk]], inc=(dc == DC - 1))
                    bks.append(bk)
                wo_banks[j] = bks

            def emit_ln(j):
                s = j % 2
                sl = j % NR
                t0 = j * 128
                select_res(sl)
                for h, bk in enumerate(wo_banks.pop(j)):
                    P.op("dve", lambda e, h=h, bk=bk, sl=sl: e.scalar_tensor_tensor(
                        Z[:, h * 512:(h + 1) * 512], Hres[sl][:, h * 512:(h + 1) * 512], ALPHA, ps[bk][:, :], ALU.mult, ALU.add),
                        reads=[hrb[sl], psb[bk]], writes=[b_Z])
                ln(Z, b_Z, H1[s][:, :], h1b[s], Gt, Bt, b_gb)
                P.dma("pool", io["h1_d"][t0:t0 + 128, :], H1[s][:, :], reads=[h1b[s]])

            def emit_tr(j):
                s = j % 2
                for q in range(2):
                    bk = self.next_bank()
                    for k in range(4):
                        dc = q * 4 + k
                        P.op("pe", lambda e, dc=dc, k=k, bk=bk, s=s: e.transpose(
                            ps[bk][:, k * 128:(k + 1) * 128], H1[s][:, dc * 128:(dc + 1) * 128], ident[:, :]),
                            reads=[h1b[s]], writes=[psb[bk]], inc=(k == 3))
                    P.op("act", lambda e, q=q, bk=bk: e.activation(
                        out=HTf[:, q * 4:(q + 1) * 4, :], in_=ps[bk][:, :].rearrange("p (a b) -> p a b", b=128), func=AF.Copy),
                        reads=[psb[bk]], writes=[b_HTf])
                bk = self.next_bank()
                for dc in range(DC):
                    P.op("pe", lambda e, dc=dc, bk=bk: e.matmul(ps[bk][:, 0:36], HTf[:, dc, :], Wr[:, dc, :],
                                                                 start=(dc == 0), stop=(dc == DC - 1)),
                         reads=[b_HTf, b_Wr], writes=[psb[bk]], inc=(dc == DC - 1))
                return bk

            def emit_chain(j, bk):
                R = [b_r]
                P.op("dve", lambda e, bk=bk: e.tensor_tensor(Lg[:, :], ps[bk][:, 0:36], Br[:, :], ALU.add), reads=[psb[bk], b_Wr], writes=R)
                P.op("dve", lambda e: e.reduce_max(sm[:, 0:1], Lg[:, 0:4], AX.X), reads=R, writes=R)
                P.op("dve", lambda e: e.tensor_scalar(oh[:, :], Lg[:, 0:4], sm[:, 0:1], None, ALU.is_ge), reads=R, writes=R)
                P.op("dve", lambda e: e.tensor_scalar(sm[:, 1:2], sm[:, 0:1], -1.0, None, ALU.mult), reads=R, writes=R)
                P.op("act", lambda e: e.activation(out=ge[:, :], in_=Lg[:, 0:4], func=AF.Exp, bias=sm[:, 1:2], scale=1.0), reads=R, writes=R)
                P.op("dve", lambda e: e.reduce_sum(sm[:, 2:3], ge[:, :], AX.X), reads=R, writes=R)
                P.op("dve", lambda e: e.tensor_scalar(pen[:, :], oh[:, :], 1.0, 1e30, ALU.subtract, ALU.mult), reads=R, writes=R)
                for g in range(4):
                    P.op("dve", lambda e, g=g: e.tensor_scalar(EM[:, g * 8:(g + 1) * 8], Lg[:, 4 + g * 8:4 + (g + 1) * 8],
                                                                 pen[:, g:g + 1], None, ALU.add), reads=R, writes=R)
                P.op("dve", lambda e: e.max(out=M8[:, :], in_=EM[:, :]), reads=R, writes=R)
                P.op("dve", lambda e: e.tensor_scalar(sel[:, :], EM[:, :], M8[:, 1:2], None, ALU.is_ge), reads=R, writes=R)
                P.op("dve", lambda e: e.tensor_scalar(sm[:, 3:4], M8[:, 0:1], -1.0, None, ALU.mult), reads=R, writes=R)
                P.op("act", lambda e: e.activation(out=ew[:, :], in_=EM[:, :], func=AF.Exp, bias=sm[:, 3:4], scale=1.0), reads=R, writes=R)
                P.op("act", lambda e: e.activation(out=e2[:, :], in_=M8[:, 0:2], func=AF.Exp, bias=sm[:, 3:4], scale=1.0), reads=R, writes=R)
                P.op("dve", lambda e: e.reduce_sum(sm[:, 4:5], e2[:, :], AX.X), reads=R, writes=R)
                P.op("dve", lambda e: e.tensor_tensor(sm[:, 5:6], sm[:, 4:5], sm[:, 2:3], ALU.mult), reads=R, writes=R)
                P.op("dve", lambda e: e.reciprocal(sm[:, 6:7], sm[:, 5:6]), reads=R, writes=R)
                SELt, SEL1t, W12t = GATES
                P.op("dve", lambda e, j=j: e.tensor_copy(out=SELt[:, j, :], in_=sel[:, :]), reads=R, writes=R)
                P.op("dve", lambda e, j=j: e.tensor_scalar(SEL1t[:, j, :], EM[:, :], M8[:, 0:1], None, ALU.is_ge), reads=R, writes=R)
                P.op("dve", lambda e, j=j: e.tensor_copy(out=W12t[:, j, 0:1], in_=sm[:, 6:7]), reads=R, writes=R)
                P.op("dve", lambda e, j=j: e.tensor_tensor(W12t[:, j, 1:2], sm[:, 6:7], e2[:, 1:2], ALU.mult), reads=R, writes=R)

            load_res(0, 0)
            if NJ > 1:
                load_res(1, 1)
            emit_wo(0)
            emit_ln(0)
            for j in range(NJ):
                if j + 2 < NJ:
                    load_res(j + 2, (j + 2) % NR)
                if j + 1 < NJ:
                    emit_wo(j + 1)
                rbk = emit_tr(j)
                if j + 1 < NJ:
                    emit_ln(j + 1)
                emit_chain(j, rbk)

    def _moe(self, li, io, GATES):
        nc, P = self.nc, self.P
        NJ, TO = self.NJ, self.TO
        ps, psb = self.ps, self.psb
        npass = (NJ + 10) // 11
        base = NJ // npass
        sizes = [base + (1 if i < NJ % npass else 0) for i in range(npass)]
        PBmax = max(sizes)
        with ExitStack() as es:
            XTs = self.sb(es, "XTs", [128, DC, PBmax * 128], BF16)
            b_X = Buf()
            ACC = self.sb(es, "ACC", [128, PBmax, 1024], F32)
            accb = [Buf() for _ in range(PBmax)]
            Wg = [self.sb(es, "Wg%d" % i, [128, DC, DEXP], BF16) for i in range(2)]
            Wu = [self.sb(es, "Wu%d" % i, [128, DC, DEXP], BF16) for i in range(2)]
            Wd = [self.sb(es, "Wd%d" % i, [128, 4, 1024], BF16) for i in range(2)]
            wgb, wub, wdb = [Buf(), Buf()], [Buf(), Buf()], [Buf(), Buf()]
            AT = [self.sb(es, "AT%d" % i, [128, 4, 512], BF16) for i in range(2)]
            atb = [Buf(), Buf()]
            SG = [self.sb(es, "SG%d" % i, [128, 512], F32) for i in range(2)]
            sgb = [Buf(), Buf()]
            Gt = self.sb(es, "Gt2", [128, 1024], F32)
            Bt = self.sb(es, "Bt2", [128, 1024], F32)
            b_gb = Buf()
            P.dma("sp", Gt[:], io["lnf_g"].partition_broadcast(128), writes=[b_gb])
            P.dma("sp", Bt[:], io["lnf_b"].partition_broadcast(128), writes=[b_gb])
            Hres = [self.sb(es, "Hr2_%d" % i, [128, 1024], F32) for i in range(2)]
            hrb = [Buf(), Buf()]
            Z = self.sb(es, "Z2", [128, 1024], F32)
            b_Z = Buf()
            H2 = [self.sb(es, "H2_%d" % i, [128, 1024], F32) for i in range(2)]
            h2b = [Buf(), Buf()]
            ln = self._layernorm(es, "f")
            if io.get("sink", "final") != "final":
                HTo = [self.sb(es, "HTo%d" % i, [128, DC, 128], BF16) for i in range(2)]
                htob = [Buf(), Buf()]
                hT2_dv = io["hT2_d"].rearrange("(dc p) t -> p dc t", p=128)
            h1T_dv = io["H1T_d"].rearrange("(dc p) t -> p dc t", p=128)
            wg_v = io["w_gate"].rearrange("e (dc p) f -> e p dc f", p=128)
            wu_v = io["w_up"].rearrange("e (dc p) f -> e p dc f", p=128)
            wd_v = io["w_down"].rearrange("e (fc p) n -> e p fc n", p=128)

            def load_w(e):
                s = e % 2
                P.dma("pool", Wg[s][:], wg_v[e], writes=[wgb[s]])
                P.dma("pool", Wu[s][:], wu_v[e], writes=[wub[s]])
                P.dma("pool", Wd[s][:], wd_v[e], writes=[wdb[s]])

            tilec = 0
            fcc = 0
            jb = 0
            for pi in range(npass):
                PB = sizes[pi]
                ntok = PB * 128
                tok0 = jb * 128
                P.dma("sp", XTs[:, :, 0:ntok], h1T_dv[:, :, tok0:tok0 + ntok], writes=[b_X])
                load_w(0)
                for e in range(NEXP):
                    s = e % 2
                    if e + 1 < NEXP:
                        load_w(e + 1)
                    for off in range(0, ntok, 512):
                        n = min(512, ntok - off)
                        ta = tilec % 2
                        tilec += 1
                        for fc in range(4):
                            bg = [0, 1][fcc % 2]
                            bu = [2, 3][fcc % 2]
                            fcc += 1
                            for dc in range(DC):
                                P.op("pe", lambda e_, dc=dc, fc=fc, s=s, bg=bg, off=off, n=n: e_.matmul(
                                    ps[bg][:, 0:n], Wg[s][:, dc, fc * 128:(fc + 1) * 128], XTs[:, dc, off:off + n],
                                    start=(dc == 0), stop=(dc == DC - 1)),
                                    reads=[wgb[s], b_X], writes=[psb[bg]], inc=(dc == DC - 1))
                            for dc in range(DC):
                                P.op("pe", lambda e_, dc=dc, fc=fc, s=s, bu=bu, off=off, n=n: e_.matmul(
                                    ps[bu][:, 0:n], Wu[s][:, dc, fc * 128:(fc + 1) * 128], XTs[:, dc, off:off + n],
                                    start=(dc == 0), stop=(dc == DC - 1)),
                                    reads=[wub[s], b_X], writes=[psb[bu]], inc=(dc == DC - 1))
                            sg = fcc % 2
                            P.op("act", lambda e_, bg=bg, sg=sg, n=n: e_.activation(out=SG[sg][:, 0:n], in_=ps[bg][:, 0:n], func=AF.Silu),
                                 reads=[psb[bg]], writes=[sgb[sg]])
                            P.op("dve", lambda e_, bu=bu, sg=sg, ta=ta, fc=fc, n=n: e_.tensor_tensor(
                                AT[ta][:, fc, 0:n], SG[sg][:, 0:n], ps[bu][:, 0:n], ALU.mult),
                                reads=[sgb[sg], psb[bu]], writes=[atb[ta]])
                        for b in range(n // 128):
                            jj = (off // 128) + b
                            for h in range(2):
                                by = [4, 5, 6, 7][(b * 2 + h) % 4]
                                for fc in range(4):
                                    P.op("pe", lambda e_, fc=fc, b=b, h=h, by=by, ta=ta, s=s: e_.matmul(
                                        ps[by][:, :], AT[ta][:, fc, b * 128:(b + 1) * 128], Wd[s][:, fc, h * 512:(h + 1) * 512],
                                        start=(fc == 0), stop=(fc == 3)),
                                        reads=[atb[ta], wdb[s]], writes=[psb[by]], inc=(fc == 3))
                                gcol = GATES[:, jb + jj, e:e + 1]
                                if e == 0:
                                    P.op("dve", lambda e_, by=by, jj=jj, h=h, gcol=gcol: e_.tensor_scalar(
                                        ACC[:, jj, h * 512:(h + 1) * 512], ps[by][:, :], gcol, None, ALU.mult),
                                        reads=[psb[by]], writes=[accb[jj]])
                                else:
                                    P.op("dve", lambda e_, by=by, jj=jj, h=h, gcol=gcol: e_.scalar_tensor_tensor(
                                        ACC[:, jj, h * 512:(h + 1) * 512], ps[by][:, :], gcol, ACC[:, jj, h * 512:(h + 1) * 512],
                                        ALU.mult, ALU.add),
                                        reads=[psb[by], accb[jj]], writes=[accb[jj]])
                P.dma("sp", Hres[0][:], io["h1_d"][tok0:tok0 + 128, :], writes=[hrb[0]])
                for jj in range(PB):
                    s = jj % 2
                    j = jb + jj
                    if jj + 1 < PB:
                        P.dma("sp", Hres[1 - s][:], io["h1_d"][(j + 1) * 128:(j + 2) * 128, :], writes=[hrb[1 - s]])
                    P.op("dve", lambda e_, jj=jj, s=s: e_.scalar_tensor_tensor(
                        Z[:, :], Hres[s][:, :], ALPHA, ACC[:, jj, :], ALU.mult, ALU.add),
                        reads=[hrb[s], accb[jj]], writes=[b_Z])
                    ln(Z, b_Z, H2[s][:, :], h2b[s], Gt, Bt, b_gb)
                    sink = io.get("sink", "final")
                    if sink == "final":
                        P.dma("sp", io["h_out"][j * 128:(j + 1) * 128, :], H2[s][:, :], reads=[h2b[s]])
                    else:
                        gb = 2 * j + sink
                        P.dma("sp", io["h2_d"][gb * 128:(gb + 1) * 128, :], H2[s][:, :], reads=[h2b[s]])
                        for q in range(2):
                            bk = self.next_bank()
                            for k in range(4):
                                dc = q * 4 + k
                                P.op("pe", lambda e_, dc=dc, k=k, bk=bk, s=s: e_.transpose(
                                    ps[bk][:, k * 128:(k + 1) * 128], H2[s][:, dc * 128:(dc + 1) * 128], self.ident[:, :]),
                                    reads=[h2b[s]], writes=[psb[bk]], inc=(k == 3))
                            self.copy(self.ev_eng(), HTo[s][:, q * 4:(q + 1) * 4, :],
                                      ps[bk][:, :].rearrange("p (a b) -> p a b", b=128), [psb[bk]], [htob[s]])
                        P.dma("sp", hT2_dv[:, :, gb * 128:(gb + 1) * 128], HTo[s][:, :, :], reads=[htob[s]])
                jb += PB


    def queue_wconv(self, io):
        lst = getattr(self, "pending_conv", [])
        Wall_d = io["Wall_d"]
        for e in range(NEXP):
            rows = Wall_d[e * 128:(e + 1) * 128, :]
            lst.append((rows[:, 0:4096].rearrange("p (dc f) -> p dc f", f=512),
                        io["w_gate"][e].rearrange("(dc p) f -> p dc f", p=128)))
            lst.append((rows[:, 4096:8192].rearrange("p (dc f) -> p dc f", f=512),
                        io["w_up"][e].rearrange("(dc p) f -> p dc f", p=128)))
            lst.append((rows[:, 8192:12288].rearrange("p (fc n) -> p fc n", n=1024),
                        io["w_down"][e].rearrange("(fc p) n -> p fc n", p=128)))
        self.pending_conv = lst

    def _moe_routed(self, li, io, GATES):
        nc, P = self.nc, self.P
        NJ, TO = self.NJ, self.TO
        ps, psb = self.ps, self.psb
        SEL, SEL1, W12 = GATES
        TS = 256
        NT = (2 * TO + TS - 1) // TS + NEXP
        Xs_d, Ys_d, Wall_d = io["Xs_d"], io["Ys_d"], io["Wall_d"]
        ones_bf = self.ones_bf
        for (dst, src) in getattr(self, "pending_conv", []):
            P.dma("pool", dst, src, bulk=True)
        self.pending_conv = []
        with ExitStack() as es:
            LT = self.sb(es, "LT", [128, 128], BF16)
            iota = self.sb(es, "iota", [128, 1], F32)
            SELb = self.sb(es, "SELb", [128, NJ, NEXP], BF16)
            RSb = self.sb(es, "RSb", [128, NJ, NEXP], BF16)
            RANK = self.sb(es, "RANK", [128, NJ, NEXP], F32)
            TMP = self.sb(es, "TMPr", [128, NJ, NEXP], F32)
            SEL2 = self.sb(es, "SEL2", [128, NJ, NEXP], F32)
            TOT = self.sb(es, "TOT", [128, NEXP], F32)
            M1 = self.sb(es, "M1", [128, NEXP], F32)
            PADD = self.sb(es, "PADD", [128, NEXP], F32)
            END = self.sb(es, "END", [128, NEXP], F32)
            START = self.sb(es, "START", [128, NEXP], F32)
            ones32 = self.sb(es, "ones32", [128, NEXP], F32)
            cmp_ = self.sb(es, "cmp", [128, NEXP], F32)
            POSf = self.sb(es, "POSf", [128, 2, NJ], F32)
            POSi = self.sb(es, "POSi", [128, 2, NJ], mybir.dt.int32)
            EID = self.sb(es, "EID", [128, NT], F32)
            IDX = self.sb(es, "IDX", [128, NT], mybir.dt.int32)
            b_c = Buf()
            R = [b_c]
            P.dma("pool", LT[:], io["ltri"], writes=R)
            P.dma("sp", iota[:], io["iota_p"], writes=R)
            P.op("dve", lambda e: e.memset(ones32[:], 1.0), writes=R)
            P.op("dve", lambda e: e.tensor_copy(out=SELb[:], in_=SEL[:]), reads=R, writes=R)
            P.op("dve", lambda e: e.tensor_copy(out=RSb[:, 0, :], in_=SELb[:, 0, :]), reads=R, writes=R)
            for j in range(1, NJ):
                P.op("dve", lambda e, j=j: e.tensor_tensor(RSb[:, j, :], RSb[:, j - 1, :], SELb[:, j, :], ALU.add), reads=R, writes=R)
            for j in range(NJ):
                bk = self.next_bank()
                P.op("pe", lambda e, j=j, bk=bk: e.matmul(ps[bk][:, 0:NEXP], LT[:, :], SELb[:, j, :], start=True, stop=(j == 0)),
                     reads=R, writes=[psb[bk]], inc=(j == 0))
                if j > 0:
                    P.op("pe", lambda e, j=j, bk=bk: e.matmul(ps[bk][:, 0:NEXP], ones_bf[:, :], RSb[:, j - 1, :], start=False, stop=True),
                         reads=R, writes=[psb[bk]])
                P.op("act", lambda e, j=j, bk=bk: e.activation(out=RANK[:, j, :], in_=ps[bk][:, 0:NEXP], func=AF.Copy),
                     reads=[psb[bk]] + R, writes=R)
            bk = self.next_bank()
            P.op("pe", lambda e, bk=bk: e.matmul(ps[bk][:, 0:NEXP], ones_bf[:, :], RSb[:, NJ - 1, :], start=True, stop=True),
                 reads=R, writes=[psb[bk]])
            P.op("act", lambda e, bk=bk: e.activation(out=TOT[:, :], in_=ps[bk][:, 0:NEXP], func=AF.Copy), reads=[psb[bk]] + R, writes=R)
            P.op("dve", lambda e: e.memset(M1[:, :], 0.0), reads=R, writes=R)
            for m in range((2 * TO + TS - 1) // TS + 1):
                P.op("dve", lambda e, m=m: e.scalar_tensor_tensor(M1[:, :], TOT[:, :], float(m * TS), M1[:, :], ALU.is_gt, ALU.add),
                     reads=R, writes=R)
            P.op("dve", lambda e: e.tensor_scalar(PADD[:, :], M1[:, :], float(TS), None, ALU.mult), reads=R, writes=R)
            P.op("dve", lambda e: e.tensor_tensor_scan(END[:, :], ones32[:, :], PADD[:, :], 0.0, ALU.mult, ALU.add), reads=R, writes=R)
            P.op("dve", lambda e: e.tensor_tensor(START[:, :], END[:, :], PADD[:, :], ALU.subtract), reads=R, writes=R)
            for j in range(NJ):
                P.op("dve", lambda e, j=j: e.tensor_tensor(RANK[:, j, :], RANK[:, j, :], START[:, :], ALU.add), reads=R, writes=R)
            P.op("dve", lambda e: e.tensor_tensor(TMP[:], SEL1[:], RANK[:], ALU.mult), reads=R, writes=R)
            P.op("dve", lambda e: e.reduce_sum(POSf[:, 0, :], TMP[:], AX.X), reads=R, writes=R)
            P.op("dve", lambda e: e.tensor_tensor(SEL2[:], SEL[:], SEL1[:], ALU.subtract), reads=R, writes=R)
            P.op("dve", lambda e: e.tensor_tensor(TMP[:], SEL2[:], RANK[:], ALU.mult), reads=R, writes=R)
            P.op("dve", lambda e: e.reduce_sum(POSf[:, 1, :], TMP[:], AX.X), reads=R, writes=R)
            P.op("dve", lambda e: e.tensor_copy(out=POSi[:], in_=POSf[:]), reads=R, writes=R)
            for t in range(NT):
                P.op("dve", lambda e, t=t: e.tensor_scalar(cmp_[:, :], END[:, :], float(t * TS), 0.0, ALU.is_le, ALU.add,
                                                           accum_out=EID[:, t:t + 1]), reads=R, writes=R)
            P.op("dve", lambda e: e.tensor_scalar(EID[:, :], EID[:, :], float(NEXP - 1), 128.0, ALU.min, ALU.mult), reads=R, writes=R)
            P.op("dve", lambda e: e.tensor_scalar(EID[:, :], EID[:, :], iota[:, 0:1], None, ALU.add), reads=R, writes=R)
            P.op("dve", lambda e: e.tensor_copy(out=IDX[:], in_=EID[:]), reads=R, writes=R)

            Hb = [self.sb(es, "Hb%d" % i, [128, 1024], F32) for i in range(2)]
            hbb = [Buf(), Buf()]
            for j in range(NJ):
                s = j % 2
                P.dma("sp", Hb[s][:], io["h1_d"][j * 128:(j + 1) * 128, :], writes=[hbb[s]])
                for r in range(2):
                    P.idma(Xs_d[:, :], Hb[s][:, :], out_off=POSi[:, r, j:j + 1], reads=[hbb[s]] + R)
            P.barrier()

            Wall = [self.sb(es, "Wall%d" % i, [128, 12288], BF16) for i in range(2)]
            wlb = [Buf(), Buf()]
            Xsl = [self.sb(es, "Xsl%d" % i, [128, 2, 1024], F32) for i in range(2)]
            xslb = [Buf(), Buf()]
            XsT = [self.sb(es, "XsT%d" % i, [128, DC, TS], BF16) for i in range(2)]
            xstb = [Buf(), Buf()]
            AT = [self.sb(es, "ATr%d" % i, [128, 4, TS], BF16) for i in range(2)]
            atb = [Buf(), Buf()]
            SG = [self.sb(es, "SGr%d" % i, [128, TS], F32) for i in range(2)]
            sgb = [Buf(), Buf()]
            Yst = [self.sb(es, "Yst%d" % i, [128, 2, 1024], F32) for i in range(2)]
            ystb = [Buf(), Buf()]
            Xs_v = Xs_d.rearrange("(t s p) n -> t p s n", s=2, p=128)
            Ys_v = Ys_d.rearrange("(t s p) n -> t p s n", s=2, p=128)

            def load_tile(t):
                s = t % 2
                P.idma(Wall[s][:, :], Wall_d[:, :], in_off=IDX[:, t:t + 1], reads=R, writes=[wlb[s]])
                P.dma("sp", Xsl[s][:], Xs_v[t], writes=[xslb[s]])

            load_tile(0)
            fcc = 0
            for t in range(NT):
                s = t % 2
                if t + 1 < NT:
                    load_tile(t + 1)
                for sub in range(2):
                    for q in range(2):
                        bk = self.next_bank()
                        for k in range(4):
                            dc = q * 4 + k
                            P.op("pe", lambda e, dc=dc, k=k, bk=bk, s=s, sub=sub: e.transpose(
                                ps[bk][:, k * 128:(k + 1) * 128], Xsl[s][:, sub, dc * 128:(dc + 1) * 128], self.ident[:, :]),
                                reads=[xslb[s]], writes=[psb[bk]], inc=(k == 3))
                        self.copy(self.ev_eng(), XsT[s][:, q * 4:(q + 1) * 4, sub * 128:(sub + 1) * 128],
                                  ps[bk][:, :].rearrange("p (a b) -> p a b", b=128), [psb[bk]], [xstb[s]])
                for fc in range(4):
                    bg = self.next_bank()
                    for dc in range(DC):
                        P.op("pe", lambda e, dc=dc, fc=fc, s=s, bg=bg: e.matmul(
                            ps[bg][:, 0:TS], Wall[s][:, dc * 512 + fc * 128:dc * 512 + (fc + 1) * 128], XsT[s][:, dc, :],
                            start=(dc == 0), stop=(dc == DC - 1)),
                            reads=[wlb[s], xstb[s]], writes=[psb[bg]], inc=(dc == DC - 1))
                    bu = self.next_bank()
                    for dc in range(DC):
                        P.op("pe", lambda e, dc=dc, fc=fc, s=s, bu=bu: e.matmul(
                            ps[bu][:, 0:TS], Wall[s][:, 4096 + dc * 512 + fc * 128:4096 + dc * 512 + (fc + 1) * 128], XsT[s][:, dc, :],
                            start=(dc == 0), stop=(dc == DC - 1)),
                            reads=[wlb[s], xstb[s]], writes=[psb[bu]], inc=(dc == DC - 1))
                    sg = fcc % 2
                    fcc += 1
                    P.op("act", lambda e, bg=bg, sg=sg: e.activation(out=SG[sg][:, :], in_=ps[bg][:, 0:TS], func=AF.Silu),
                         reads=[psb[bg]], writes=[sgb[sg]])
                    P.op("dve", lambda e, bu=bu, sg=sg, s=s, fc=fc: e.tensor_tensor(AT[s][:, fc, :], SG[sg][:, :], ps[bu][:, 0:TS], ALU.mult),
                         reads=[sgb[sg], psb[bu]], writes=[atb[s]])
                for sub in range(2):
                    for h in range(2):
                        by = self.next_bank()
                        for fc in range(4):
                            P.op("pe", lambda e, fc=fc, sub=sub, h=h, by=by, s=s: e.matmul(
                                ps[by][:, :], AT[s][:, fc, sub * 128:(sub + 1) * 128],
                                Wall[s][:, 8192 + fc * 1024 + h * 512:8192 + fc * 1024 + (h + 1) * 512],
                                start=(fc == 0), stop=(fc == 3)),
                                reads=[atb[s], wlb[s]], writes=[psb[by]], inc=(fc == 3))
                        self.copy(self.ev_eng(), Yst[s][:, sub, h * 512:(h + 1) * 512], ps[by][:, :], [psb[by]], [ystb[s]])
                P.dma("sp", Ys_v[t], Yst[s][:], reads=[ystb[s]])
            P.barrier()

            Gt = self.sb(es, "Gt2", [128, 1024], F32)
            Bt = self.sb(es, "Bt2", [128, 1024], F32)
            b_gb = Buf()
            P.dma("sp", Gt[:], io["lnf_g"].partition_broadcast(128), writes=[b_gb])
            P.dma("sp", Bt[:], io["lnf_b"].partition_broadcast(128), writes=[b_gb])
            Hres = [self.sb(es, "Hr2_%d" % i, [128, 1024], F32) for i in range(2)]
            hrb = [Buf(), Buf()]
            Y1 = [self.sb(es, "Y1_%d" % i, [128, 1024], F32) for i in range(2)]
            Y2 = [self.sb(es, "Y2_%d" % i, [128, 1024], F32) for i in range(2)]
            y1b, y2b = [Buf(), Buf()], [Buf(), Buf()]
            Z = self.sb(es, "Z2", [128, 1024], F32)
            b_Z = Buf()
            H2 = [self.sb(es, "H2_%d" % i, [128, 1024], F32) for i in range(2)]
            h2b = [Buf(), Buf()]
            ln = self._layernorm(es, "f")
            sink = io.get("sink", "final")
            if sink != "final":
                HTo = [self.sb(es, "HTo%d" % i, [128, DC, 128], BF16) for i in range(2)]
                htob = [Buf(), Buf()]
                hT2_dv = io["hT2_d"].rearrange("(dc p) t -> p dc t", p=128)

            def load_blk(j):
                s = j % 2
                P.dma("sp", Hres[s][:], io["h1_d"][j * 128:(j + 1) * 128, :], writes=[hrb[s]])
                P.idma(Y1[s][:, :], Ys_d[:, :], in_off=POSi[:, 0, j:j + 1], reads=R, writes=[y1b[s]])
                P.idma(Y2[s][:, :], Ys_d[:, :], in_off=POSi[:, 1, j:j + 1], reads=R, writes=[y2b[s]])

            load_blk(0)
            for j in range(NJ):
                s = j % 2
                if j + 1 < NJ:
                    load_blk(j + 1)
                P.op("dve", lambda e, j=j, s=s: e.tensor_scalar(Z[:, :], Y1[s][:, :], W12[:, j, 0:1], None, ALU.mult),
                     reads=[y1b[s]], writes=[b_Z])
                P.op("dve", lambda e, j=j, s=s: e.scalar_tensor_tensor(Z[:, :], Y2[s][:, :], W12[:, j, 1:2], Z[:, :], ALU.mult, ALU.add),
                     reads=[y2b[s], b_Z], writes=[b_Z])
                P.op("dve", lambda e, s=s: e.scalar_tensor_tensor(Z[:, :], Hres[s][:, :], ALPHA, Z[:, :], ALU.mult, ALU.add),
                     reads=[hrb[s], b_Z], writes=[b_Z])
                ln(Z, b_Z, H2[s][:, :], h2b[s], Gt, Bt, b_gb)
                if sink == "final":
                    P.dma("sp", io["h_out"][j * 128:(j + 1) * 128, :], H2[s][:, :], reads=[h2b[s]])
                else:
                    gb = 2 * j + sink
                    P.dma("sp", io["h2_d"][gb * 128:(gb + 1) * 128, :], H2[s][:, :], reads=[h2b[s]])
                    for q in range(2):
                        bk = self.next_bank()
                        for k in range(4):
                            dc = q * 4 + k
                            P.op("pe", lambda e_, dc=dc, k=k, bk=bk, s=s: e_.transpose(
                                ps[bk][:, k * 128:(k + 1) * 128], H2[s][:, dc * 128:(dc + 1) * 128], self.ident[:, :]),
                                reads=[h2b[s]], writes=[psb[bk]], inc=(k == 3))
                        self.copy(self.ev_eng(), HTo[s][:, q * 4:(q + 1) * 4, :],
                                  ps[bk][:, :].rearrange("p (a b) -> p a b", b=128), [psb[bk]], [htob[s]])
                    P.dma("sp", hT2_dv[:, :, gb * 128:(gb + 1) * 128], HTo[s][:, :, :], reads=[htob[s]])


def build_fused_program(LP):
    nc = bass.Bass("TRN2", target_bir_lowering=False)
    NB = LP // 128
    NJ = NB // 2
    TO = NJ * 128

    def din(name, shape, dt=F32):
        return nc.dram_tensor(name, list(shape), dt, kind="ExternalInput").ap()

    def dscr(name, shape, dt):
        return nc.dram_tensor(name, list(shape), dt, kind="Internal").ap()

    g = {}
    g["xT_seq0"] = din("xT_seq0", [D, LP])
    g["xT_own0"] = din("xT_own0", [2, D, TO])
    g["x_own0"] = din("x_own0", [2, TO, D])
    g["ident"] = din("ident", [128, 128])
    g["nb_mask_s"] = din("nb_mask_s", [2, 3, 128, 128])
    g["nb_T_s"] = din("nb_T_s", [2, 3, 16, 128, 128])
    g["nb_mask_d"] = din("nb_mask_d", [3, 128, 128])
    g["b31"] = din("b31", [16])
    g["lam"] = din("lam", [256])
    g["subln"] = din("subln", [128, 1])
    g["b_f"] = din("b_f", [16, 1])
    g["npar"] = din("npar", [16, 2])
    g["par"] = din("par", [128, 2])
    g["pars"] = din("pars", [128, 2])
    for li in range(2):
        g["w_in%d" % li] = din("w_in%d" % li, [D, 3072 if li == 0 else 3088])
        g["w_o%d" % li] = din("w_o%d" % li, [D, D])
        for k in ("lnm_g", "lnm_b", "lnf_g", "lnf_b"):
            g["%s%d" % (k, li)] = din("%s%d" % (k, li), [D])
        g["w_r%d" % li] = din("w_r%d" % li, [D, 36])
        g["b_r%d" % li] = din("b_r%d" % li, [36])
        g["w_gate%d" % li] = din("w_gate%d" % li, [NEXP, D, DEXP])
        g["w_up%d" % li] = din("w_up%d" % li, [NEXP, D, DEXP])
        g["w_down%d" % li] = din("w_down%d" % li, [NEXP, DEXP, D])
    g["CQ3_d"] = dscr("CQ3_d", [16, 3, TO], BF16)
    g["KT_d"] = dscr("KT_d", [D, LP], BF16)
    g["QT_d"] = dscr("QT_d", [D, TO], BF16)
    g["V_d"] = dscr("V_d", [LP, D], BF16)
    g["h1_d"] = dscr("h1_d", [TO, D], F32)
    g["H1T_d"] = dscr("H1T_d", [D, TO], BF16)
    g["h2_d"] = dscr("h2_d", [LP, D], F32)
    g["hT2_d"] = dscr("hT2_d", [D, LP], BF16)
    g["h_out"] = nc.dram_tensor("h_out", [TO, D], F32, kind="ExternalOutput").ap()
    NT = (2 * TO + 255) // 256 + NEXP
    g["Xs_d"] = dscr("Xs_d", [NT * 256, D], F32)
    g["Ys_d"] = dscr("Ys_d", [NT * 256, D], F32)
    g["Wall_d0"] = dscr("Wall_d0", [NEXP * 128, 12288], BF16)
    g["Wall_d1"] = dscr("Wall_d1", [NEXP * 128, 12288], BF16)
    g["ltri"] = din("ltri", [128, 128])
    g["iota_p"] = din("iota_p", [128, 1])

    def layer_io(li):
        io = {}
        for k in ("w_in", "w_o", "lnm_g", "lnm_b", "lnf_g", "lnf_b", "w_r", "b_r", "w_gate", "w_up", "w_down"):
            io[k] = g["%s%d" % (k, li)]
        for k in ("ident", "KT_d", "QT_d", "V_d", "h1_d", "H1T_d", "h2_d", "hT2_d", "h_out", "CQ3_d",
                  "b31", "lam", "subln", "b_f", "npar", "par", "pars", "Xs_d", "Ys_d", "ltri", "iota_p"):
            io[k] = g[k]
        io["Wall_d"] = g["Wall_d%d" % li]
        return io

    with ExitStack() as es:
        P = Prog(nc, es)
        em = LayerEmitter(nc, P, es, LP)
        for c in range(2):
            io = layer_io(0)
            io.update(xT_seq=g["xT_seq0"], xT_own=g["xT_own0"][c], x_own=g["x_own0"][c],
                      nb_mask=g["nb_mask_s"][c], nb_T=g["nb_T_s"][c],
                      do_kv=(c == 0), q_mode="static", res_mode="static", sink=c)
            em.queue_wconv(layer_io(c))
            with ExitStack() as es_layer:
                em.emit_layer("diff", 0, io, es_layer)
            P.barrier()
        io = layer_io(1)
        io.update(xT_seq=g["hT2_d"], nb_mask=g["nb_mask_d"], do_kv=True, q_mode="dyn", res_mode="dyn", sink="final")
        with ExitStack() as es_layer:
            em.emit_layer("fox", 1, io, es_layer)
        P.barrier()
    return nc, P


def _t5_bucket_np(dist):
    n = np.maximum(np.asarray(dist, np.int32), 0)
    nf = np.maximum(n, 1).astype(np.float32)
    large = 16 + (np.log(nf / np.float32(16)) / np.float32(math.log(128 / 16)) * np.float32(16)).astype(np.int32)
    large = np.minimum(large, 31)
    return np.where(n < 16, n, large)


def _near_tables(c):
    qi = np.arange(128)[None, :]
    ki = np.arange(128)[:, None]
    idx = np.zeros((3, 128, 128), np.int32)
    msk = np.zeros((3, 128, 128), np.float32)
    for s in range(3):
        dist = (c + 1 - s) * 128 + qi - ki
        idx[s] = _t5_bucket_np(np.maximum(dist, 0))
        msk[s] = np.where(dist < 0, NEGM, 0.0)
    return idx, msk


_PROG_CACHE = {}


def _get_prog(LP):
    if LP not in _PROG_CACHE:
        _PROG_CACHE[LP] = build_fused_program(LP)[0]
    return _PROG_CACHE[LP]


def kernel(**inputs):
    inp = {k: np.asarray(v) for k, v in inputs.items()}
    x = inp["x"].astype(np.float32, copy=False)
    B, S, _ = x.shape
    L = S + N_META
    LP = ((L + 255) // 256) * 256
    NB = LP // 128
    NJ = NB // 2
    h = np.zeros((B, LP, D), np.float32)
    h[:, :N_META] = inp["meta_tokens"][None]
    h[:, N_META:L] = x
    nc = _get_prog(LP)
    ca = np.ascontiguousarray
    common = {"ident": np.eye(128, dtype=np.float32)}
    common["ltri"] = np.triu(np.ones((128, 128), np.float32), 1)
    common["iota_p"] = np.arange(128, dtype=np.float32).reshape(128, 1)
    idx0, msk0 = _near_tables(0)
    idx1, msk1 = _near_tables(1)
    rb = inp["rel_bias"]
    common["nb_mask_s"] = ca(np.stack([msk0, msk1]))
    common["nb_T_s"] = ca(np.stack([np.transpose(rb[idx0], (0, 3, 1, 2)), np.transpose(rb[idx1], (0, 3, 1, 2))]))
    common["b31"] = ca(rb[31])
    common["lam"] = ca(inp["diff_lambda"][0].reshape(256))
    common["subln"] = ca(inp["diff_subln"][0].reshape(128, 1))
    common["b_f"] = ca(inp["fox_b_f"][0].reshape(16, 1))
    for li in range(2):
        common["w_in%d" % li] = ca(inp["diff_w_qkv"][0] if li == 0 else inp["fox_w_in"][0])
        common["w_o%d" % li] = ca(inp["diff_w_o"][0] if li == 0 else inp["fox_w_o"][0])
        common["lnm_g%d" % li] = ca(inp["ln_mix_g"][li])
        common["lnm_b%d" % li] = ca(inp["ln_mix_b"][li])
        common["lnf_g%d" % li] = ca(inp["ln_ffn_g"][li])
        common["lnf_b%d" % li] = ca(inp["ln_ffn_b"][li])
        common["w_r%d" % li] = ca(np.concatenate([inp["router_group_w"][li], inp["router_expert_w"][li].reshape(D, 32)], axis=1))
        common["b_r%d" % li] = ca(np.concatenate([inp["router_group_b"][li], inp["router_expert_b"][li].reshape(32)]))
        common["w_gate%d" % li] = ca(inp["expert_w_gate"][li])
        common["w_up%d" % li] = ca(inp["expert_w_up"][li])
        common["w_down%d" % li] = ca(inp["expert_w_down"][li])
    in_maps = []
    for b in range(B):
        hT = ca(h[b].T)
        hb = h[b].reshape(NJ, 2, 128, D)
        own = ca(np.transpose(hb, (1, 0, 2, 3)).reshape(2, NJ * 128, D))
        ownT = ca(np.transpose(own, (0, 2, 1)))
        for c in range(2):
            m = dict(common)
            m["xT_seq0"] = hT
            m["x_own0"] = own
            m["xT_own0"] = ownT
            m["nb_mask_d"] = msk0 if c == 0 else msk1
            sel = np.zeros((128, 2), np.float32)
            sel[:, c] = 1.0
            m["par"] = sel
            m["pars"] = sel * np.float32(0.125)
            m["npar"] = ca(-sel[:16])
            in_maps.append(m)
    res = run_bass_kernel_spmd(nc, in_maps, core_ids=list(range(len(in_maps))))
    out = np.zeros((B, NJ, 2, 128, D), np.float32)
    k = 0
    for b in range(B):
        for c in range(2):
            out[b, :, c] = np.asarray(res.results[k]["h_out"]).reshape(NJ, 128, D)
            k += 1
    return ca(out.reshape(B, LP, D)[:, N_META:L])
```

```python
import math
from contextlib import ExitStack

import numpy as np
import concourse.bass as bass
import concourse.mybir as mybir
from concourse.bass_utils import run_bass_kernel_spmd

F32 = mybir.dt.float32
BF16 = mybir.dt.bfloat16
AF = mybir.ActivationFunctionType
ALU = mybir.AluOpType
AX = mybir.AxisListType

D = 1024
DC = 8
N_META = 16
NEXP = 32
DEXP = 512
DEPTH = 2
ALPHA = (2 * DEPTH) ** 0.25
LN_EPS = 1e-5
NEGM = -30000.0
NDS = 56
NBULK = 8
NSP = 20
import os
DEBUG = os.environ.get("KDEBUG", "0") == "1"
DBG = {}


def diff_lambda_init(layer_idx):
    return 0.8 - 0.6 * math.exp(-0.3 * layer_idx)


class Buf:
    __slots__ = ("w", "r", "excl")

    def __init__(self, excl=False):
        self.w = None
        self.r = {}
        self.excl = excl


class Prog:
    def __init__(self, nc, es):
        self.nc = nc
        self.E = {"pe": nc.tensor, "act": nc.scalar, "dve": nc.vector, "pool": nc.gpsimd, "sp": nc.sync}
        self.semobj = {}
        for k in self.E:
            self.semobj[k] = es.enter_context(nc.semaphore("s_" + k))
        self.cnt = {k: 0 for k in self.E}
        self.pending = {k: False for k in self.E}
        self.waited = {k: {} for k in self.E}
        self.dval = [0] * NDS
        for i in range(NDS):
            self.semobj[("d", i)] = es.enter_context(nc.semaphore("d%d" % i))
        self.dnext = 0
        self.bnext = 0
        self.pnext = 0
        self.nins = 0

    def _wait(self, eng, tok):
        key, val = tok
        if self.waited[eng].get(key, 0) >= val:
            return
        self.E[eng].wait_ge(self.semobj[key], val)
        self.waited[eng][key] = val

    def _deps(self, eng, reads, writes):
        toks = []
        for b in reads:
            if b.w is not None:
                toks.append(b.w)
            if b.excl:
                toks.extend(b.r.items())
        for b in writes:
            if b.w is not None:
                toks.append(b.w)
            toks.extend(b.r.items())
        for t in toks:
            if eng == "pe" and t[0] == "pe":
                continue
            self._wait(eng, t)

    def _mark(self, tok, reads, writes):
        for b in reads:
            if b.excl:
                b.w = tok
                b.r = {}
            else:
                b.r[tok[0]] = tok[1]
        for b in writes:
            b.w = tok
            b.r = {}

    def op(self, eng, fn, reads=(), writes=(), inc=True):
        self._deps(eng, reads, writes)
        ins = fn(self.E[eng])
        self.nins += 1
        if inc:
            self.cnt[eng] += 1
            ins.then_inc(self.semobj[eng], 1)
            tok = (eng, self.cnt[eng])
            self.pending[eng] = False
        else:
            tok = (eng, self.cnt[eng] + 1)
            self.pending[eng] = True
        self._mark(tok, reads, writes)
        return tok

    def dma(self, eng, out, in_, reads=(), writes=(), bulk=False):
        if bulk:
            i = NDS - NBULK + self.bnext
            self.bnext = (self.bnext + 1) % NBULK
        elif eng == "pool":
            i = NSP + self.pnext
            self.pnext = (self.pnext + 1) % (NDS - NBULK - NSP)
        else:
            i = self.dnext
            self.dnext = (i + 1) % NSP
        key = ("d", i)
        if self.dval[i] > 0:
            self._wait(eng, (key, self.dval[i]))
        self._deps(eng, reads, writes)
        ins = self.E[eng].dma_start(out=out, in_=in_)
        self.nins += 1
        self.dval[i] += 16
        ins.then_inc(self.semobj[key], 16)
        tok = (key, self.dval[i])
        self._mark(tok, reads, writes)
        return tok

    def idma(self, out, in_, out_off=None, in_off=None, reads=(), writes=()):
        eng = "pool"
        i = NSP + self.pnext
        self.pnext = (self.pnext + 1) % (NDS - NBULK - NSP)
        key = ("d", i)
        if self.dval[i] > 0:
            self._wait(eng, (key, self.dval[i]))
        self._deps(eng, reads, writes)
        oo = bass.IndirectOffsetOnAxis(ap=out_off, axis=0) if out_off is not None else None
        io_ = bass.IndirectOffsetOnAxis(ap=in_off, axis=0) if in_off is not None else None
        ins = self.E[eng].indirect_dma_start(out=out, out_offset=oo, in_=in_, in_offset=io_)
        self.nins += 1
        self.dval[i] += 16
        ins.then_inc(self.semobj[key], 16)
        tok = (key, self.dval[i])
        self._mark(tok, reads, writes)
        return tok

    def barrier(self):
        for k in self.E:
            assert not self.pending[k], k
        toks = [(k, self.cnt[k]) for k in self.E if self.cnt[k] > 0]
        toks += [(("d", i), v) for i, v in enumerate(self.dval) if v > 0]
        for eng in self.E:
            for t in toks:
                self._wait(eng, t)


def _divisor_le(n, m):
    for d in range(min(n, m), 0, -1):
        if n % d == 0:
            return d
    return 1


class LayerEmitter:
    def __init__(self, nc, P, es, LP):
        self.nc = nc
        self.P = P
        self.LP = LP
        self.NB = LP // 128
        self.NJ = self.NB // 2
        self.TO = self.NJ * 128
        self.ps = []
        self.psb = []
        for i in range(8):
            t = es.enter_context(nc.psum_tensor("ps%d" % i, [128, 512], F32))
            self.ps.append(t)
            self.psb.append(Buf(excl=True))
        self.psrr = 0
        self.evrr = 0

    def sb(self, es, name, shape, dt):
        self.sbn = getattr(self, "sbn", 0) + 1
        return es.enter_context(self.nc.sbuf_tensor("sb%d_%s" % (self.sbn, name), shape, dt))

    def next_bank(self):
        i = self.psrr
        self.psrr = (i + 1) % 8
        return i

    def ev_eng(self):
        self.evrr ^= 1
        return "act" if self.evrr else "dve"

    def copy(self, eng, out, in_, reads, writes, scale=None):
        P = self.P
        if eng == "act":
            if scale is None:
                return P.op("act", lambda e: e.activation(out=out, in_=in_, func=AF.Copy), reads, writes)
            return P.op("act", lambda e: e.activation(out=out, in_=in_, func=AF.Copy, scale=scale), reads, writes)
        if scale is None:
            return P.op(eng, lambda e: e.tensor_copy(out=out, in_=in_), reads, writes)
        return P.op(eng, lambda e: e.tensor_scalar(out, in_, scale, None, ALU.mult), reads, writes)

    def emit_layer(self, ltype, li, io, es_layer):
        nc, P = self.nc, self.P
        LP, NB, NJ, TO = self.LP, self.NB, self.NJ, self.TO
        ps, psb = self.ps, self.psb
        fox = ltype == "fox"
        NCOL = 3088 if fox else 3072
        lam_init = diff_lambda_init(li)
        do_kv = io.get("do_kv", True)
        q_mode = io.get("q_mode", "static")

        ident = self.sb(es_layer, "ident%d" % li, [128, 128], F32)
        self.ident = ident
        ones_bf = self.sb(es_layer, "ones_bf%d" % li, [128, 128], BF16)
        ones_f = self.sb(es_layer, "ones_f%d" % li, [128, 128], F32)
        cb = Buf()
        P.dma("sp", ident[:], io["ident"], writes=[cb])
        P.op("dve", lambda e: e.memset(ones_bf[:], 1.0), writes=[cb])
        P.op("dve", lambda e: e.memset(ones_f[:], 1.0), writes=[cb])
        SEL = self.sb(es_layer, "SEL%d" % li, [128, NJ, NEXP], F32)
        SEL1 = self.sb(es_layer, "SEL1_%d" % li, [128, NJ, NEXP], F32)
        W12 = self.sb(es_layer, "W12_%d" % li, [128, NJ, 2], F32)
        GATES = (SEL, SEL1, W12)
        self.ones_bf = ones_bf
        NCK = None
        if fox:
            NCK = self.sb(es_layer, "nck%d" % li, [128, NB * 16], F32)

        xT_seq = io["xT_seq"].rearrange("(dc p) t -> p dc t", p=128)
        q_jobs = io.get("q_jobs", []) if q_mode == "static" else []
        w_in = io["w_in"].rearrange("(dc p) n -> p dc n", p=128)
        KT_d, QT_d, V_d = io["KT_d"], io["QT_d"], io["V_d"]
        KT_dv = KT_d.rearrange("(c p) t -> p c t", p=128)
        QT_dv = QT_d.rearrange("(c p) t -> p c t", p=128)
        V_dv = V_d.rearrange("(b p) n -> p b n", p=128)

        if fox:
            with ExitStack() as es:
                pairs = _divisor_le(NJ, 11)
                CH = pairs * 256
                nchunks = NJ // pairs
                Wf = self.sb(es, "wf", [128, DC, 16], BF16)
                nbf = self.sb(es, "nbf", [16, 1], F32)
                npar = self.sb(es, "npar", [16, 2], F32)
                ones_s = self.sb(es, "ones_s", [16, CH], F32)
                carry = self.sb(es, "carry", [16, 1], F32)
                hTs = [self.sb(es, "hTf%d" % i, [128, DC, 512], BF16) for i in range(2)]
                hTb = [Buf(), Buf()]
                Et = self.sb(es, "Et", [16, 512], F32)
                LF = self.sb(es, "LF", [16, CH], F32)
                CN = self.sb(es, "CN", [16, CH], F32)
                T1 = self.sb(es, "T1", [16, pairs * 128], F32)
                CQ = self.sb(es, "CQ", [16, pairs * 128], F32)
                R1 = self.sb(es, "R1", [16, pairs * 128], F32)
                R2 = self.sb(es, "R2", [16, pairs * 128], F32)
                C3 = self.sb(es, "C3", [16, 3, pairs * 128], BF16)
                b_wf, b_nbf, b_par, b_ones, b_carry = Buf(), Buf(), Buf(), Buf(), Buf()
                b_Et, b_LF, b_CN, b_T1, b_CQ, b_R1, b_R2, b_C3 = (Buf() for _ in range(8))
                b_nck = Buf()
                P.dma("pool", Wf[:], w_in[:, :, 3072:3088], writes=[b_wf])
                P.dma("sp", nbf[:], io["b_f"], writes=[b_nbf])
                P.op("dve", lambda e: e.tensor_scalar(nbf[:], nbf[:], -1.0, None, ALU.mult), reads=[b_nbf], writes=[b_nbf])
                P.dma("sp", npar[:], io["npar"], writes=[b_par])
                P.op("dve", lambda e: e.memset(ones_s[:], 1.0), writes=[b_ones])
                P.op("dve", lambda e: e.memset(carry[:], 0.0), writes=[b_carry])
                tcount = 0
                for ci in range(nchunks):
                    t0 = ci * CH
                    off = 0
                    while off < CH:
                        n = min(512, CH - off)
                        s = tcount % 2
                        tcount += 1
                        P.dma("pool", hTs[s][:, :, 0:n], xT_seq[:, :, t0 + off:t0 + off + n], writes=[hTb[s]])
                        bk = self.next_bank()
                        for dc in range(DC):
                            P.op("pe", lambda e, dc=dc, s=s, n=n, bk=bk: e.matmul(
                                ps[bk][0:16, 0:n], Wf[:, dc, :], hTs[s][:, dc, 0:n], start=(dc == 0), stop=(dc == DC - 1)),
                                reads=[b_wf, hTb[s]], writes=[psb[bk]], inc=(dc == DC - 1))
                        P.op("act", lambda e, n=n, bk=bk: e.activation(out=Et[:, 0:n], in_=ps[bk][0:16, 0:n], func=AF.Exp,
                                                                 bias=nbf[:, 0:1], scale=-1.0),
                             reads=[psb[bk], b_nbf], writes=[b_Et])
                        P.op("act", lambda e, n=n, off=off: e.activation(out=LF[:, off:off + n], in_=Et[:, 0:n], func=AF.Ln,
                                                                       bias=1.0, scale=1.0),
                             reads=[b_Et], writes=[b_LF])
                        off += n
                    P.op("dve", lambda e: e.tensor_tensor_scan(CN[:, :], ones_s[:, :], LF[:, :], carry[:, 0:1], ALU.mult, ALU.add),
                         reads=[b_ones, b_LF, b_carry], writes=[b_CN])
                    P.op("dve", lambda e: e.tensor_copy(out=carry[:, 0:1], in_=CN[:, CH - 1:CH]), reads=[b_CN], writes=[b_carry])
                    nblk = CH // 128
                    bk = self.next_bank()
                    for b in range(nblk):
                        P.op("pe", lambda e, b=b, bk=bk: e.transpose(ps[bk][:, b * 16:(b + 1) * 16], CN[:, b * 128:(b + 1) * 128],
                                                                  ident[0:16, 0:16]),
                             reads=[b_CN, cb], writes=[psb[bk]], inc=(b == nblk - 1))
                    g0 = t0 // 128
                    P.op("act", lambda e, bk=bk, g0=g0, nblk=nblk: e.activation(out=NCK[:, g0 * 16:(g0 + nblk) * 16],
                                                                               in_=ps[bk][:, 0:nblk * 16], func=AF.Copy),
                         reads=[psb[bk]], writes=[b_nck])
                    CNv = CN[:, :].rearrange("p (a two q) -> p a two q", two=2, q=128)
                    T1v = T1[:, :].rearrange("p (a q) -> p a q", q=128)
                    CQv = CQ[:, :].rearrange("p (a q) -> p a q", q=128)
                    P.op("dve", lambda e: e.tensor_scalar(T1v, CNv[:, :, 1, :], npar[:, 1:2], None, ALU.mult),
                         reads=[b_CN, b_par], writes=[b_T1])
                    P.op("dve", lambda e: e.scalar_tensor_tensor(CQv, CNv[:, :, 0, :], npar[:, 0:1], T1v, ALU.mult, ALU.add),
                         reads=[b_CN, b_par, b_T1], writes=[b_CQ])
                    P.op("dve", lambda e: e.tensor_copy(out=C3[:, 0, :], in_=CQ[:, :]), reads=[b_CQ], writes=[b_C3])
                    P.op("dve", lambda e: e.tensor_tensor(R1[:, :], CQ[:, :], C3[:, 0, :], ALU.subtract), reads=[b_CQ, b_C3], writes=[b_R1])
                    P.op("dve", lambda e: e.tensor_copy(out=C3[:, 1, :], in_=R1[:, :]), reads=[b_R1], writes=[b_C3])
                    P.op("dve", lambda e: e.tensor_tensor(R2[:, :], R1[:, :], C3[:, 1, :], ALU.subtract), reads=[b_R1, b_C3], writes=[b_R2])
                    P.op("dve", lambda e: e.tensor_copy(out=C3[:, 2, :], in_=R2[:, :]), reads=[b_R2], writes=[b_C3])
                    o0 = ci * pairs * 128
                    P.dma("sp", io["CQ3_d"][:, :, o0:o0 + pairs * 128], C3[:, :, :], reads=[b_C3])
                P.barrier()

        with ExitStack() as es:
            Wb = self.sb(es, "Wb", [128, DC, 3072], BF16)
            b_W = Buf()
            for pc in range(6 if do_kv else (2 if (q_jobs or q_mode == "dyn") else 0)):
                P.dma("pool", Wb[:, :, pc * 512:(pc + 1) * 512], w_in[:, :, pc * 512:(pc + 1) * 512], writes=[b_W])
            if q_mode == "dyn":
                stq = [self.sb(es, "stq%d" % i, [128, 8, 256], BF16) for i in range(2)]
                stqb = [Buf(), Buf()]
                tmpq = self.sb(es, "tmpq", [128, 2, 128], F32)
                b_tmpq = Buf()
                pars = self.sb(es, "pars", [128, 2], F32)
                b_pars = Buf()
                P.dma("sp", pars[:], io["pars"], writes=[b_pars])
            hTs = [self.sb(es, "hTa%d" % i, [128, DC, 512], BF16) for i in range(2)]
            hTb = [Buf(), Buf()]
            stk = [self.sb(es, "stk%d" % i, [128, 8, 512], BF16) for i in range(2)]
            stkb = [Buf(), Buf()]
            stv = [self.sb(es, "stv%d" % i, [128, 4, 1024], BF16) for i in range(2)]
            stvb = [Buf(), Buf()]
            tcount = 0
            for t0 in (range(0, LP, 512) if do_kv else []):
                n = min(512, LP - t0)
                s = tcount % 2
                tcount += 1
                P.dma("pool", hTs[s][:, :, 0:n], xT_seq[:, :, t0:t0 + n], writes=[hTb[s]])
                for c in range(8):
                    bk = self.next_bank()
                    for dc in range(DC):
                        P.op("pe", lambda e, c=c, dc=dc, s=s, n=n, bk=bk: e.matmul(
                            ps[bk][:, 0:n], Wb[:, dc, 1024 + c * 128:1024 + (c + 1) * 128], hTs[s][:, dc, 0:n],
                            start=(dc == 0), stop=(dc == DC - 1)),
                            reads=[b_W, hTb[s]], writes=[psb[bk]], inc=(dc == DC - 1))
                    self.copy(self.ev_eng(), stk[s][:, c, 0:n], ps[bk][:, 0:n], [psb[bk]], [stkb[s]])
                P.dma("sp", KT_dv[:, :, t0:t0 + n], stk[s][:, :, 0:n], reads=[stkb[s]])
                nb = n // 128
                for b in range(nb):
                    for half in range(2):
                        bk = self.next_bank()
                        for dc in range(DC):
                            P.op("pe", lambda e, b=b, half=half, dc=dc, s=s, bk=bk: e.matmul(
                                ps[bk][:, :], hTs[s][:, dc, b * 128:(b + 1) * 128],
                                Wb[:, dc, 2048 + half * 512:2048 + (half + 1) * 512],
                                start=(dc == 0), stop=(dc == DC - 1)),
                                reads=[b_W, hTb[s]], writes=[psb[bk]], inc=(dc == DC - 1))
                        self.copy(self.ev_eng(), stv[s][:, b, half * 512:(half + 1) * 512], ps[bk][:, :], [psb[bk]], [stvb[s]])
                blk0 = t0 // 128
                P.dma("sp", V_dv[:, blk0:blk0 + nb, :], stv[s][:, 0:nb, :], reads=[stvb[s]])
                if q_mode == "dyn":
                    npair = n // 256
                    for c in range(8):
                        bk = self.next_bank()
                        for dc in range(DC):
                            P.op("pe", lambda e, c=c, dc=dc, s=s, n=n, bk=bk: e.matmul(
                                ps[bk][:, 0:n], Wb[:, dc, c * 128:(c + 1) * 128], hTs[s][:, dc, 0:n],
                                start=(dc == 0), stop=(dc == DC - 1)),
                                reads=[b_W, hTb[s]], writes=[psb[bk]], inc=(dc == DC - 1))
                        psv = ps[bk][:, 0:n].rearrange("p (a two q) -> p a two q", two=2, q=128)
                        P.op("dve", lambda e, psv=psv, npair=npair: e.tensor_scalar(
                            tmpq[:, 0:npair, :], psv[:, :, 1, :], pars[:, 1:2], None, ALU.mult),
                            reads=[psb[bk], b_pars], writes=[b_tmpq])
                        P.op("dve", lambda e, psv=psv, npair=npair, c=c, s=s: e.scalar_tensor_tensor(
                            stq[s][:, c, 0:npair * 128].rearrange("p (a q) -> p a q", q=128), psv[:, :, 0, :], pars[:, 0:1],
                            tmpq[:, 0:npair, :], ALU.mult, ALU.add),
                            reads=[psb[bk], b_pars, b_tmpq], writes=[stqb[s]])
                    P.dma("sp", QT_dv[:, :, t0 // 2:t0 // 2 + npair * 128], stq[s][:, :, 0:npair * 128], reads=[stqb[s]])
            for (xo_, qd_, t0) in [(xo_, qd_, t0) for (xo_, qd_) in q_jobs for t0 in range(0, TO, 512)]:
                xT_own = xo_.rearrange("(dc p) t -> p dc t", p=128)
                QT_dv = qd_.rearrange("(c p) t -> p c t", p=128)
                n = min(512, TO - t0)
                s = tcount % 2
                tcount += 1
                P.dma("pool", hTs[s][:, :, 0:n], xT_own[:, :, t0:t0 + n], writes=[hTb[s]])
                for c in range(8):
                    bk = self.next_bank()
                    for dc in range(DC):
                        P.op("pe", lambda e, c=c, dc=dc, s=s, n=n, bk=bk: e.matmul(
                            ps[bk][:, 0:n], Wb[:, dc, c * 128:(c + 1) * 128], hTs[s][:, dc, 0:n],
                            start=(dc == 0), stop=(dc == DC - 1)),
                            reads=[b_W, hTb[s]], writes=[psb[bk]], inc=(dc == DC - 1))
                    self.copy(self.ev_eng(), stk[s][:, c, 0:n], ps[bk][:, 0:n], [psb[bk]], [stkb[s]], scale=0.125)
                P.dma("sp", QT_dv[:, :, t0:t0 + n], stk[s][:, :, 0:n], reads=[stkb[s]])
            P.barrier()

        with ExitStack() as es_x:
            XT = self.sb(es_x, "XT", [128, DC, TO], BF16)
            self._attention(ltype, li, io, XT, ident, ones_bf, ones_f, NCK, lam_init)
            P.barrier()
            self._post_attention(li, io, XT, ident, GATES)
            P.barrier()
        self._moe_routed(li, io, GATES)
        P.barrier()

    def _attention(self, ltype, li, io, XT, ident, ones_bf, ones_f, NCK, lam_init):
        nc, P = self.nc, self.P
        LP, NB, NJ, TO = self.LP, self.NB, self.NJ, self.TO
        ps, psb = self.ps, self.psb
        fox = ltype == "fox"
        Kd = 67 if fox else 64
        KT_d, QT_d, V_d = io["KT_d"], io["QT_d"], io["V_d"]
        with ExitStack() as es:
            KT = [self.sb(es, "KT%d" % i, [128, LP], BF16) for i in range(2)]
            QT = [self.sb(es, "QT%d" % i, [128, TO], BF16) for i in range(2)]
            Vl = [self.sb(es, "Vl%d" % i, [128, NB, 128], BF16) for i in range(2)]
            ktb, qtb, vlb = [Buf(), Buf()], [Buf(), Buf()], [Buf(), Buf()]
            NPT = 4
            PT = [self.sb(es, "PT%d" % i, [128, 512], BF16) for i in range(NPT)]
            ptb = [Buf() for _ in range(NPT)]
            rS = self.sb(es, "rS", [128, 512], F32)
            tO = self.sb(es, "tO", [128, 512], F32)
            b_rS, b_tO = Buf(), Buf()
            mask = self.sb(es, "mask", [128, 3, 128], F32)
            b_mask = Buf()
            P.dma("sp", mask[:], io["nb_mask"].rearrange("s k q -> k s q"), writes=[b_mask])
            if fox:
                for i in range(2):
                    P.op("pool", lambda e, i=i: e.memset(KT[i][64:67, :], 1.0), writes=[ktb[i]])
                    oth = 1 - i
                    P.op("pool", lambda e, i=i, oth=oth: e.memset(Vl[i][:, :, oth * 64:(oth + 1) * 64], 0.0), writes=[vlb[i]])
                NBt = [mask, mask]
                nbb = [b_mask, b_mask]
            else:
                NBt = [self.sb(es, "NBt%d" % i, [128, 3, 128], F32) for i in range(2)]
                nbb = [Buf(), Buf()]
                Kd = 128
                for i in range(2):
                    P.op("pool", lambda e, i=i: e.memset(KT[i][64:128, :], 0.0), writes=[ktb[i]])
                    P.op("pool", lambda e, i=i: e.memset(QT[i][64:128, :], 0.0), writes=[qtb[i]])
                B31 = self.sb(es, "B31", [128, 16], F32)
                lam = self.sb(es, "lam", [128, 256], F32)
                lpr = self.sb(es, "lpr", [128, 128], F32)
                lsum = self.sb(es, "lsum", [128, 2], F32)
                nlam = self.sb(es, "nlam", [128, 1], F32)
                gsub = self.sb(es, "gsub", [128, 1], F32)
                dA = self.sb(es, "dA", [128, TO], F32)
                dd = self.sb(es, "dd", [128, 512], F32)
                sq = self.sb(es, "sq", [128, 512], BF16)
                rstd = self.sb(es, "rstd", [128, 512], F32)
                b_B31, b_lam, b_nlam, b_gsub, b_dd, b_sq, b_rstd = (Buf() for _ in range(7))
                P.dma("sp", B31[:], io["b31"].partition_broadcast(128), writes=[b_B31])
                P.dma("sp", lam[:], io["lam"].partition_broadcast(128), writes=[b_lam])
                P.dma("sp", gsub[:], io["subln"], writes=[b_gsub])
                P.op("dve", lambda e: e.tensor_scalar(gsub[:], gsub[:], 1.0 - lam_init, None, ALU.mult), reads=[b_gsub], writes=[b_gsub])
                lamv = lam[:, :].rearrange("p (a two d) -> p a two d", two=2, d=64)
                lprv = lpr[:, :].rearrange("p (a d) -> p a d", d=64)
                P.op("dve", lambda e: e.tensor_tensor(lprv, lamv[:, :, 0, :], lamv[:, :, 1, :], ALU.mult), reads=[b_lam], writes=[b_lam])
                P.op("dve", lambda e: e.reduce_sum(lsum[:, 0:1], lpr[:, 0:64], AX.X), reads=[b_lam], writes=[b_nlam])
                P.op("dve", lambda e: e.reduce_sum(lsum[:, 1:2], lpr[:, 64:128], AX.X), reads=[b_lam], writes=[b_nlam])
                P.op("act", lambda e: e.activation(out=lsum[:, :], in_=lsum[:, :], func=AF.Exp), reads=[b_nlam], writes=[b_nlam])
                P.op("dve", lambda e: e.scalar_tensor_tensor(nlam[:, 0:1], lsum[:, 1:2], -lam_init, lsum[:, 0:1], ALU.add, ALU.subtract),
                     reads=[b_nlam], writes=[b_nlam])
                dAb = {}

            def load_map(m):
                s = m % 2
                if not fox:
                    P.dma("sp", NBt[s][:], io["nb_T"][:, m].rearrange("s k q -> k s q"), writes=[nbb[s]])
                P.dma("sp", KT[s][0:64, :], KT_d[m * 64:(m + 1) * 64, :], writes=[ktb[s]])
                P.dma("sp", QT[s][0:64, :], QT_d[m * 64:(m + 1) * 64, :], writes=[qtb[s]])
                if fox:
                    P.dma("sp", QT[s][64:67, :], io["CQ3_d"][m, :, :], writes=[qtb[s]])
                    P.dma("sp", Vl[s][:, :, s * 64:(s + 1) * 64],
                          V_d.rearrange("(b p) n -> p b n", p=128)[:, :, m * 64:(m + 1) * 64], writes=[vlb[s]])
                else:
                    P.dma("sp", Vl[s][:, :, :], V_d.rearrange("(b p) n -> p b n", p=128)[:, :, (m // 2) * 128:(m // 2 + 1) * 128], writes=[vlb[s]])

            def fix_map(m):
                if not fox:
                    s = m % 2
                    P.op("dve", lambda e, s=s, m=m: e.scalar_tensor_tensor(NBt[s][:], NBt[s][:], B31[:, m:m + 1], mask[:],
                                                                            ALU.subtract, ALU.add),
                         reads=[nbb[s], b_B31, b_mask], writes=[nbb[s]])

            load_map(0)
            fix_map(0)
            dq = []
            itc = [0]
            last_pe = [None]

            def run_due():
                while dq and dq[0][0] <= itc[0]:
                    dq.pop(0)[1]()
            NG = (NJ + 3) // 4
            LA = 2
            SB = [0, 1, 2]
            OS = [(3, 4), (5, 6)]
            gcount = 0
            for m in range(16):
                s = m % 2
                if m + 1 < 16:
                    load_map(m + 1)
                c = m // 2
                half = m % 2
                for g in range(NG):
                    j0 = 4 * g
                    j1 = min(j0 + 4, NJ)
                    nq = (j1 - j0) * 128
                    nkb = 2 * j1
                    bo, bs_ = OS[gcount % 2]
                    gcount += 1
                    info = {}
                    tail0 = max(2 * j0 - 1, 0)
                    bulk = list(range(0, tail0))
                    tail = list(range(tail0, nkb))
                    if len(bulk) > 1 and not fox:
                        head = bulk[:max(1, len(bulk) - len(tail))]
                        rest = bulk[len(head):]
                        order = list(head)
                        for a in range(max(len(rest), len(tail))):
                            if a < len(rest):
                                order.append(rest[a])
                            if a < len(tail):
                                order.append(tail[a])
                    else:
                        order = bulk + tail
                    assert sorted(order) == list(range(nkb)) and order[0] == 0
                    for i in range(nkb + LA):
                        run_due()
                        itc[0] += 1
                        if i < nkb:
                            kb = order[i]
                            jmin = max(j0, kb // 2)
                            qoff = (jmin - j0) * 128
                            sbk = SB[i % 3]
                            pt = i % NPT
                            info[i] = (qoff, pt)
                            P.op("pe", lambda e, kb=kb, qoff=qoff, sbk=sbk, s=s: e.matmul(
                                ps[sbk][:, qoff:nq], KT[s][0:Kd, kb * 128:(kb + 1) * 128],
                                QT[s][0:Kd, j0 * 128 + qoff:j0 * 128 + nq], start=True, stop=True),
                                reads=[ktb[s], qtb[s]], writes=[psb[sbk]])
                            for j in range(jmin, j1):
                                slot = kb - (2 * j - 1)
                                if slot < 0 or slot > 2:
                                    continue
                                if fox and slot == 0:
                                    continue
                                cq = (j - j0) * 128
                                P.op("dve", lambda e, sbk=sbk, cq=cq, slot=slot, s=s: e.tensor_tensor(
                                    ps[sbk][:, cq:cq + 128], ps[sbk][:, cq:cq + 128], NBt[s][:, slot, :], ALU.add),
                                    reads=[psb[sbk], nbb[s]], writes=[psb[sbk]])
                            if fox:
                                P.op("act", lambda e, sbk=sbk, qoff=qoff, pt=pt, kb=kb, m=m: e.activation(
                                    out=PT[pt][:, qoff:nq], in_=ps[sbk][:, qoff:nq], func=AF.Exp,
                                    bias=NCK[:, kb * 16 + m:kb * 16 + m + 1], scale=1.0),
                                    reads=[psb[sbk]], writes=[ptb[pt]])
                            else:
                                P.op("act", lambda e, sbk=sbk, qoff=qoff, pt=pt: e.activation(
                                    out=PT[pt][:, qoff:nq], in_=ps[sbk][:, qoff:nq], func=AF.Exp),
                                    reads=[psb[sbk]], writes=[ptb[pt]])
                        if i >= LA:
                            ii = i - LA
                            kb = order[ii]
                            qoff, pt = info.pop(ii)
                            P.op("pe", lambda e, kb=kb, ii=ii, qoff=qoff, pt=pt, s=s, bo=bo: e.matmul(
                                ps[bo][:, qoff:nq], Vl[s][:, kb, :], PT[pt][:, qoff:nq], start=(ii == 0), stop=(ii == nkb - 1)),
                                reads=[vlb[s], ptb[pt]], writes=[psb[bo]], inc=False)
                            last_pe[0] = P.op("pe", lambda e, ii=ii, qoff=qoff, pt=pt, bs_=bs_: e.matmul(
                                ps[bs_][:, qoff:nq], ones_bf[:, :], PT[pt][:, qoff:nq], start=(ii == 0), stop=(ii == nkb - 1)),
                                reads=[ptb[pt]], writes=[psb[bs_]], inc=True)
                    q0 = j0 * 128

                    def stA(g=g, c=c, half=half, q0=q0, nq=nq, bo=bo, bs_=bs_):
                        if fox:
                            r0, r1 = half * 64, (half + 1) * 64
                            P.op("dve", lambda e: e.reciprocal(rS[r0:r1, 0:nq], ps[bs_][r0:r1, 0:nq]),
                                 reads=[psb[bs_]], writes=[b_rS])
                            P.op("dve", lambda e: e.tensor_tensor(
                                XT[r0:r1, c, q0:q0 + nq], ps[bo][r0:r1, 0:nq], rS[r0:r1, 0:nq], ALU.mult),
                                reads=[psb[bo], b_rS], writes=[])
                            return
                        P.op("act", lambda e: e.activation(out=rS[:, 0:nq], in_=ps[bs_][:, 0:nq], func=AF.Ln), reads=[psb[bs_]], writes=[b_rS])
                        P.op("act", lambda e: e.activation(out=rS[:, 0:nq], in_=rS[:, 0:nq], func=AF.Exp, scale=-1.0), reads=[b_rS], writes=[b_rS])

                    def stB(g=g, c=c, half=half, q0=q0, nq=nq, bo=bo, bs_=bs_):
                        if half == 0:
                            dAb[g] = Buf()
                            P.op("dve", lambda e: e.tensor_tensor(dA[:, q0:q0 + nq], ps[bo][:, 0:nq], rS[:, 0:nq], ALU.mult),
                                 reads=[psb[bo], b_rS], writes=[dAb[g]])
                        else:
                            P.op("dve", lambda e: e.tensor_tensor(tO[:, 0:nq], ps[bo][:, 0:nq], rS[:, 0:nq], ALU.mult),
                                 reads=[psb[bo], b_rS], writes=[b_tO])
                            P.op("dve", lambda e: e.scalar_tensor_tensor(dd[:, 0:nq], tO[:, 0:nq], nlam[:, 0:1], dA[:, q0:q0 + nq],
                                                                         ALU.mult, ALU.add),
                                 reads=[b_tO, b_nlam, dAb[g]], writes=[b_dd])

                    def stC1(nq=nq):
                        P.op("act", lambda e: e.activation(out=sq[:, 0:nq], in_=dd[:, 0:nq], func=AF.Square), reads=[b_dd], writes=[b_sq])

                    def stC2(nq=nq):
                        P.op("pe", lambda e: e.matmul(ps[7][:, 0:nq], ones_bf[:, :], sq[:, 0:nq], start=True, stop=True),
                             reads=[b_sq], writes=[psb[7]])
                        P.op("dve", lambda e: e.tensor_scalar(rstd[:, 0:nq], ps[7][:, 0:nq], 1.0 / 128.0, LN_EPS, ALU.mult, ALU.add),
                             reads=[psb[7]], writes=[b_rstd])

                    def stC3(nq=nq):
                        P.op("act", lambda e: e.activation(out=rstd[:, 0:nq], in_=rstd[:, 0:nq], func=AF.Ln), reads=[b_rstd], writes=[b_rstd])
                        P.op("act", lambda e: e.activation(out=rstd[:, 0:nq], in_=rstd[:, 0:nq], func=AF.Exp, scale=-0.5), reads=[b_rstd], writes=[b_rstd])

                    def stC4(c=c, q0=q0, nq=nq):
                        P.op("dve", lambda e: e.scalar_tensor_tensor(XT[:, c, q0:q0 + nq], dd[:, 0:nq], gsub[:, 0:1], rstd[:, 0:nq],
                                                                     ALU.mult, ALU.mult),
                             reads=[b_dd, b_gsub, b_rstd], writes=[])

                    T = itc[0]
                    dq.append((T + 3, stA))
                    if not fox:
                        dq.append((T + 5, stB))
                        if half == 1:
                            dq.append((T + 8, stC1))
                            dq.append((T + 10, stC2))
                            dq.append((T + 12, stC3))
                            dq.append((T + 14, stC4))
                    if m == 1 and g == 0:
                        gate = Buf()
                        gate.w = last_pe[0]
                        first = True
                        for (dst, src) in getattr(self, "pending_conv", []):
                            P.dma("pool", dst, src, reads=([gate] if first else []), bulk=True)
                            first = False
                        self.pending_conv = []
                if m + 1 < 16:
                    fix_map(m + 1)
            for _, f_ in dq:
                f_()

    def _layernorm(self, es, tag):
        P = self.P
        st = self.sb(es, "st" + tag, [128, 2, 6], F32)
        mv = self.sb(es, "mv" + tag, [128, 2], F32)
        rs = self.sb(es, "rs" + tag, [128, 1], F32)
        b_st, b_mv, b_rs = Buf(), Buf(), Buf()

        def ln(Z, b_Z, out, b_out, Gt, Bt, b_gb):
            for h in range(2):
                P.op("dve", lambda e, h=h: e.bn_stats(st[:, h, :], Z[:, h * 512:(h + 1) * 512]), reads=[b_Z], writes=[b_st])
            P.op("dve", lambda e: e.bn_aggr(mv[:, :], st[:, :, :].rearrange("p a b -> p (a b)")), reads=[b_st], writes=[b_mv])
            P.op("dve", lambda e: e.tensor_scalar(rs[:, :], mv[:, 1:2], LN_EPS, None, ALU.add), reads=[b_mv], writes=[b_rs])
            P.op("act", lambda e: e.activation(out=rs[:, :], in_=rs[:, :], func=AF.Ln), reads=[b_rs], writes=[b_rs])
            P.op("act", lambda e: e.activation(out=rs[:, :], in_=rs[:, :], func=AF.Exp, scale=-0.5), reads=[b_rs], writes=[b_rs])
            P.op("dve", lambda e: e.tensor_scalar(Z[:, :], Z[:, :], mv[:, 0:1], rs[:, 0:1], ALU.subtract, ALU.mult),
                 reads=[b_Z, b_mv, b_rs], writes=[b_Z])
            P.op("dve", lambda e: e.tensor_tensor(Z[:, :], Z[:, :], Gt[:, :], ALU.mult), reads=[b_Z, b_gb], writes=[b_Z])
            P.op("dve", lambda e: e.tensor_tensor(out, Z[:, :], Bt[:, :], ALU.add), reads=[b_Z, b_gb], writes=[b_out])
        return ln

    def _post_attention(self, li, io, XT, ident, GATES):
        nc, P = self.nc, self.P
        NJ, TO = self.NJ, self.TO
        ps, psb = self.ps, self.psb
        with ExitStack() as es:
            Wo = self.sb(es, "Wo", [128, DC, 1024], BF16)
            b_Wo = Buf()
            wo_v = io["w_o"].rearrange("(dc p) n -> p dc n", p=128)
            for h in range(2):
                P.dma("pool", Wo[:, :, h * 512:(h + 1) * 512], wo_v[:, :, h * 512:(h + 1) * 512], writes=[b_Wo])
            Gt = self.sb(es, "Gt", [128, 1024], F32)
            Bt = self.sb(es, "Bt", [128, 1024], F32)
            b_gb = Buf()
            P.dma("sp", Gt[:], io["lnm_g"].partition_broadcast(128), writes=[b_gb])
            P.dma("sp", Bt[:], io["lnm_b"].partition_broadcast(128), writes=[b_gb])
            Wr = self.sb(es, "Wr", [128, DC, 36], F32)
            Br = self.sb(es, "Br", [128, 36], F32)
            b_Wr = Buf()
            P.dma("sp", Wr[:], io["w_r"].rearrange("(dc p) n -> p dc n", p=128), writes=[b_Wr])
            P.dma("sp", Br[:], io["b_r"].partition_broadcast(128), writes=[b_Wr])
            NR = 3
            Hres = [self.sb(es, "Hres%d" % i, [128, 1024], F32) for i in range(NR)]
            hrb = [Buf() for _ in range(NR)]
            Z = self.sb(es, "Z", [128, 1024], F32)
            b_Z = Buf()
            H1 = [self.sb(es, "H1_%d" % i, [128, 1024], F32) for i in range(2)]
            h1b = [Buf(), Buf()]
            HTf = self.sb(es, "HTf", [128, DC, 128], F32)
            b_HTf = Buf()
            Lg = self.sb(es, "Lg", [128, 36], F32)
            EM = self.sb(es, "EM", [128, 32], F32)
            sm = self.sb(es, "sm", [128, 16], F32)
            oh = self.sb(es, "oh", [128, 4], F32)
            pen = self.sb(es, "pen", [128, 4], F32)
            ge = self.sb(es, "ge", [128, 4], F32)
            M8 = self.sb(es, "M8", [128, 8], F32)
            e2 = self.sb(es, "e2", [128, 2], F32)
            sel = self.sb(es, "sel", [128, 32], F32)
            ew = self.sb(es, "ew", [128, 32], F32)
            b_r = Buf()
            ln = self._layernorm(es, "m")
            h1T_dv = io["H1T_d"].rearrange("(dc p) t -> p dc t", p=128)

            res_mode = io.get("res_mode", "static")
            if res_mode == "dyn":
                Hp = [self.sb(es, "Hp%d" % i, [128, 2, 1024], F32) for i in range(NR)]
                hpb = [Buf() for _ in range(NR)]
                par = self.sb(es, "par", [128, 2], F32)
                b_par = Buf()
                P.dma("sp", par[:], io["par"], writes=[b_par])

            def load_res(j, sl):
                if res_mode == "static":
                    P.dma("sp", Hres[sl][:], io["x_own"][j * 128:(j + 1) * 128, :], writes=[hrb[sl]])
                else:
                    P.dma("sp", Hp[sl][:], io["h2_d"][2 * j * 128:(2 * j + 2) * 128, :].rearrange("(two p) n -> p two n", p=128),
                          writes=[hpb[sl]])

            def select_res(sl):
                if res_mode != "static":
                    P.op("dve", lambda e, sl=sl: e.tensor_scalar(Hres[sl][:, :], Hp[sl][:, 1, :], par[:, 1:2], None, ALU.mult),
                         reads=[hpb[sl], b_par], writes=[hrb[sl]])
                    P.op("dve", lambda e, sl=sl: e.scalar_tensor_tensor(Hres[sl][:, :], Hp[sl][:, 0, :], par[:, 0:1], Hres[sl][:, :],
                                                                         ALU.mult, ALU.add),
                         reads=[hpb[sl], b_par, hrb[sl]], writes=[hrb[sl]])

            wo_banks = {}

            def emit_wo(j):
                t0 = j * 128
                bks = []
                for h in range(2):
                    bk = self.next_bank()
                    for dc in range(DC):
                        P.op("pe", lambda e, dc=dc, h=h, bk=bk: e.matmul(
                            ps[bk][:, :], XT[:, dc, t0:t0 + 128], Wo[:, dc, h * 512:(h + 1) * 512],
                            start=(dc == 0), stop=(dc == DC - 1)),
                            reads=[b_Wo], writes=[psb[bk]], inc=(dc == DC - 1))
                    bks.append(bk)
                wo_banks[j] = bks

            def emit_ln(j):
                s = j % 2
                sl = j % NR
                t0 = j * 128
                select_res(sl)
                for h, bk in enumerate(wo_banks.pop(j)):
                    P.op("dve", lambda e, h=h, bk=bk, sl=sl: e.scalar_tensor_tensor(
                        Z[:, h * 512:(h + 1) * 512], Hres[sl][:, h * 512:(h + 1) * 512], ALPHA, ps[bk][:, :], ALU.mult, ALU.add),
                        reads=[hrb[sl], psb[bk]], writes=[b_Z])
                ln(Z, b_Z, H1[s][:, :], h1b[s], Gt, Bt, b_gb)
                P.dma("pool", io["h1_d"][t0:t0 + 128, :], H1[s][:, :], reads=[h1b[s]])

            def emit_tr(j):
                s = j % 2
                for q in range(2):
                    bk = self.next_bank()
                    for k in range(4):
                        dc = q * 4 + k
                        P.op("pe", lambda e, dc=dc, k=k, bk=bk, s=s: e.transpose(
                            ps[bk][:, k * 128:(k + 1) * 128], H1[s][:, dc * 128:(dc + 1) * 128], ident[:, :]),
                            reads=[h1b[s]], writes=[psb[bk]], inc=(k == 3))
                    P.op("act", lambda e, q=q, bk=bk: e.activation(
                        out=HTf[:, q * 4:(q + 1) * 4, :], in_=ps[bk][:, :].rearrange("p (a b) -> p a b", b=128), func=AF.Copy),
                        reads=[psb[bk]], writes=[b_HTf])
                bk = self.next_bank()
                for dc in range(DC):
                    P.op("pe", lambda e, dc=dc, bk=bk: e.matmul(ps[bk][:, 0:36], HTf[:, dc, :], Wr[:, dc, :],
                                                                 start=(dc == 0), stop=(dc == DC - 1)),
                         reads=[b_HTf, b_Wr], writes=[psb[bk]], inc=(dc == DC - 1))
                return bk

            def emit_chain(j, bk):
                R = [b_r]
                P.op("dve", lambda e, bk=bk: e.tensor_tensor(Lg[:, :], ps[bk][:, 0:36], Br[:, :], ALU.add), reads=[psb[bk], b_Wr], writes=R)
                P.op("dve", lambda e: e.reduce_max(sm[:, 0:1], Lg[:, 0:4], AX.X), reads=R, writes=R)
                P.op("dve", lambda e: e.tensor_scalar(oh[:, :], Lg[:, 0:4], sm[:, 0:1], None, ALU.is_ge), reads=R, writes=R)
                P.op("dve", lambda e: e.tensor_scalar(sm[:, 1:2], sm[:, 0:1], -1.0, None, ALU.mult), reads=R, writes=R)
                P.op("act", lambda e: e.activation(out=ge[:, :], in_=Lg[:, 0:4], func=AF.Exp, bias=sm[:, 1:2], scale=1.0), reads=R, writes=R)
                P.op("dve", lambda e: e.reduce_sum(sm[:, 2:3], ge[:, :], AX.X), reads=R, writes=R)
                P.op("dve", lambda e: e.tensor_scalar(pen[:, :], oh[:, :], 1.0, 1e30, ALU.subtract, ALU.mult), reads=R, writes=R)
                for g in range(4):
                    P.op("dve", lambda e, g=g: e.tensor_scalar(EM[:, g * 8:(g + 1) * 8], Lg[:, 4 + g * 8:4 + (g + 1) * 8],
                                                                 pen[:, g:g + 1], None, ALU.add), reads=R, writes=R)
                P.op("dve", lambda e: e.max(out=M8[:, :], in_=EM[:, :]), reads=R, writes=R)
                P.op("dve", lambda e: e.tensor_scalar(sel[:, :], EM[:, :], M8[:, 1:2], None, ALU.is_ge), reads=R, writes=R)
                P.op("dve", lambda e: e.tensor_scalar(sm[:, 3:4], M8[:, 0:1], -1.0, None, ALU.mult), reads=R, writes=R)
                P.op("act", lambda e: e.activation(out=ew[:, :], in_=EM[:, :], func=AF.Exp, bias=sm[:, 3:4], scale=1.0), reads=R, writes=R)
                P.op("act", lambda e: e.activation(out=e2[:, :], in_=M8[:, 0:2], func=AF.Exp, bias=sm[:, 3:4], scale=1.0), reads=R, writes=R)
                P.op("dve", lambda e: e.reduce_sum(sm[:, 4:5], e2[:, :], AX.X), reads=R, writes=R)
                P.op("dve", lambda e: e.tensor_tensor(sm[:, 5:6], sm[:, 4:5], sm[:, 2:3], ALU.mult), reads=R, writes=R)
                P.op("dve", lambda e: e.reciprocal(sm[:, 6:7], sm[:, 5:6]), reads=R, writes=R)
                SELt, SEL1t, W12t = GATES
                P.op("dve", lambda e, j=j: e.tensor_copy(out=SELt[:, j, :], in_=sel[:, :]), reads=R, writes=R)
                P.op("dve", lambda e, j=j: e.tensor_scalar(SEL1t[:, j, :], EM[:, :], M8[:, 0:1], None, ALU.is_ge), reads=R, writes=R)
                P.op("dve", lambda e, j=j: e.tensor_copy(out=W12t[:, j, 0:1], in_=sm[:, 6:7]), reads=R, writes=R)
                P.op("dve", lambda e, j=j: e.tensor_tensor(W12t[:, j, 1:2], sm[:, 6:7], e2[:, 1:2], ALU.mult), reads=R, writes=R)

            load_res(0, 0)
            if NJ > 1:
                load_res(1, 1)
            emit_wo(0)
            emit_ln(0)
            for j in range(NJ):
                if j + 2 < NJ:
                    load_res(j + 2, (j + 2) % NR)
                if j + 1 < NJ:
                    emit_wo(j + 1)
                rbk = emit_tr(j)
                if j + 1 < NJ:
                    emit_ln(j + 1)
                emit_chain(j, rbk)

    def _moe(self, li, io, GATES):
        nc, P = self.nc, self.P
        NJ, TO = self.NJ, self.TO
        ps, psb = self.ps, self.psb
        npass = (NJ + 10) // 11
        base = NJ // npass
        sizes = [base + (1 if i < NJ % npass else 0) for i in range(npass)]
        PBmax = max(sizes)
        with ExitStack() as es:
            XTs = self.sb(es, "XTs", [128, DC, PBmax * 128], BF16)
            b_X = Buf()
            ACC = self.sb(es, "ACC", [128, PBmax, 1024], F32)
            accb = [Buf() for _ in range(PBmax)]
            Wg = [self.sb(es, "Wg%d" % i, [128, DC, DEXP], BF16) for i in range(2)]
            Wu = [self.sb(es, "Wu%d" % i, [128, DC, DEXP], BF16) for i in range(2)]
            Wd = [self.sb(es, "Wd%d" % i, [128, 4, 1024], BF16) for i in range(2)]
            wgb, wub, wdb = [Buf(), Buf()], [Buf(), Buf()], [Buf(), Buf()]
            AT = [self.sb(es, "AT%d" % i, [128, 4, 512], BF16) for i in range(2)]
            atb = [Buf(), Buf()]
            SG = [self.sb(es, "SG%d" % i, [128, 512], F32) for i in range(2)]
            sgb = [Buf(), Buf()]
            Gt = self.sb(es, "Gt2", [128, 1024], F32)
            Bt = self.sb(es, "Bt2", [128, 1024], F32)
            b_gb = Buf()
            P.dma("sp", Gt[:], io["lnf_g"].partition_broadcast(128), writes=[b_gb])
            P.dma("sp", Bt[:], io["lnf_b"].partition_broadcast(128), writes=[b_gb])
            Hres = [self.sb(es, "Hr2_%d" % i, [128, 1024], F32) for i in range(2)]
            hrb = [Buf(), Buf()]
            Z = self.sb(es, "Z2", [128, 1024], F32)
            b_Z = Buf()
            H2 = [self.sb(es, "H2_%d" % i, [128, 1024], F32) for i in range(2)]
            h2b = [Buf(), Buf()]
            ln = self._layernorm(es, "f")
            if io.get("sink", "final") != "final":
                HTo = [self.sb(es, "HTo%d" % i, [128, DC, 128], BF16) for i in range(2)]
                htob = [Buf(), Buf()]
                hT2_dv = io["hT2_d"].rearrange("(dc p) t -> p dc t", p=128)
            h1T_dv = io["H1T_d"].rearrange("(dc p) t -> p dc t", p=128)
            wg_v = io["w_gate"].rearrange("e (dc p) f -> e p dc f", p=128)
            wu_v = io["w_up"].rearrange("e (dc p) f -> e p dc f", p=128)
            wd_v = io["w_down"].rearrange("e (fc p) n -> e p fc n", p=128)

            def load_w(e):
                s = e % 2
                P.dma("pool", Wg[s][:], wg_v[e], writes=[wgb[s]])
                P.dma("pool", Wu[s][:], wu_v[e], writes=[wub[s]])
                P.dma("pool", Wd[s][:], wd_v[e], writes=[wdb[s]])

            tilec = 0
            fcc = 0
            jb = 0
            for pi in range(npass):
                PB = sizes[pi]
                ntok = PB * 128
                tok0 = jb * 128
                P.dma("sp", XTs[:, :, 0:ntok], h1T_dv[:, :, tok0:tok0 + ntok], writes=[b_X])
                load_w(0)
                for e in range(NEXP):
                    s = e % 2
                    if e + 1 < NEXP:
                        load_w(e + 1)
                    for off in range(0, ntok, 512):
                        n = min(512, ntok - off)
                        ta = tilec % 2
                        tilec += 1
                        for fc in range(4):
                            bg = [0, 1][fcc % 2]
                            bu = [2, 3][fcc % 2]
                            fcc += 1
                            for dc in range(DC):
                                P.op("pe", lambda e_, dc=dc, fc=fc, s=s, bg=bg, off=off, n=n: e_.matmul(
                                    ps[bg][:, 0:n], Wg[s][:, dc, fc * 128:(fc + 1) * 128], XTs[:, dc, off:off + n],
                                    start=(dc == 0), stop=(dc == DC - 1)),
                                    reads=[wgb[s], b_X], writes=[psb[bg]], inc=(dc == DC - 1))
                            for dc in range(DC):
                                P.op("pe", lambda e_, dc=dc, fc=fc, s=s, bu=bu, off=off, n=n: e_.matmul(
                                    ps[bu][:, 0:n], Wu[s][:, dc, fc * 128:(fc + 1) * 128], XTs[:, dc, off:off + n],
                                    start=(dc == 0), stop=(dc == DC - 1)),
                                    reads=[wub[s], b_X], writes=[psb[bu]], inc=(dc == DC - 1))
                            sg = fcc % 2
                            P.op("act", lambda e_, bg=bg, sg=sg, n=n: e_.activation(out=SG[sg][:, 0:n], in_=ps[bg][:, 0:n], func=AF.Silu),
                                 reads=[psb[bg]], writes=[sgb[sg]])
                            P.op("dve", lambda e_, bu=bu, sg=sg, ta=ta, fc=fc, n=n: e_.tensor_tensor(
                                AT[ta][:, fc, 0:n], SG[sg][:, 0:n], ps[bu][:, 0:n], ALU.mult),
                                reads=[sgb[sg], psb[bu]], writes=[atb[ta]])
                        for b in range(n // 128):
                            jj = (off // 128) + b
                            for h in range(2):
                                by = [4, 5, 6, 7][(b * 2 + h) % 4]
                                for fc in range(4):
                                    P.op("pe", lambda e_, fc=fc, b=b, h=h, by=by, ta=ta, s=s: e_.matmul(
                                        ps[by][:, :], AT[ta][:, fc, b * 128:(b + 1) * 128], Wd[s][:, fc, h * 512:(h + 1) * 512],
                                        start=(fc == 0), stop=(fc == 3)),
                                        reads=[atb[ta], wdb[s]], writes=[psb[by]], inc=(fc == 3))
                                gcol = GATES[:, jb + jj, e:e + 1]
                                if e == 0:
                                    P.op("dve", lambda e_, by=by, jj=jj, h=h, gcol=gcol: e_.tensor_scalar(
                                        ACC[:, jj, h * 512:(h + 1) * 512], ps[by][:, :], gcol, None, ALU.mult),
                                        reads=[psb[by]], writes=[accb[jj]])
                                else:
                                    P.op("dve", lambda e_, by=by, jj=jj, h=h, gcol=gcol: e_.scalar_tensor_tensor(
                                        ACC[:, jj, h * 512:(h + 1) * 512], ps[by][:, :], gcol, ACC[:, jj, h * 512:(h + 1) * 512],
                                        ALU.mult, ALU.add),
                                        reads=[psb[by], accb[jj]], writes=[accb[jj]])
                P.dma("sp", Hres[0][:], io["h1_d"][tok0:tok0 + 128, :], writes=[hrb[0]])
                for jj in range(PB):
                    s = jj % 2
                    j = jb + jj
                    if jj + 1 < PB:
                        P.dma("sp", Hres[1 - s][:], io["h1_d"][(j + 1) * 128:(j + 2) * 128, :], writes=[hrb[1 - s]])
                    P.op("dve", lambda e_, jj=jj, s=s: e_.scalar_tensor_tensor(
                        Z[:, :], Hres[s][:, :], ALPHA, ACC[:, jj, :], ALU.mult, ALU.add),
                        reads=[hrb[s], accb[jj]], writes=[b_Z])
                    ln(Z, b_Z, H2[s][:, :], h2b[s], Gt, Bt, b_gb)
                    sink = io.get("sink", "final")
                    if sink == "final":
                        P.dma("sp", io["h_out"][j * 128:(j + 1) * 128, :], H2[s][:, :], reads=[h2b[s]])
                    else:
                        gb = 2 * j + sink
                        P.dma("sp", io["h2_d"][gb * 128:(gb + 1) * 128, :], H2[s][:, :], reads=[h2b[s]])
                        for q in range(2):
                            bk = self.next_bank()
                            for k in range(4):
                                dc = q * 4 + k
                                P.op("pe", lambda e_, dc=dc, k=k, bk=bk, s=s: e_.transpose(
                                    ps[bk][:, k * 128:(k + 1) * 128], H2[s][:, dc * 128:(dc + 1) * 128], self.ident[:, :]),
                                    reads=[h2b[s]], writes=[psb[bk]], inc=(k == 3))
                            self.copy(self.ev_eng(), HTo[s][:, q * 4:(q + 1) * 4, :],
                                      ps[bk][:, :].rearrange("p (a b) -> p a b", b=128), [psb[bk]], [htob[s]])
                        P.dma("sp", hT2_dv[:, :, gb * 128:(gb + 1) * 128], HTo[s][:, :, :], reads=[htob[s]])
                jb += PB


    def queue_wconv(self, io):
        lst = getattr(self, "pending_conv", [])
        Wall_d = io["Wall_d"]
        for e in range(NEXP):
            rows = Wall_d[e * 128:(e + 1) * 128, :]
            lst.append((rows[:, 0:4096].rearrange("p (dc f) -> p dc f", f=512),
                        io["w_gate"][e].rearrange("(dc p) f -> p dc f", p=128)))
            lst.append((rows[:, 4096:8192].rearrange("p (dc f) -> p dc f", f=512),
                        io["w_up"][e].rearrange("(dc p) f -> p dc f", p=128)))
            lst.append((rows[:, 8192:12288].rearrange("p (fc n) -> p fc n", n=1024),
                        io["w_down"][e].rearrange("(fc p) n -> p fc n", p=128)))
        self.pending_conv = lst

    def _moe_routed(self, li, io, GATES):
        nc, P = self.nc, self.P
        NJ, TO = self.NJ, self.TO
        ps, psb = self.ps, self.psb
        SEL, SEL1, W12 = GATES
        TS = 256
        NT = (2 * TO + TS - 1) // TS + NEXP
        Xs_d, Ys_d, Wall_d = io["Xs_d"], io["Ys_d"], io["Wall_d"]
        ones_bf = self.ones_bf
        for (dst, src) in getattr(self, "pending_conv", []):
            P.dma("pool", dst, src, bulk=True)
        self.pending_conv = []
        with ExitStack() as es:
            LT = self.sb(es, "LT", [128, 128], BF16)
            iota = self.sb(es, "iota", [128, 1], F32)
            SELb = self.sb(es, "SELb", [128, NJ, NEXP], BF16)
            RSb = self.sb(es, "RSb", [128, NJ, NEXP], BF16)
            RANK = self.sb(es, "RANK", [128, NJ, NEXP], F32)
            TMP = self.sb(es, "TMPr", [128, NJ, NEXP], F32)
            SEL2 = self.sb(es, "SEL2", [128, NJ, NEXP], F32)
            TOT = self.sb(es, "TOT", [128, NEXP], F32)
            M1 = self.sb(es, "M1", [128, NEXP], F32)
            PADD = self.sb(es, "PADD", [128, NEXP], F32)
            END = self.sb(es, "END", [128, NEXP], F32)
            START = self.sb(es, "START", [128, NEXP], F32)
            ones32 = self.sb(es, "ones32", [128, NEXP], F32)
            cmp_ = self.sb(es, "cmp", [128, NEXP], F32)
            POSf = self.sb(es, "POSf", [128, 2, NJ], F32)
            POSi = self.sb(es, "POSi", [128, 2, NJ], mybir.dt.int32)
            EID = self.sb(es, "EID", [128, NT], F32)
            IDX = self.sb(es, "IDX", [128, NT], mybir.dt.int32)
            b_c = Buf()
            R = [b_c]
            P.dma("pool", LT[:], io["ltri"], writes=R)
            P.dma("sp", iota[:], io["iota_p"], writes=R)
            P.op("dve", lambda e: e.memset(ones32[:], 1.0), writes=R)
            P.op("dve", lambda e: e.tensor_copy(out=SELb[:], in_=SEL[:]), reads=R, writes=R)
            P.op("dve", lambda e: e.tensor_copy(out=RSb[:, 0, :], in_=SELb[:, 0, :]), reads=R, writes=R)
            for j in range(1, NJ):
                P.op("dve", lambda e, j=j: e.tensor_tensor(RSb[:, j, :], RSb[:, j - 1, :], SELb[:, j, :], ALU.add), reads=R, writes=R)
            for j in range(NJ):
                bk = self.next_bank()
                P.op("pe", lambda e, j=j, bk=bk: e.matmul(ps[bk][:, 0:NEXP], LT[:, :], SELb[:, j, :], start=True, stop=(j == 0)),
                     reads=R, writes=[psb[bk]], inc=(j == 0))
                if j > 0:
                    P.op("pe", lambda e, j=j, bk=bk: e.matmul(ps[bk][:, 0:NEXP], ones_bf[:, :], RSb[:, j - 1, :], start=False, stop=True),
                         reads=R, writes=[psb[bk]])
                P.op("act", lambda e, j=j, bk=bk: e.activation(out=RANK[:, j, :], in_=ps[bk][:, 0:NEXP], func=AF.Copy),
                     reads=[psb[bk]] + R, writes=R)
            bk = self.next_bank()
            P.op("pe", lambda e, bk=bk: e.matmul(ps[bk][:, 0:NEXP], ones_bf[:, :], RSb[:, NJ - 1, :], start=True, stop=True),
                 reads=R, writes=[psb[bk]])
            P.op("act", lambda e, bk=bk: e.activation(out=TOT[:, :], in_=ps[bk][:, 0:NEXP], func=AF.Copy), reads=[psb[bk]] + R, writes=R)
            P.op("dve", lambda e: e.memset(M1[:, :], 0.0), reads=R, writes=R)
            for m in range((2 * TO + TS - 1) // TS + 1):
                P.op("dve", lambda e, m=m: e.scalar_tensor_tensor(M1[:, :], TOT[:, :], float(m * TS), M1[:, :], ALU.is_gt, ALU.add),
                     reads=R, writes=R)
            P.op("dve", lambda e: e.tensor_scalar(PADD[:, :], M1[:, :], float(TS), None, ALU.mult), reads=R, writes=R)
            P.op("dve", lambda e: e.tensor_tensor_scan(END[:, :], ones32[:, :], PADD[:, :], 0.0, ALU.mult, ALU.add), reads=R, writes=R)
            P.op("dve", lambda e: e.tensor_tensor(START[:, :], END[:, :], PADD[:, :], ALU.subtract), reads=R, writes=R)
            for j in range(NJ):
                P.op("dve", lambda e, j=j: e.tensor_tensor(RANK[:, j, :], RANK[:, j, :], START[:, :], ALU.add), reads=R, writes=R)
            P.op("dve", lambda e: e.tensor_tensor(TMP[:], SEL1[:], RANK[:], ALU.mult), reads=R, writes=R)
            P.op("dve", lambda e: e.reduce_sum(POSf[:, 0, :], TMP[:], AX.X), reads=R, writes=R)
            P.op("dve", lambda e: e.tensor_tensor(SEL2[:], SEL[:], SEL1[:], ALU.subtract), reads=R, writes=R)
            P.op("dve", lambda e: e.tensor_tensor(TMP[:], SEL2[:], RANK[:], ALU.mult), reads=R, writes=R)
            P.op("dve", lambda e: e.reduce_sum(POSf[:, 1, :], TMP[:], AX.X), reads=R, writes=R)
            P.op("dve", lambda e: e.tensor_copy(out=POSi[:], in_=POSf[:]), reads=R, writes=R)
            for t in range(NT):
                P.op("dve", lambda e, t=t: e.tensor_scalar(cmp_[:, :], END[:, :], float(t * TS), 0.0, ALU.is_le, ALU.add,
                                                           accum_out=EID[:, t:t + 1]), reads=R, writes=R)
            P.op("dve", lambda e: e.tensor_scalar(EID[:, :], EID[:, :], float(NEXP - 1), 128.0, ALU.min, ALU.mult), reads=R, writes=R)
            P.op("dve", lambda e: e.tensor_scalar(EID[:, :], EID[:, :], iota[:, 0:1], None, ALU.add), reads=R, writes=R)
            P.op("dve", lambda e: e.tensor_copy(out=IDX[:], in_=EID[:]), reads=R, writes=R)

            NHB = 6
            with ExitStack() as es_sc:
                Hb = [self.sb(es_sc, "Hb%d" % i, [128, 1024], F32) for i in range(NHB)]
                hbb = [Buf() for _ in range(NHB)]
                for j in range(NJ):
                    s = j % NHB
                    P.dma("sp", Hb[s][:], io["h1_d"][j * 128:(j + 1) * 128, :], writes=[hbb[s]])
                    for r in range(2):
                        P.idma(Xs_d[:, :], Hb[s][:, :], out_off=POSi[:, r, j:j + 1], reads=[hbb[s]] + R)
                P.barrier()

            Wall = [self.sb(es, "Wall%d" % i, [128, 12288], BF16) for i in range(2)]
            wlb = [Buf(), Buf()]
            Xsl = [self.sb(es, "Xsl%d" % i, [128, 2, 1024], F32) for i in range(2)]
            xslb = [Buf(), Buf()]
            XsT = [self.sb(es, "XsT%d" % i, [128, DC, TS], BF16) for i in range(2)]
            xstb = [Buf(), Buf()]
            AT = [self.sb(es, "ATr%d" % i, [128, 4, TS], BF16) for i in range(2)]
            atb = [Buf(), Buf()]
            SG = [self.sb(es, "SGr%d" % i, [128, TS], F32) for i in range(2)]
            sgb = [Buf(), Buf()]
            Yst = [self.sb(es, "Yst%d" % i, [128, 2, 1024], F32) for i in range(2)]
            ystb = [Buf(), Buf()]
            Xs_v = Xs_d.rearrange("(t s p) n -> t p s n", s=2, p=128)
            Ys_v = Ys_d.rearrange("(t s p) n -> t p s n", s=2, p=128)

            def load_tile(t):
                s = t % 2
                P.idma(Wall[s][:, :], Wall_d[:, :], in_off=IDX[:, t:t + 1], reads=R, writes=[wlb[s]])
                P.dma("sp", Xsl[s][:], Xs_v[t], writes=[xslb[s]])

            load_tile(0)
            fcc = 0
            for t in range(NT):
                s = t % 2
                if t + 1 < NT:
                    load_tile(t + 1)
                for sub in range(2):
                    for q in range(2):
                        bk = self.next_bank()
                        for k in range(4):
                            dc = q * 4 + k
                            P.op("pe", lambda e, dc=dc, k=k, bk=bk, s=s, sub=sub: e.transpose(
                                ps[bk][:, k * 128:(k + 1) * 128], Xsl[s][:, sub, dc * 128:(dc + 1) * 128], self.ident[:, :]),
                                reads=[xslb[s]], writes=[psb[bk]], inc=(k == 3))
                        self.copy(self.ev_eng(), XsT[s][:, q * 4:(q + 1) * 4, sub * 128:(sub + 1) * 128],
                                  ps[bk][:, :].rearrange("p (a b) -> p a b", b=128), [psb[bk]], [xstb[s]])
                for fc in range(4):
                    bg = self.next_bank()
                    for dc in range(DC):
                        P.op("pe", lambda e, dc=dc, fc=fc, s=s, bg=bg: e.matmul(
                            ps[bg][:, 0:TS], Wall[s][:, dc * 512 + fc * 128:dc * 512 + (fc + 1) * 128], XsT[s][:, dc, :],
                            start=(dc == 0), stop=(dc == DC - 1)),
                            reads=[wlb[s], xstb[s]], writes=[psb[bg]], inc=(dc == DC - 1))
                    bu = self.next_bank()
                    for dc in range(DC):
                        P.op("pe", lambda e, dc=dc, fc=fc, s=s, bu=bu: e.matmul(
                            ps[bu][:, 0:TS], Wall[s][:, 4096 + dc * 512 + fc * 128:4096 + dc * 512 + (fc + 1) * 128], XsT[s][:, dc, :],
                            start=(dc == 0), stop=(dc == DC - 1)),
                            reads=[wlb[s], xstb[s]], writes=[psb[bu]], inc=(dc == DC - 1))
                    sg = fcc % 2
                    fcc += 1
                    P.op("act", lambda e, bg=bg, sg=sg: e.activation(out=SG[sg][:, :], in_=ps[bg][:, 0:TS], func=AF.Silu),
                         reads=[psb[bg]], writes=[sgb[sg]])
                    P.op("dve", lambda e, bu=bu, sg=sg, s=s, fc=fc: e.tensor_tensor(AT[s][:, fc, :], SG[sg][:, :], ps[bu][:, 0:TS], ALU.mult),
                         reads=[sgb[sg], psb[bu]], writes=[atb[s]])
                for sub in range(2):
                    for h in range(2):
                        by = self.next_bank()
                        for fc in range(4):
                            P.op("pe", lambda e, fc=fc, sub=sub, h=h, by=by, s=s: e.matmul(
                                ps[by][:, :], AT[s][:, fc, sub * 128:(sub + 1) * 128],
                                Wall[s][:, 8192 + fc * 1024 + h * 512:8192 + fc * 1024 + (h + 1) * 512],
                                start=(fc == 0), stop=(fc == 3)),
                                reads=[atb[s], wlb[s]], writes=[psb[by]], inc=(fc == 3))
                        self.copy(self.ev_eng(), Yst[s][:, sub, h * 512:(h + 1) * 512], ps[by][:, :], [psb[by]], [ystb[s]])
                P.dma("sp", Ys_v[t], Yst[s][:], reads=[ystb[s]])
            P.barrier()

            Gt = self.sb(es, "Gt2", [128, 1024], F32)
            Bt = self.sb(es, "Bt2", [128, 1024], F32)
            b_gb = Buf()
            P.dma("sp", Gt[:], io["lnf_g"].partition_broadcast(128), writes=[b_gb])
            P.dma("sp", Bt[:], io["lnf_b"].partition_broadcast(128), writes=[b_gb])
            Hres = [self.sb(es, "Hr2_%d" % i, [128, 1024], F32) for i in range(2)]
            hrb = [Buf(), Buf()]
            Y1 = [self.sb(es, "Y1_%d" % i, [128, 1024], F32) for i in range(2)]
            Y2 = [self.sb(es, "Y2_%d" % i, [128, 1024], F32) for i in range(2)]
            y1b, y2b = [Buf(), Buf()], [Buf(), Buf()]
            Z = self.sb(es, "Z2", [128, 1024], F32)
            b_Z = Buf()
            H2 = [self.sb(es, "H2_%d" % i, [128, 1024], F32) for i in range(2)]
            h2b = [Buf(), Buf()]
            ln = self._layernorm(es, "f")
            sink = io.get("sink", "final")
            if sink != "final":
                HTo = [self.sb(es, "HTo%d" % i, [128, DC, 128], BF16) for i in range(2)]
                htob = [Buf(), Buf()]
                hT2_dv = io["hT2_d"].rearrange("(dc p) t -> p dc t", p=128)

            def load_blk(j):
                s = j % 2
                P.dma("sp", Hres[s][:], io["h1_d"][j * 128:(j + 1) * 128, :], writes=[hrb[s]])
                P.idma(Y1[s][:, :], Ys_d[:, :], in_off=POSi[:, 0, j:j + 1], reads=R, writes=[y1b[s]])
                P.idma(Y2[s][:, :], Ys_d[:, :], in_off=POSi[:, 1, j:j + 1], reads=R, writes=[y2b[s]])

            load_blk(0)
            for j in range(NJ):
                s = j % 2
                if j + 1 < NJ:
                    load_blk(j + 1)
                P.op("dve", lambda e, j=j, s=s: e.tensor_scalar(Z[:, :], Y1[s][:, :], W12[:, j, 0:1], None, ALU.mult),
                     reads=[y1b[s]], writes=[b_Z])
                P.op("dve", lambda e, j=j, s=s: e.scalar_tensor_tensor(Z[:, :], Y2[s][:, :], W12[:, j, 1:2], Z[:, :], ALU.mult, ALU.add),
                     reads=[y2b[s], b_Z], writes=[b_Z])
                P.op("dve", lambda e, s=s: e.scalar_tensor_tensor(Z[:, :], Hres[s][:, :], ALPHA, Z[:, :], ALU.mult, ALU.add),
                     reads=[hrb[s], b_Z], writes=[b_Z])
                ln(Z, b_Z, H2[s][:, :], h2b[s], Gt, Bt, b_gb)
                if sink == "final":
                    P.dma("sp", io["h_out"][j * 128:(j + 1) * 128, :], H2[s][:, :], reads=[h2b[s]])
                else:
                    gb = 2 * j + sink
                    P.dma("sp", io["h2_d"][gb * 128:(gb + 1) * 128, :], H2[s][:, :], reads=[h2b[s]])
                    for q in range(2):
                        bk = self.next_bank()
                        for k in range(4):
                            dc = q * 4 + k
                            P.op("pe", lambda e_, dc=dc, k=k, bk=bk, s=s: e_.transpose(
                                ps[bk][:, k * 128:(k + 1) * 128], H2[s][:, dc * 128:(dc + 1) * 128], self.ident[:, :]),
                                reads=[h2b[s]], writes=[psb[bk]], inc=(k == 3))
                        self.copy(self.ev_eng(), HTo[s][:, q * 4:(q + 1) * 4, :],
                                  ps[bk][:, :].rearrange("p (a b) -> p a b", b=128), [psb[bk]], [htob[s]])
                    P.dma("sp", hT2_dv[:, :, gb * 128:(gb + 1) * 128], HTo[s][:, :, :], reads=[htob[s]])


def build_fused_program(LP):
    nc = bass.Bass("TRN2", target_bir_lowering=False)
    NB = LP // 128
    NJ = NB // 2
    TO = NJ * 128

    def din(name, shape, dt=F32):
        return nc.dram_tensor(name, list(shape), dt, kind="ExternalInput").ap()

    def dscr(name, shape, dt):
        return nc.dram_tensor(name, list(shape), dt, kind="Internal").ap()

    g = {}
    g["xT_seq0"] = din("xT_seq0", [D, LP])
    g["xT_own0"] = din("xT_own0", [2, D, TO])
    g["x_own0"] = din("x_own0", [2, TO, D])
    g["ident"] = din("ident", [128, 128])
    g["nb_mask_s"] = din("nb_mask_s", [2, 3, 128, 128])
    g["nb_T_s"] = din("nb_T_s", [2, 3, 16, 128, 128])
    g["nb_mask_d"] = din("nb_mask_d", [3, 128, 128])
    g["b31"] = din("b31", [16])
    g["lam"] = din("lam", [256])
    g["subln"] = din("subln", [128, 1])
    g["b_f"] = din("b_f", [16, 1])
    g["npar"] = din("npar", [16, 2])
    g["par"] = din("par", [128, 2])
    g["pars"] = din("pars", [128, 2])
    for li in range(2):
        g["w_in%d" % li] = din("w_in%d" % li, [D, 3072 if li == 0 else 3088])
        g["w_o%d" % li] = din("w_o%d" % li, [D, D])
        for k in ("lnm_g", "lnm_b", "lnf_g", "lnf_b"):
            g["%s%d" % (k, li)] = din("%s%d" % (k, li), [D])
        g["w_r%d" % li] = din("w_r%d" % li, [D, 36])
        g["b_r%d" % li] = din("b_r%d" % li, [36])
        g["w_gate%d" % li] = din("w_gate%d" % li, [NEXP, D, DEXP])
        g["w_up%d" % li] = din("w_up%d" % li, [NEXP, D, DEXP])
        g["w_down%d" % li] = din("w_down%d" % li, [NEXP, DEXP, D])
    g["CQ3_d"] = dscr("CQ3_d", [16, 3, TO], BF16)
    g["KT_d"] = dscr("KT_d", [D, LP], BF16)
    g["QT_d2"] = dscr("QT_d2", [2, D, TO], BF16)
    g["QT_d"] = g["QT_d2"][0]
    g["V_d"] = dscr("V_d", [LP, D], BF16)
    g["h1_d"] = dscr("h1_d", [TO, D], F32)
    g["H1T_d"] = dscr("H1T_d", [D, TO], BF16)
    g["h2_d"] = dscr("h2_d", [LP, D], F32)
    g["hT2_d"] = dscr("hT2_d", [D, LP], BF16)
    g["h_out"] = nc.dram_tensor("h_out", [TO, D], F32, kind="ExternalOutput").ap()
    NT = (2 * TO + 255) // 256 + NEXP
    g["Xs_d"] = dscr("Xs_d", [NT * 256, D], F32)
    g["Ys_d"] = dscr("Ys_d", [NT * 256, D], F32)
    g["Wall_d0"] = dscr("Wall_d0", [NEXP * 128, 12288], BF16)
    g["Wall_d1"] = dscr("Wall_d1", [NEXP * 128, 12288], BF16)
    g["ltri"] = din("ltri", [128, 128])
    g["iota_p"] = din("iota_p", [128, 1])

    def layer_io(li):
        io = {}
        for k in ("w_in", "w_o", "lnm_g", "lnm_b", "lnf_g", "lnf_b", "w_r", "b_r", "w_gate", "w_up", "w_down"):
            io[k] = g["%s%d" % (k, li)]
        for k in ("ident", "KT_d", "QT_d", "V_d", "h1_d", "H1T_d", "h2_d", "hT2_d", "h_out", "CQ3_d",
                  "b31", "lam", "subln", "b_f", "npar", "par", "pars", "Xs_d", "Ys_d", "ltri", "iota_p"):
            io[k] = g[k]
        io["Wall_d"] = g["Wall_d%d" % li]
        return io

    with ExitStack() as es:
        P = Prog(nc, es)
        em = LayerEmitter(nc, P, es, LP)
        for c in range(2):
            io = layer_io(0)
            io.update(xT_seq=g["xT_seq0"], x_own=g["x_own0"][c],
                      nb_mask=g["nb_mask_s"][c], nb_T=g["nb_T_s"][c],
                      do_kv=(c == 0), q_mode="static", res_mode="static", sink=c)
            io["QT_d"] = g["QT_d2"][c]
            io["q_jobs"] = [(g["xT_own0"][cc], g["QT_d2"][cc]) for cc in range(2)] if c == 0 else []
            em.queue_wconv(layer_io(c))
            with ExitStack() as es_layer:
                em.emit_layer("diff", 0, io, es_layer)
            P.barrier()
        io = layer_io(1)
        io.update(xT_seq=g["hT2_d"], nb_mask=g["nb_mask_d"], do_kv=True, q_mode="dyn", res_mode="dyn", sink="final")
        with ExitStack() as es_layer:
            em.emit_layer("fox", 1, io, es_layer)
        P.barrier()
    return nc, P


def _t5_bucket_np(dist):
    n = np.maximum(np.asarray(dist, np.int32), 0)
    nf = np.maximum(n, 1).astype(np.float32)
    large = 16 + (np.log(nf / np.float32(16)) / np.float32(math.log(128 / 16)) * np.float32(16)).astype(np.int32)
    large = np.minimum(large, 31)
    return np.where(n < 16, n, large)


def _near_tables(c):
    qi = np.arange(128)[None, :]
    ki = np.arange(128)[:, None]
    idx = np.zeros((3, 128, 128), np.int32)
    msk = np.zeros((3, 128, 128), np.float32)
    for s in range(3):
        dist = (c + 1 - s) * 128 + qi - ki
        idx[s] = _t5_bucket_np(np.maximum(dist, 0))
        msk[s] = np.where(dist < 0, NEGM, 0.0)
    return idx, msk


_PROG_CACHE = {}


def _get_prog(LP):
    if LP not in _PROG_CACHE:
        _PROG_CACHE[LP] = build_fused_program(LP)[0]
    return _PROG_CACHE[LP]


def kernel(**inputs):
    inp = {k: np.asarray(v) for k, v in inputs.items()}
    x = inp["x"].astype(np.float32, copy=False)
    B, S, _ = x.shape
    L = S + N_META
    LP = ((L + 255) // 256) * 256
    NB = LP // 128
    NJ = NB // 2
    h = np.zeros((B, LP, D), np.float32)
    h[:, :N_META] = inp["meta_tokens"][None]
    h[:, N_META:L] = x
    nc = _get_prog(LP)
    ca = np.ascontiguousarray
    common = {"ident": np.eye(128, dtype=np.float32)}
    common["ltri"] = np.triu(np.ones((128, 128), np.float32), 1)
    common["iota_p"] = np.arange(128, dtype=np.float32).reshape(128, 1)
    idx0, msk0 = _near_tables(0)
    idx1, msk1 = _near_tables(1)
    rb = inp["rel_bias"]
    common["nb_mask_s"] = ca(np.stack([msk0, msk1]))
    common["nb_T_s"] = ca(np.stack([np.transpose(rb[idx0], (0, 3, 1, 2)), np.transpose(rb[idx1], (0, 3, 1, 2))]))
    common["b31"] = ca(rb[31])
    common["lam"] = ca(inp["diff_lambda"][0].reshape(256))
    common["subln"] = ca(inp["diff_subln"][0].reshape(128, 1))
    common["b_f"] = ca(inp["fox_b_f"][0].reshape(16, 1))
    for li in range(2):
        common["w_in%d" % li] = ca(inp["diff_w_qkv"][0] if li == 0 else inp["fox_w_in"][0])
        common["w_o%d" % li] = ca(inp["diff_w_o"][0] if li == 0 else inp["fox_w_o"][0])
        common["lnm_g%d" % li] = ca(inp["ln_mix_g"][li])
        common["lnm_b%d" % li] = ca(inp["ln_mix_b"][li])
        common["lnf_g%d" % li] = ca(inp["ln_ffn_g"][li])
        common["lnf_b%d" % li] = ca(inp["ln_ffn_b"][li])
        common["w_r%d" % li] = ca(np.concatenate([inp["router_group_w"][li], inp["router_expert_w"][li].reshape(D, 32)], axis=1))
        common["b_r%d" % li] = ca(np.concatenate([inp["router_group_b"][li], inp["router_expert_b"][li].reshape(32)]))
        common["w_gate%d" % li] = ca(inp["expert_w_gate"][li])
        common["w_up%d" % li] = ca(inp["expert_w_up"][li])
        common["w_down%d" % li] = ca(inp["expert_w_down"][li])
    in_maps = []
    for b in range(B):
        hT = ca(h[b].T)
        hb = h[b].reshape(NJ, 2, 128, D)
        own = ca(np.transpose(hb, (1, 0, 2, 3)).reshape(2, NJ * 128, D))
        ownT = ca(np.transpose(own, (0, 2, 1)))
        for c in range(2):
            m = dict(common)
            m["xT_seq0"] = hT
            m["x_own0"] = own
            m["xT_own0"] = ownT
            m["nb_mask_d"] = msk0 if c == 0 else msk1
            sel = np.zeros((128, 2), np.float32)
            sel[:, c] = 1.0
            m["par"] = sel
            m["pars"] = sel * np.float32(0.125)
            m["npar"] = ca(-sel[:16])
            in_maps.append(m)
    res = run_bass_kernel_spmd(nc, in_maps, core_ids=list(range(len(in_maps))))
    out = np.zeros((B, NJ, 2, 128, D), np.float32)
    k = 0
    for b in range(B):
        for c in range(2):
            out[b, :, c] = np.asarray(res.results[k]["h_out"]).reshape(NJ, 128, D)
            k += 1
    return ca(out.reshape(B, LP, D)[:, N_META:L])
```
